# Optimizing a Trainium2 kernel written in Bass

```python
import math
import jax
import jax.numpy as jnp
from jax import lax
import numpy as np

D_MODEL = 2048
BATCH = 4
SEQ = 2048
DEPTH = 2

CTX_LEN = 256
GRID_W = 64
N_DENSE_LAYERS = (DEPTH + 1) // 2
N_MOE_LAYERS = DEPTH // 2
EPS = 1e-6

POOL_WINDOWS = (2, 4, 8, 16)
POOL_GROUPS = len(POOL_WINDOWS)
POOL_WIDTH = D_MODEL // 2
POOL_GROUP_DIM = POOL_WIDTH // POOL_GROUPS
SGU_WIDTH = D_MODEL // 2
SGU_GROUPS = 8
SGU_GROUP_DIM = SGU_WIDTH // SGU_GROUPS
CHUNK = 128
DIFF_HEADS = D_MODEL // 256
DIFF_QK_DIM = 64
DIFF_V_DIM = 2 * DIFF_QK_DIM
DIFF_WIDTH = DIFF_HEADS * DIFF_V_DIM
ROPE_BASE = 10000.0
Q_BLOCK = 128
N_BRANCHES = 3
OFF_A = 0
OFF_B = OFF_A + POOL_WIDTH
OFF_Q = OFF_B + 2 * SGU_WIDTH
OFF_K = OFF_Q + DIFF_HEADS * 2 * DIFF_QK_DIM
OFF_V = OFF_K + DIFF_HEADS * 2 * DIFF_QK_DIM
OFF_G = OFF_V + DIFF_WIDTH
IN_WIDTH = OFF_G + N_BRANCHES * D_MODEL
D_FF = 5632
N_EXPERTS = 8
TOP_K = 2
D_FF_EXPERT = D_MODEL * 7 // 2

kernel_name = 'hybrid_pool_sgu_diffattn_moe_dit'


def rms_norm(x, g):
    xf = x.astype(jnp.float32)
    y = xf * lax.rsqrt(jnp.mean(xf * xf, axis=-1, keepdims=True) + EPS)
    return (y * g.astype(jnp.float32)).astype(x.dtype)


def layer_norm(x, g):
    xf = x.astype(jnp.float32)
    mu = jnp.mean(xf, axis=-1, keepdims=True)
    var = jnp.mean(jnp.square(xf - mu), axis=-1, keepdims=True)
    return ((xf - mu) * lax.rsqrt(var + EPS) * g.astype(jnp.float32)).astype(x.dtype)


def modulate(h, shift, scale):
    return h * (1 + scale) + shift


def axial_rope_tables(rows):
    half = DIFF_QK_DIM // 2
    inv_freq = jnp.power(ROPE_BASE, -jnp.arange(0, half, 2, dtype=jnp.float32) / half)
    row = jnp.repeat(jnp.arange(rows, dtype=jnp.float32), GRID_W)
    col = jnp.tile(jnp.arange(GRID_W, dtype=jnp.float32), rows)
    ang_r = row[:, None] * inv_freq[None, :]
    ang_c = col[:, None] * inv_freq[None, :]
    ang = jnp.concatenate([ang_r, ang_r, ang_c, ang_c], axis=-1)
    return jnp.cos(ang), jnp.sin(ang)


def rotate_half(y):
    y1, y2 = jnp.split(y, 2, axis=-1)
    return jnp.concatenate([-y2, y1], axis=-1)


def apply_axial_rope(x, cos, sin):
    xr, xc = jnp.split(x, 2, axis=-1)
    rotated = jnp.concatenate([rotate_half(xr), rotate_half(xc)], axis=-1)
    return x * cos.astype(x.dtype) + rotated * sin.astype(x.dtype)


def multiscale_pool(a, w_pool, pool_scale):
    B, S, _ = a.shape
    af = a.astype(jnp.float32)
    cs = jnp.concatenate([jnp.zeros((B, 1, POOL_WIDTH), jnp.float32), jnp.cumsum(af, axis=1)], axis=1)
    t = jnp.arange(S)
    outs = []
    for g, w in enumerate(POOL_WINDOWS):
        lo = jnp.clip(t - w // 2, 0, S)
        hi = jnp.clip(t + w - w // 2, 0, S)
        sl = slice(g * POOL_GROUP_DIM, (g + 1) * POOL_GROUP_DIM)
        csg = cs[..., sl]
        cnt = (hi - lo).astype(jnp.float32)[None, :, None]
        outs.append((csg[:, hi] - csg[:, lo]) / cnt - af[..., sl])
    pooled = jnp.stack(outs, axis=2).astype(a.dtype)
    mixed = jnp.einsum('bsgc,gcd->bsgd', pooled, w_pool)
    return mixed.reshape(B, S, POOL_WIDTH) * pool_scale


def chunk_sgu(z, norm_g, w_s, b_s):
    B, S, _ = z.shape
    z = jax.nn.gelu(z)
    u, v = z[..., :SGU_WIDTH], z[..., SGU_WIDTH:]
    v = layer_norm(v, norm_g)
    v = v.reshape(B, S // CHUNK, CHUNK, SGU_GROUPS, SGU_GROUP_DIM)
    mixed = jnp.einsum('gpq,bnqgc->bnpgc', w_s, v) + b_s.T[None, None, :, :, None]
    return u * mixed.reshape(B, S, SGU_WIDTH)


def qk_heads(cols):
    B, S, _ = cols.shape
    return cols.reshape(B, S, DIFF_HEADS, 2, DIFF_QK_DIM).transpose(3, 0, 2, 1, 4)


def v_heads(cols):
    B, S, _ = cols.shape
    return cols.reshape(B, S, DIFF_HEADS, DIFF_V_DIM).transpose(0, 2, 1, 3)


def diff_attend(q, k, v, lam):
    _, B, H, Sq, dh = q.shape
    nb = Sq // Q_BLOCK
    scale = 1.0 / math.sqrt(DIFF_QK_DIM)
    qb = q.reshape(2, B, H, nb, Q_BLOCK, dh).transpose(3, 0, 1, 2, 4, 5)

    def one_block(qblk):
        s = jnp.einsum('ibhqd,ibhkd->ibhqk', qblk, k).astype(jnp.float32) * scale
        p = jax.nn.softmax(s, axis=-1)
        a = p[0] - lam * p[1]
        return jnp.einsum('bhqk,bhkd->bhqd', a.astype(v.dtype), v)

    o = lax.map(one_block, qb)
    return o.transpose(1, 2, 0, 3, 4).reshape(B, H, Sq, DIFF_V_DIM)


def diff_output(o, subln_g, lambda_init):
    B, H, S, _ = o.shape
    o = rms_norm(o, subln_g) * (1.0 - lambda_init)
    return o.transpose(0, 2, 1, 3).reshape(B, S, DIFF_WIDTH)


def merge_branches(p, a_out, b_out, c_out, w_pa, w_pb, w_pc, w_o):
    g = jax.nn.sigmoid(p[..., OFF_G:])
    ga, gb, gc = g[..., :D_MODEL], g[..., D_MODEL:2 * D_MODEL], g[..., 2 * D_MODEL:]
    merged = ga * (a_out @ w_pa) + gb * (b_out @ w_pb) + gc * (c_out @ w_pc)
    return merged @ w_o


def swiglu(h, w1, w3, w2):
    return (jax.nn.silu(h @ w1) * (h @ w3)) @ w2


def moe_swiglu(h, w_router, w1, w3, w2):
    logits = (h @ w_router).astype(jnp.float32)
    top_vals, top_idx = lax.top_k(logits, TOP_K)
    weights = jax.nn.softmax(top_vals, axis=-1)
    combine = jnp.sum(jax.nn.one_hot(top_idx, N_EXPERTS, dtype=jnp.float32) * weights[..., None], axis=-2)
    out = jnp.zeros_like(h)
    for e in range(N_EXPERTS):
        out = out + combine[..., e:e + 1].astype(h.dtype) * swiglu(h, w1[e], w3[e], w2[e])
    return out


def setup_inputs(seed: int = 0) -> dict:
    key = jax.random.key(seed)
    ks = iter(jax.random.split(key, 40))

    def nrm(shape, scale):
        return jax.random.normal(next(ks), shape, jnp.float32) * scale

    L, D = DEPTH, D_MODEL
    return {
        'x': nrm((BATCH, SEQ, D), 1.0),
        'c': nrm((BATCH, D), 1.0),
        'ctx': nrm((BATCH, CTX_LEN, D), 1.0),
        'c_ctx': nrm((D,), 1.0),
        'w_mod': nrm((L, D, 6 * D), 0.5 * D ** -0.5),
        'b_mod': nrm((L, 6 * D), 0.02),
        'norm1_g': 1.0 + nrm((L, D), 0.1),
        'norm2_g': 1.0 + nrm((L, D), 0.1),
        'w_in': nrm((L, D, IN_WIDTH), D ** -0.5),
        'pool_w': nrm((L, POOL_GROUPS, POOL_GROUP_DIM, POOL_GROUP_DIM), POOL_GROUP_DIM ** -0.5),
        'pool_scale': 1.0 + nrm((L, POOL_WIDTH), 0.1),
        'sgu_norm_g': 1.0 + nrm((L, SGU_WIDTH), 0.1),
        'sgu_w': nrm((L, SGU_GROUPS, CHUNK, CHUNK), CHUNK ** -0.5),
        'sgu_b': 1.0 + nrm((L, SGU_GROUPS, CHUNK), 0.1),
        'lambda_q1': nrm((L, DIFF_QK_DIM), 0.1),
        'lambda_k1': nrm((L, DIFF_QK_DIM), 0.1),
        'lambda_q2': nrm((L, DIFF_QK_DIM), 0.1),
        'lambda_k2': nrm((L, DIFF_QK_DIM), 0.1),
        'attn_subln_g': 1.0 + nrm((L, DIFF_V_DIM), 0.1),
        'w_proj_a': nrm((L, POOL_WIDTH, D), POOL_WIDTH ** -0.5),
        'w_proj_b': nrm((L, SGU_WIDTH, D), SGU_WIDTH ** -0.5),
        'w_proj_c': nrm((L, DIFF_WIDTH, D), DIFF_WIDTH ** -0.5),
        'w_o': nrm((L, D, D), D ** -0.5),
        'ffn_w1': nrm((N_DENSE_LAYERS, D, D_FF), D ** -0.5),
        'ffn_w3': nrm((N_DENSE_LAYERS, D, D_FF), D ** -0.5),
        'ffn_w2': nrm((N_DENSE_LAYERS, D_FF, D), D_FF ** -0.5),
        'moe_router': nrm((N_MOE_LAYERS, D, N_EXPERTS), D ** -0.5),
        'moe_w1': nrm((N_MOE_LAYERS, N_EXPERTS, D, D_FF_EXPERT), D ** -0.5),
        'moe_w3': nrm((N_MOE_LAYERS, N_EXPERTS, D, D_FF_EXPERT), D ** -0.5),
        'moe_w2': nrm((N_MOE_LAYERS, N_EXPERTS, D_FF_EXPERT, D), D_FF_EXPERT ** -0.5),
        'final_norm_g': 1.0 + nrm((D,), 0.1),
    }


def reference(x, c, ctx, c_ctx, w_mod, b_mod, norm1_g, norm2_g, w_in, pool_w, pool_scale,
              sgu_norm_g, sgu_w, sgu_b, lambda_q1, lambda_k1, lambda_q2, lambda_k2, attn_subln_g,
              w_proj_a, w_proj_b, w_proj_c, w_o, ffn_w1, ffn_w3, ffn_w2,
              moe_router, moe_w1, moe_w3, moe_w2, final_norm_g):
    S = x.shape[1]
    ROWS = S // GRID_W
    cos, sin = axial_rope_tables(ROWS)

    for l in range(DEPTH):
        last = l == DEPTH - 1
        lambda_init = 0.8 - 0.6 * math.exp(-0.3 * l)
        mod = jax.nn.silu(c) @ w_mod[l] + b_mod[l]
        sh1, sc1, gt1, sh2, sc2, gt2 = jnp.split(mod[:, None, :], 6, axis=-1)
        mod_c = jax.nn.silu(c_ctx) @ w_mod[l] + b_mod[l]
        csh1, csc1, cgt1, csh2, csc2, cgt2 = jnp.split(mod_c, 6, axis=-1)
        lam = (jnp.exp(jnp.sum(lambda_q1[l] * lambda_k1[l]).astype(jnp.float32))
               - jnp.exp(jnp.sum(lambda_q2[l] * lambda_k2[l]).astype(jnp.float32)) + lambda_init)

        h = modulate(rms_norm(x, norm1_g[l]), sh1, sc1)
        hc = modulate(rms_norm(ctx, norm1_g[l]), csh1, csc1)
        p = h @ w_in[l]
        q = apply_axial_rope(qk_heads(p[..., OFF_Q:OFF_K]), cos, sin)
        k = apply_axial_rope(qk_heads(p[..., OFF_K:OFF_V]), cos, sin)
        v = v_heads(p[..., OFF_V:OFF_G])
        if last:
            pc_kv = hc @ w_in[l][:, OFF_K:OFF_G]
            kc = qk_heads(pc_kv[..., :OFF_V - OFF_K])
            vc = v_heads(pc_kv[..., OFF_V - OFF_K:])
        else:
            pc = hc @ w_in[l]
            kc = qk_heads(pc[..., OFF_K:OFF_V])
            vc = v_heads(pc[..., OFF_V:OFF_G])
        k_all = jnp.concatenate([k, kc], axis=3)
        v_all = jnp.concatenate([v, vc], axis=2)
        c_out = diff_output(diff_attend(q, k_all, v_all, lam), attn_subln_g[l], lambda_init)
        a_out = multiscale_pool(p[..., OFF_A:OFF_B], pool_w[l], pool_scale[l])
        b_out = chunk_sgu(p[..., OFF_B:OFF_Q], sgu_norm_g[l], sgu_w[l], sgu_b[l])
        x = x + gt1 * merge_branches(p, a_out, b_out, c_out, w_proj_a[l], w_proj_b[l], w_proj_c[l], w_o[l])
        if not last:
            qc = qk_heads(pc[..., OFF_Q:OFF_K])
            cc_out = diff_output(diff_attend(qc, kc, vc, lam), attn_subln_g[l], lambda_init)
            ac_out = multiscale_pool(pc[..., OFF_A:OFF_B], pool_w[l], pool_scale[l])
            bc_out = chunk_sgu(pc[..., OFF_B:OFF_Q], sgu_norm_g[l], sgu_w[l], sgu_b[l])
            ctx = ctx + cgt1 * merge_branches(pc, ac_out, bc_out, cc_out,
                                              w_proj_a[l], w_proj_b[l], w_proj_c[l], w_o[l])

        h2 = modulate(rms_norm(x, norm2_g[l]), sh2, sc2)
        if l % 2 == 0:
            e = l // 2
            x = x + gt2 * swiglu(h2, ffn_w1[e], ffn_w3[e], ffn_w2[e])
            if not last:
                hc2 = modulate(rms_norm(ctx, norm2_g[l]), csh2, csc2)
                ctx = ctx + cgt2 * swiglu(hc2, ffn_w1[e], ffn_w3[e], ffn_w2[e])
        else:
            e = l // 2
            x = x + gt2 * moe_swiglu(h2, moe_router[e], moe_w1[e], moe_w3[e], moe_w2[e])
            if not last:
                hc2 = modulate(rms_norm(ctx, norm2_g[l]), csh2, csc2)
                ctx = ctx + cgt2 * moe_swiglu(hc2, moe_router[e], moe_w1[e], moe_w3[e], moe_w2[e])

    return rms_norm(x, final_norm_g)
```

```python
import numpy as np
import ml_dtypes
import concourse.bass as bass
import concourse.mybir as mybir
from concourse.bass_utils import run_bass_kernel_spmd

F32 = mybir.dt.float32
BF16 = mybir.dt.bfloat16
AF = mybir.ActivationFunctionType
ALU = mybir.AluOpType
AX = mybir.AxisListType

D = 2048
KC = 16
NH = 8
OFF_A, OFF_B, OFF_Q, OFF_K, OFF_V, OFF_G, INW = 0, 1024, 3072, 4096, 5120, 6144, 12288
EPS = 1e-6
NE = 8
FULL_CFG = dict(SEQ=2048, CTX=256, DFF=5632, DFFE=7168)
SAME_ENGINE_SYNC = True
NDS = 6
ARENA32 = 46 * 1024


class Reg:
    __slots__ = ("w", "rs")

    def __init__(self):
        self.w = None
        self.rs = {}


class Op:
    __slots__ = ("eng", "fn", "deps", "dma", "idx", "inc", "cnt", "dslot", "dval")


class Sched:
    ENG = ("pe", "act", "dve", "pool", "sp")

    def __init__(self):
        self.ops = {e: [] for e in self.ENG}
        self.ndma = {e: 0 for e in self.ENG}
        self.lastdma = {}

    def add(self, eng, fn, reads=(), writes=(), dma=False):
        op = Op()
        op.eng, op.fn, op.dma, op.inc, op.cnt = eng, fn, dma, False, 0
        deps = []
        for r in reads:
            if r.w is not None:
                deps.append(r.w)
        for w in writes:
            if w.w is not None:
                deps.append(w.w)
            deps.extend(w.rs.values())
        op.deps = deps
        op.idx = len(self.ops[eng])
        self.ops[eng].append(op)
        if dma:
            n = self.ndma[eng]
            self.ndma[eng] += 1
            op.dslot = n % NDS
            op.dval = 16 * (n // NDS + 1)
            self.lastdma[(eng, op.dslot)] = op
            key = (eng, "d", op.idx)
        else:
            key = eng
        for r in reads:
            r.rs[key] = op
        for w in writes:
            w.w = op
            w.rs = {}
        return op

    def barrier(self):
        lasts = []
        for e in self.ENG:
            for o in reversed(self.ops[e]):
                if o.fn is not None and not o.dma:
                    lasts.append(o)
                    break
        lasts.extend(self.lastdma.values())
        for e in self.ENG:
            op = Op()
            op.eng, op.fn, op.dma, op.inc, op.cnt = e, None, False, False, 0
            op.deps = list(lasts)
            op.idx = len(self.ops[e])
            self.ops[e].append(op)

    def _skip(self, op, d):
        if d.dma or op.dma or op.fn is None:
            return False
        if d.eng != op.eng:
            return False
        return op.eng == "pe" or not SAME_ENGINE_SYNC

    def finalize(self):
        for e in self.ENG:
            for op in self.ops[e]:
                for d in op.deps:
                    if not d.dma and not self._skip(op, d):
                        d.inc = True
        for e in self.ENG:
            c = 0
            for op in self.ops[e]:
                if op.inc:
                    c += 1
                op.cnt = c

    def emit(self, e, eng, csem, dsem):
        waited = {}
        for op in self.ops[e]:
            need = {}
            for d in op.deps:
                if d.dma:
                    key, val = ("d", d.eng, d.dslot), d.dval
                else:
                    if self._skip(op, d):
                        continue
                    key, val = d.eng, d.cnt
                if waited.get(key, 0) < val and need.get(key, 0) < val:
                    need[key] = val
            if op.dma and op.dval > 16:
                key = ("d", e, op.dslot)
                val = op.dval - 16
                if waited.get(key, 0) < val and need.get(key, 0) < val:
                    need[key] = val
            for key, val in need.items():
                sem = dsem[key[1]][key[2]] if isinstance(key, tuple) else csem[key]
                eng.wait_ge(sem, val)
                waited[key] = val
            if op.fn is None:
                continue
            ins = op.fn(eng)
            if op.dma:
                ins.then_inc(dsem[e][op.dslot], 16)
            elif op.inc:
                ins.then_inc(csem[e], 1)
        for (qe, slot), o in self.lastdma.items():
            if qe == e:
                eng.wait_ge(dsem[e][slot], o.dval)


def MM(out, lhsT, rhs, start=True, stop=True):
    return lambda e: e.matmul(out, lhsT=lhsT, rhs=rhs, start=start, stop=stop)


def ACT(out, in_, func, bias=None, scale=None):
    kw = {}
    if bias is not None:
        kw["bias"] = bias
    if scale is not None:
        kw["scale"] = scale
    return lambda e: e.activation(out=out, in_=in_, func=func, **kw)


def TT(out, a, b, op):
    return lambda e: e.tensor_tensor(out=out, in0=a, in1=b, op=op)


def TS(out, a, s1, op0, s2=None, op1=None):
    if op1 is None:
        return lambda e: e.tensor_scalar(out=out, in0=a, scalar1=s1, scalar2=None, op0=op0)
    return lambda e: e.tensor_scalar(out=out, in0=a, scalar1=s1, scalar2=s2, op0=op0, op1=op1)


def STT(out, in0, scalar, in1, op0, op1):
    return lambda e: e.scalar_tensor_tensor(out=out, in0=in0, scalar=scalar, in1=in1, op0=op0, op1=op1)


def CP(out, in_):
    return lambda e: e.tensor_copy(out=out, in_=in_)


def DMA(out, in_):
    return lambda e: e.dma_start(out=out, in_=in_)


def blocks(n):
    return [(o, min(512, n - o)) for o in range(0, n, 512)]


class Rot:
    def __init__(self, items):
        self.items = [(it, Reg()) for it in items]
        self.i = 0

    def next(self):
        it = self.items[self.i % len(self.items)]
        self.i += 1
        return it


def build(cfg, debug_outs=()):
    SEQ, CTX, DFF, DFFE = cfg["SEQ"], cfg["CTX"], cfg["DFF"], cfg["DFFE"]
    T = SEQ // 2
    NK = SEQ + CTX
    nc = bass.Bass("TRN2", target_bir_lowering=False)
    S = Sched()

    def din(name, shape, dt=F32):
        return nc.dram_tensor(name, list(shape), dt, kind="ExternalInput").ap()

    def dscr(name, shape, dt):
        kind = "ExternalOutput" if name in debug_outs else "Internal"
        return nc.dram_tensor(name, list(shape), dt, kind=kind).ap()

    tiles = {
        "own": dict(n=T, koff=0, rope=True, mc=0),
        "oth": dict(n=T, koff=T, rope=True, mc=0),
        "ctx": dict(n=CTX, koff=2 * T, rope=False, mc=1),
    }
    xin = {t: din("xT_" + t, [D, tiles[t]["n"]]) for t in tiles}
    cvec = din("cvec", [128, KC, 2])
    w_mod = din("w_mod", [2, D, 6 * D])
    b_mod = din("b_mod", [2, 128, 96])
    n1g = din("n1g", [2, 128, KC])
    n2g = din("n2g", [2, 128, KC])
    fng = din("fng", [128, KC])
    w_in = din("w_in", [2, D, INW])
    pool_w = din("pool_w", [2, 4, 256, 256])
    pool_sc = din("pool_sc", [2, 128, 8])
    sgu_ng = din("sgu_ng", [2, 1024])
    sgu_wT = din("sgu_wT", [2, 8, 128, 128])
    sgu_b = din("sgu_b", [2, 1024])
    lamv = din("lamv", [2, 4, 64])
    subln = din("subln", [2, 128, 1])
    w_pa = din("w_pa", [2, 1024, D])
    w_pb = din("w_pb", [2, 1024, D])
    w_pc = din("w_pc", [2, 1024, D])
    w_o = din("w_o", [2, D, D])
    ffn_w1 = din("ffn_w1", [1, D, DFF])
    ffn_w3 = din("ffn_w3", [1, D, DFF])
    ffn_w2 = din("ffn_w2", [1, DFF, D])
    router = din("router", [128, KC, NE])
    moe_w1 = din("moe_w1", [NE, D, DFFE])
    moe_w3 = din("moe_w3", [NE, D, DFFE])
    moe_w2 = din("moe_w2", [NE, DFFE, D])
    rope_t = {"own": din("rope_own", [2, 128, T]), "oth": din("rope_oth", [2, 128, T])}
    cmat = din("cmat", [3, 128, 128])
    hmask = din("hmask", [128, 4])
    pedge = {t: din("pedge_" + t, [128, 4, 16]) for t in tiles}
    outT = nc.dram_tensor("outT", [D, T], F32, kind="ExternalOutput").ap()

    h_s = {t: dscr("h_" + t, [D, tiles[t]["n"]], BF16) for t in tiles}
    xs = {t: dscr("xs_" + t, [D, tiles[t]["n"]], F32) for t in tiles}
    a_s = [{t: dscr("a%d_%s" % (l, t), [1024, tiles[t]["n"]], F32) for t in tiles} for l in range(2)]
    KT = [dscr("KT%d" % l, [NH, 128, NK], BF16) for l in range(2)]
    VV = [dscr("VV%d" % l, [NK, 1024], BF16) for l in range(2)]
    u_s = dscr("u_s", [1024, T], BF16)
    vn_s = dscr("vn_s", [T, 1024], BF16)
    q_s = dscr("q_s", [1024, T], BF16)
    g_s = dscr("g_s", [6144, T], BF16)
    ao_s = dscr("ao_s", [1024, T], BF16)
    bo_s = dscr("bo_s", [1024, T], BF16)
    co_s = dscr("co_s", [1024, T], BF16)
    h2_s = dscr("h2_s", [D, T], BF16)
    comb_s = dscr("comb_s", [NE, 128, T], F32)
    xcur = dict(xin)

    ctxs = []

    def enter(cm):
        ctxs.append(cm)
        return cm.__enter__()

    arena_t = enter(nc.sbuf_tensor("arena", [128, ARENA32], F32))
    PS = [enter(nc.psum_tensor("ps%d" % i, [128, 512], F32))[:] for i in range(8)]
    PSR = [Reg() for _ in range(8)]
    csem = {e: enter(nc.semaphore("c_" + e)) for e in Sched.ENG}
    dsem = {e: [enter(nc.semaphore("d_%s%d" % (e, i))) for i in range(NDS)] for e in ("sp", "pool")}

    class Arena:
        def __init__(self):
            self.off = 0
            self.base = 0

        def alloc(self, shape, dt):
            ne = int(np.prod(shape))
            n32 = ne if dt == F32 else (ne + 1) // 2
            n32 = (n32 + 7) // 8 * 8
            a = arena_t[:, self.off:self.off + n32]
            self.off += n32
            assert self.off <= ARENA32, ("arena overflow", self.off)
            if dt != F32:
                a = a.bitcast(dt)
            a = a[:, 0:ne]
            if len(shape) == 2:
                a = a.rearrange("p (a b) -> p a b", a=shape[0])
            elif len(shape) == 3:
                a = a.rearrange("p (a b c) -> p a b c", a=shape[0], b=shape[1])
            elif len(shape) == 4:
                a = a.rearrange("p (a b c d) -> p a b c d", a=shape[0], b=shape[1], c=shape[2])
            return a

        def reset(self):
            self.off = self.base

    A = Arena()

    def stage():
        S.barrier()
        A.reset()

    ones32 = A.alloc([128], F32)
    ident32 = A.alloc([128], F32)
    rmat32 = A.alloc([128], F32)
    onesbf = A.alloc([128], BF16)
    hmask_t = A.alloc([4], F32)
    modv = A.alloc([2, 2, 6, KC], F32)
    lam_t = A.alloc([2, 2], F32)
    subg_t = A.alloc([2], F32)
    fng_t = A.alloc([KC], F32)
    cR = Reg()
    S.add("sp", DMA(ones32, cmat[0]), writes=[cR], dma=True)
    S.add("sp", DMA(ident32, cmat[1]), writes=[cR], dma=True)
    S.add("sp", DMA(rmat32, cmat[2]), writes=[cR], dma=True)
    S.add("sp", DMA(hmask_t, hmask), writes=[cR], dma=True)
    S.add("sp", DMA(fng_t, fng), writes=[cR], dma=True)
    S.add("dve", CP(onesbf, ones32), reads=[cR], writes=[cR])
    A.base = A.off

    def wsrc(w2d, c0, c1):
        return w2d.rearrange("(kc p) n -> p kc n", p=128)[:, :, c0:c1]

    def stage_mod():
        stage()
        cv = A.alloc([KC, 2], F32)
        sc = A.alloc([KC, 2], BF16)
        r0 = Reg()
        S.add("sp", DMA(cv, cvec), writes=[r0], dma=True)
        S.add("act", ACT(sc, cv, AF.Silu), reads=[r0], writes=[r0])
        ws = Rot([A.alloc([KC, 512], BF16) for _ in range(2)])
        lam_init = [0.8 - 0.6 * float(np.exp(-0.3 * l)) for l in range(2)]
        for l in range(2):
            bm = A.alloc([96], F32)
            mraw = A.alloc([96, 2], F32)
            g1 = A.alloc([KC], F32)
            g2 = A.alloc([KC], F32)
            rb = Reg()
            S.add("sp", DMA(bm, b_mod[l]), writes=[rb], dma=True)
            S.add("sp", DMA(g1, n1g[l]), writes=[rb], dma=True)
            S.add("sp", DMA(g2, n2g[l]), writes=[rb], dma=True)
            for bi in range(24):
                w, wr = ws.next()
                S.add("pool", DMA(w, wsrc(w_mod[l], bi * 512, (bi + 1) * 512)), writes=[wr], dma=True)
                bank = bi % 2
                for c in range(4):
                    j = bi * 4 + c
                    for kc in range(KC):
                        S.add("pe", MM(PS[bank][:, 2 * c:2 * c + 2], w[:, kc, c * 128:(c + 1) * 128], sc[:, kc, :],
                                       kc == 0, kc == KC - 1), reads=[wr, r0], writes=[PSR[bank]])
                S.add("dve", CP(mraw[:, bi * 4:(bi + 1) * 4, :], PS[bank][:, 0:8].rearrange("p (a b) -> p a b", a=4)),
                      reads=[PSR[bank]], writes=[rb])
            for col in range(2):
                S.add("dve", TT(mraw[:, :, col], mraw[:, :, col], bm, ALU.add), reads=[rb], writes=[rb])
            for col in range(2):
                mv = modv[:, l, col]
                for half, g in ((0, g1), (1, g2)):
                    sh = mraw[:, (3 * half + 0) * KC:(3 * half + 1) * KC, col]
                    scl = mraw[:, (3 * half + 1) * KC:(3 * half + 2) * KC, col]
                    gt = mraw[:, (3 * half + 2) * KC:(3 * half + 3) * KC, col]
                    S.add("dve", STT(mv[:, 3 * half + 0, :], scl, 1.0, g, ALU.add, ALU.mult), reads=[rb], writes=[rb])
                    S.add("dve", CP(mv[:, 3 * half + 1, :], sh), reads=[rb], writes=[rb])
                    S.add("dve", CP(mv[:, 3 * half + 2, :], gt), reads=[rb], writes=[rb])
            lv = A.alloc([4, 64], F32)
            lp = A.alloc([2, 64], F32)
            ls = A.alloc([2], F32)
            sg = A.alloc([1], F32)
            S.add("sp", DMA(lv, lamv[l].partition_broadcast(128)), writes=[rb], dma=True)
            S.add("sp", DMA(sg, subln[l]), writes=[rb], dma=True)
            S.add("dve", TT(lp[:, 0, :], lv[:, 0, :], lv[:, 1, :], ALU.mult), reads=[rb], writes=[rb])
            S.add("dve", TT(lp[:, 1, :], lv[:, 2, :], lv[:, 3, :], ALU.mult), reads=[rb], writes=[rb])
            S.add("dve", lambda e, lp=lp, ls=ls: e.tensor_reduce(out=ls, in_=lp, axis=AX.X, op=ALU.add), reads=[rb], writes=[rb])
            S.add("act", ACT(ls, ls, AF.Exp), reads=[rb], writes=[rb])
            S.add("dve", STT(lam_t[:, l, 0:1], ls[:, 1:2], -lam_init[l], ls[:, 0:1], ALU.add, ALU.subtract), reads=[rb], writes=[rb])
            S.add("dve", TS(subg_t[:, l:l + 1], sg, 1.0 - lam_init[l], ALU.mult), reads=[rb], writes=[rb])

    def stage_norm(l, tile, which, dst, moe=False):
        ti = tiles[tile]
        n = ti["n"]
        mc = ti["mc"]
        src = xcur[tile]
        stage()
        blks = blocks(n)
        X = A.alloc([KC, n], F32)
        Xr = [Reg() for _ in range(KC)]
        for kc in range(KC):
            S.add("sp", DMA(X[:, kc, :], src[kc * 128:(kc + 1) * 128, :]), writes=[Xr[kc]], dma=True)
        sq = Rot([A.alloc([n], F32) for _ in range(2)])
        acc = A.alloc([n], F32)
        accr = Reg()
        rstd = A.alloc([n], F32)
        rstd_r = [Reg() for _ in blks]
        tmp = Rot([A.alloc([n], F32) for _ in range(2)])
        hf = Rot([A.alloc([n], F32) for _ in range(2)])
        hst = Rot([A.alloc([n], BF16) for _ in range(3)])
        Av = modv[:, l, mc, 3 * which + 0, :]
        Bv = modv[:, l, mc, 3 * which + 1, :]
        for kc in range(KC):
            if kc == 0:
                S.add("act", ACT(acc, X[:, kc, :], AF.Square), reads=[Xr[kc]], writes=[accr])
            else:
                s_, sr = sq.next()
                S.add("act", ACT(s_, X[:, kc, :], AF.Square), reads=[Xr[kc]], writes=[sr])
                S.add("dve", TT(acc, acc, s_, ALU.add), reads=[sr, accr], writes=[accr])
        for bi, (o, nb) in enumerate(blks):
            S.add("pe", MM(PS[bi][:, 0:nb], ones32, acc[:, o:o + nb]), reads=[accr, cR], writes=[PSR[bi]])
            s_, sr = sq.next()
            S.add("act", ACT(s_[:, 0:nb], PS[bi][:, 0:nb], AF.Sqrt, bias=EPS_AP, scale=1.0 / D), reads=[PSR[bi], cR], writes=[sr])
            S.add("dve", lambda e, a=rstd[:, o:o + nb], b=s_[:, 0:nb]: e.reciprocal(out=a, in_=b), reads=[sr], writes=[rstd_r[bi]])
        if moe:
            wr_t = A.alloc([KC, NE], F32)
            wrr = Reg()
            S.add("sp", DMA(wr_t, router), writes=[wrr], dma=True)
        ntt = n // 128
        for kc in range(KC):
            x, xreg = X[:, kc, :], Xr[kc]
            t, tr = tmp.next()
            for bi, (o, nb) in enumerate(blks):
                S.add("dve", STT(t[:, o:o + nb], x[:, o:o + nb], Av[:, kc:kc + 1], rstd[:, o:o + nb], ALU.mult, ALU.mult),
                      reads=[xreg, rstd_r[bi]], writes=[tr])
            hs, hr = hst.next()
            if not moe:
                S.add("act", ACT(hs, t, AF.Identity, bias=Bv[:, kc:kc + 1]), reads=[tr], writes=[hr])
            else:
                f, fr = hf.next()
                S.add("act", ACT(f, t, AF.Identity, bias=Bv[:, kc:kc + 1]), reads=[tr], writes=[fr])
                S.add("dve", CP(hs, f), reads=[fr], writes=[hr])
                for tt in range(ntt):
                    S.add("pe", MM(PS[tt][:, 0:NE], f[:, tt * 128:(tt + 1) * 128], wr_t[:, kc, :], kc == 0, kc == KC - 1),
                          reads=[fr, wrr], writes=[PSR[tt]])
            S.add("sp", DMA(dst[kc * 128:(kc + 1) * 128, :], hs), reads=[hr], dma=True)
        if moe:
            comb = A.alloc([ntt, NE], F32)
            cr = [Reg() for _ in range(ntt)]
            sm = A.alloc([ntt, 24], F32)
            for tt in range(ntt):
                lg = sm[:, tt, 0:8]
                m8 = sm[:, tt, 8:16]
                ex = sm[:, tt, 16:24]
                r = cr[tt]
                S.add("dve", CP(lg, PS[tt][:, 0:NE]), reads=[PSR[tt]], writes=[r])
                S.add("dve", lambda e, a=m8, b=lg: e.max(out=a, in_=b), reads=[r], writes=[r])
                S.add("dve", TS(comb[:, tt, 0:1], m8[:, 0:1], -1.0, ALU.mult), reads=[r], writes=[r])
                S.add("act", ACT(ex, lg, AF.Exp, bias=comb[:, tt, 0:1]), reads=[r], writes=[r])
                S.add("dve", TS(lg, lg, m8[:, 1:2], ALU.is_ge), reads=[r], writes=[r])
                S.add("dve", TT(ex, ex, lg, ALU.mult), reads=[r], writes=[r])
                S.add("dve", lambda e, a=m8[:, 2:3], b=ex: e.tensor_reduce(out=a, in_=b, axis=AX.X, op=ALU.add), reads=[r], writes=[r])
                S.add("dve", lambda e, a=m8[:, 3:4], b=m8[:, 2:3]: e.reciprocal(out=a, in_=b), reads=[r], writes=[r])
                S.add("dve", TS(comb[:, tt, :], ex, m8[:, 3:4], ALU.mult), reads=[r], writes=[r])
            cb = Rot([A.alloc([128], F32) for _ in range(3)])
            cst = Rot([A.alloc([n], F32) for _ in range(2)])
            for e_ in range(NE):
                for tt in range(ntt):
                    c, creg = cb.next()
                    S.add("dve", CP(c, comb[:, tt, e_:e_ + 1].to_broadcast([128, 128])), reads=[cr[tt]], writes=[creg])
                    bank = 4 + 2 * (e_ % 2) + tt // 4
                    S.add("pe", MM(PS[bank][:, (tt % 4) * 128:(tt % 4 + 1) * 128], c, ident32), reads=[creg, cR], writes=[PSR[bank]])
                st, sr = cst.next()
                for bi, (o, nb) in enumerate(blks):
                    bank = 4 + 2 * (e_ % 2) + bi
                    S.add("act", ACT(st[:, o:o + nb], PS[bank][:, 0:nb], AF.Copy), reads=[PSR[bank]], writes=[sr])
                S.add("sp", DMA(comb_s[e_, :, 0:n], st), reads=[sr], dma=True)

    EPS_AP = None

    def stage_win(l, tile, kinds):
        ti = tiles[tile]
        n = ti["n"]
        koff = ti["koff"]
        rope = ti["rope"]
        blks = blocks(n)
        ntt = n // 128
        stage()
        H = A.alloc([KC, n], BF16)
        Hr = Reg()
        S.add("sp", DMA(H, h_s[tile].rearrange("(kc p) t -> p kc t", p=128)), writes=[Hr], dma=True)
        ws = Rot([A.alloc([KC, 512], BF16) for _ in range(2)])
        st32 = Rot([A.alloc([n], F32) for _ in range(2)])
        st16 = Rot([A.alloc([n], BF16) for _ in range(3)])
        stv = Rot([A.alloc([512], BF16) for _ in range(3)])
        if rope and ("Q" in kinds or "K" in kinds):
            cs = A.alloc([2, n], F32)
            csr = Reg()
            S.add("sp", DMA(cs, rope_t[tile].rearrange("a p t -> p a t")), writes=[csr], dma=True)
            t1 = Rot([A.alloc([512], F32) for _ in range(2)])
            t2 = Rot([A.alloc([512], F32) for _ in range(2)])
        if "VS" in kinds:
            vg = A.alloc([ntt, 1024], F32)
            vgr = [Reg() for _ in range(ntt)]
            ngt = A.alloc([1024], F32)
            ngr = Reg()
            S.add("sp", DMA(ngt, sgu_ng[l].partition_broadcast(128)), writes=[ngr], dma=True)
            bst = A.alloc([ntt, 2, 6], F32)
            mvv = A.alloc([ntt, 4], F32)
            stn = Rot([A.alloc([1024], BF16) for _ in range(2)])
        plan = {"A": (0, 2), "U": (2, 2), "VS": (4, 2), "Q": (6, 2), "K": (8, 2), "V": (10, 2), "G": (12, 12)}
        bank_i = [0]

        def nextbank():
            b = bank_i[0] % 6
            bank_i[0] += 1
            return b

        for kind in kinds:
            b0, nbk = plan[kind]
            for bb in range(nbk):
                bi = b0 + bb
                w, wr = ws.next()
                S.add("pool", DMA(w, wsrc(w_in[l], bi * 512, (bi + 1) * 512)), writes=[wr], dma=True)
                if kind in ("VS", "V"):
                    for tt in range(ntt):
                        bank = nextbank()
                        for kc in range(KC):
                            S.add("pe", MM(PS[bank], H[:, kc, tt * 128:(tt + 1) * 128], w[:, kc, :], kc == 0, kc == KC - 1),
                                  reads=[Hr, wr], writes=[PSR[bank]])
                        if kind == "V":
                            s, sr = stv.next()
                            S.add("act", ACT(s, PS[bank], AF.Copy), reads=[PSR[bank]], writes=[sr])
                            S.add("sp", DMA(VV[l][koff + tt * 128:koff + (tt + 1) * 128, bb * 512:(bb + 1) * 512], s), reads=[sr], dma=True)
                        else:
                            S.add("act", ACT(vg[:, tt, bb * 512:(bb + 1) * 512], PS[bank], AF.Gelu), reads=[PSR[bank]], writes=[vgr[tt]])
                            S.add("dve", lambda e, a=bst[:, tt, bb, :], b=vg[:, tt, bb * 512:(bb + 1) * 512]: e.bn_stats(out=a, in_=b),
                                  reads=[vgr[tt]], writes=[vgr[tt]])
                            if bb == 1:
                                r = vgr[tt]
                                S.add("dve", lambda e, a=mvv[:, tt, 0:2], b=bst[:, tt].rearrange("p a b -> p (a b)"): e.bn_aggr(out=a, in_=b),
                                      reads=[r], writes=[r])
                                S.add("act", ACT(mvv[:, tt, 2:3], mvv[:, tt, 1:2], AF.Sqrt, bias=EPS_AP), reads=[r, cR], writes=[r])
                                S.add("dve", lambda e, a=mvv[:, tt, 3:4], b=mvv[:, tt, 2:3]: e.reciprocal(out=a, in_=b), reads=[r], writes=[r])
                                S.add("dve", TS(vg[:, tt, :], vg[:, tt, :], mvv[:, tt, 0:1], ALU.subtract, mvv[:, tt, 3:4], ALU.mult),
                                      reads=[r], writes=[r])
                                s, sr = stn.next()
                                S.add("dve", TT(s, vg[:, tt, :], ngt, ALU.mult), reads=[r, ngr], writes=[sr])
                                S.add("sp", DMA(vn_s[tt * 128:(tt + 1) * 128, :], s), reads=[sr], dma=True)
                    continue
                for c in range(4):
                    gc = bb * 4 + c
                    if kind == "A":
                        s, sr = st32.next()
                    else:
                        s, sr = st16.next()
                    for (o, nb) in blks:
                        bank = nextbank()
                        for kc in range(KC):
                            S.add("pe", MM(PS[bank][:, 0:nb], w[:, kc, c * 128:(c + 1) * 128], H[:, kc, o:o + nb], kc == 0, kc == KC - 1),
                                  reads=[Hr, wr], writes=[PSR[bank]])
                        if kind == "A":
                            S.add("act", ACT(s[:, o:o + nb], PS[bank][:, 0:nb], AF.Copy), reads=[PSR[bank]], writes=[sr])
                        elif kind == "U":
                            S.add("act", ACT(s[:, o:o + nb], PS[bank][:, 0:nb], AF.Gelu), reads=[PSR[bank]], writes=[sr])
                        elif kind == "G":
                            S.add("act", ACT(s[:, o:o + nb], PS[bank][:, 0:nb], AF.Sigmoid), reads=[PSR[bank]], writes=[sr])
                        elif not rope:
                            S.add("act", ACT(s[:, o:o + nb], PS[bank][:, 0:nb], AF.Copy), reads=[PSR[bank]], writes=[sr])
                        else:
                            a1, r1 = t1.next()
                            a2, r2 = t2.next()
                            S.add("act", ACT(a1[:, 0:nb], PS[bank][:, 0:nb], AF.Copy), reads=[PSR[bank]], writes=[r1])
                            S.add("pe", MM(PS[6][:, 0:nb], rmat32, a1[:, 0:nb]), reads=[r1, cR], writes=[PSR[6]])
                            S.add("dve", TT(a2[:, 0:nb], PS[6][:, 0:nb], cs[:, 1, o:o + nb], ALU.mult), reads=[PSR[6], csr], writes=[r2])
                            S.add("dve", TT(a1[:, 0:nb], a1[:, 0:nb], cs[:, 0, o:o + nb], ALU.mult), reads=[r1, csr], writes=[r1])
                            S.add("dve", TT(s[:, o:o + nb], a1[:, 0:nb], a2[:, 0:nb], ALU.add), reads=[r1, r2], writes=[sr])
                    if kind == "A":
                        dst = a_s[l][tile][gc * 128:(gc + 1) * 128, :]
                    elif kind == "U":
                        dst = u_s[gc * 128:(gc + 1) * 128, 0:n]
                    elif kind == "G":
                        dst = g_s[gc * 128:(gc + 1) * 128, 0:n]
                    elif kind == "Q":
                        dst = q_s[gc * 128:(gc + 1) * 128, 0:n]
                    else:
                        dst = KT[l][gc, :, koff:koff + n]
                    S.add("sp", DMA(dst, s), reads=[sr], dma=True)

    def stage_pool(l, tile):
        ti = tiles[tile]
        n = ti["n"]
        L = n + 16
        blks = blocks(n)
        stage()
        P = A.alloc([8, n], BF16)
        Pr = [Reg() for _ in range(8)]
        pw = A.alloc([4, 2, 256], BF16)
        pwr = Reg()
        for g in range(4):
            S.add("pool", DMA(pw[:, g], pool_w[l, g].rearrange("(cc p) d -> p cc d", p=128)), writes=[pwr], dma=True)
        psc = A.alloc([8], F32)
        S.add("sp", DMA(psc, pool_sc[l]), writes=[pwr], dma=True)
        pe_t = A.alloc([4, 16], F32)
        S.add("sp", DMA(pe_t, pedge[tile]), writes=[pwr], dma=True)
        ab = Rot([A.alloc([L], F32) for _ in range(2)])
        wa = Rot([A.alloc([L], F32) for _ in range(2)])
        wb = Rot([A.alloc([L], F32) for _ in range(2)])
        ed = Rot([A.alloc([16], F32) for _ in range(2)])
        nb_tile = {"own": "oth", "oth": "own"}.get(tile)
        mi = {"own": 0, "oth": 2}.get(tile, 0)
        for c in range(8):
            g = c // 2
            a, ar = ab.next()
            src = a_s[l][tile][c * 128:(c + 1) * 128, :]
            S.add("sp", DMA(a[:, 8:8 + n], src), writes=[ar], dma=True)
            if nb_tile is None:
                S.add("dve", lambda e, x=a[:, 0:8]: e.memset(x, 0.0), writes=[ar])
                S.add("dve", lambda e, x=a[:, 8 + n:L]: e.memset(x, 0.0), writes=[ar])
            else:
                nsrc = a_s[l][nb_tile][c * 128:(c + 1) * 128, :]
                S.add("sp", DMA(a[:, 0:8], nsrc[:, n - 8:n]), writes=[ar], dma=True)
                S.add("sp", DMA(a[:, 8 + n:L], nsrc[:, 0:8]), writes=[ar], dma=True)
                S.add("dve", TS(a[:, 0:8], a[:, 0:8], hmask_t[:, mi:mi + 1], ALU.mult), reads=[ar, cR], writes=[ar])
                S.add("dve", TS(a[:, 8 + n:L], a[:, 8 + n:L], hmask_t[:, mi + 1:mi + 2], ALU.mult), reads=[ar, cR], writes=[ar])
            w1, w1r = wa.next()
            w2, w2r = wb.next()
            S.add("dve", TT(w1[:, 1:L], a[:, 0:L - 1], a[:, 1:L], ALU.add), reads=[ar], writes=[w1r])
            cur, curr = w1, w1r
            if g >= 1:
                S.add("dve", TT(w2[:, 2:L - 1], w1[:, 1:L - 2], w1[:, 3:L], ALU.add), reads=[w1r], writes=[w2r])
                cur, curr = w2, w2r
            if g >= 2:
                S.add("dve", TT(w1[:, 4:L - 3], w2[:, 2:L - 5], w2[:, 6:L - 1], ALU.add), reads=[w2r], writes=[w1r])
                cur, curr = w1, w1r
            if g >= 3:
                S.add("dve", TT(w2[:, 8:L - 7], w1[:, 4:L - 11], w1[:, 12:L - 3], ALU.add), reads=[w1r], writes=[w2r])
                cur, curr = w2, w2r
            wsz = float(2 ** (g + 1))
            S.add("dve", STT(P[:, c, :], cur[:, 8:8 + n], 1.0 / wsz, a[:, 8:8 + n], ALU.mult, ALU.subtract), reads=[curr, ar], writes=[Pr[c]])
            e_, er = ed.next()
            for (lo, eo) in ((0, 0), (n - 8, 8)):
                S.add("dve", TT(e_[:, eo:eo + 8], cur[:, 8 + lo:16 + lo], pe_t[:, g, eo:eo + 8], ALU.mult), reads=[curr, pwr], writes=[er])
                S.add("dve", TT(P[:, c, lo:lo + 8], e_[:, eo:eo + 8], a[:, 8 + lo:16 + lo], ALU.subtract), reads=[er, ar, Pr[c]], writes=[Pr[c]])
        st = Rot([A.alloc([n], BF16) for _ in range(2)])
        bk = 0
        for g in range(4):
            for dd in range(2):
                s, sr = st.next()
                for (o, nb) in blks:
                    bank = bk % 4
                    bk += 1
                    for cc in range(2):
                        S.add("pe", MM(PS[bank][:, 0:nb], pw[:, g, cc, dd * 128:(dd + 1) * 128], P[:, 2 * g + cc, o:o + nb], cc == 0, cc == 1),
                              reads=[pwr, Pr[2 * g + cc]], writes=[PSR[bank]])
                    S.add("act", ACT(s[:, o:o + nb], PS[bank][:, 0:nb], AF.Copy, scale=psc[:, 2 * g + dd:2 * g + dd + 1]), reads=[PSR[bank], pwr], writes=[sr])
                S.add("sp", DMA(ao_s[(2 * g + dd) * 128:(2 * g + dd + 1) * 128, 0:n], s), reads=[sr], dma=True)

    def stage_sgu(l, tile):
        n = tiles[tile]["n"]
        ntt = n // 128
        stage()
        U = A.alloc([8, n], BF16)
        Ur = Reg()
        S.add("sp", DMA(U, u_s[:, 0:n].rearrange("(g p) t -> p g t", p=128)), writes=[Ur], dma=True)
        wt = A.alloc([8, 128], BF16)
        S.add("pool", DMA(wt, sgu_wT[l].rearrange("g q p -> q g p")), writes=[Ur], dma=True)
        bt = A.alloc([8, 128], F32)
        S.add("sp", DMA(bt, sgu_b[l].partition_broadcast(128).rearrange("p (g q) -> p g q", g=8)), writes=[Ur], dma=True)
        vn = Rot([A.alloc([1024], BF16) for _ in range(2)])
        tp = Rot([A.alloc([4, 128], F32) for _ in range(2)])
        BO = A.alloc([8, n], BF16)
        BOr = Reg()
        for tt in range(ntt):
            v, vr = vn.next()
            S.add("sp", DMA(v, vn_s[tt * 128:(tt + 1) * 128, :]), writes=[vr], dma=True)
            for gh in range(2):
                bank = (2 * tt + gh) % 4
                for gg in range(4):
                    g = gh * 4 + gg
                    S.add("pe", MM(PS[bank][:, gg * 128:(gg + 1) * 128], v[:, g * 128:(g + 1) * 128], wt[:, g, :]), reads=[vr, Ur], writes=[PSR[bank]])
                t, tr = tp.next()
                S.add("dve", TT(t, PS[bank].rearrange("p (a b) -> p a b", a=4), bt[:, gh * 4:(gh + 1) * 4, :], ALU.add), reads=[PSR[bank], Ur], writes=[tr])
                S.add("dve", TT(BO[:, gh * 4:(gh + 1) * 4, tt * 128:(tt + 1) * 128], t, U[:, gh * 4:(gh + 1) * 4, tt * 128:(tt + 1) * 128], ALU.mult),
                      reads=[tr, Ur], writes=[BOr])
        S.add("sp", DMA(bo_s[:, 0:n].rearrange("(g p) t -> p g t", p=128), BO), reads=[BOr], dma=True)

    def stage_att(l, tile):
        ti = tiles[tile]
        n = ti["n"]
        blks = blocks(n)
        if tile == "ctx":
            k0, nk = 2 * T, CTX
        else:
            k0, nk = 0, NK
        nkc = nk // 128
        stage()
        qb = Rot([A.alloc([n], BF16) for _ in range(2)])
        kb = Rot([A.alloc([nk], BF16) for _ in range(2)])
        vb = Rot([A.alloc([nkc, 128], BF16) for _ in range(2)])
        eb = [Rot([A.alloc([512], BF16) for _ in range(3)]) for _ in range(2)]
        rc = [A.alloc([512], F32) for _ in range(2)]
        o1 = A.alloc([512], F32)
        o2 = A.alloc([512], F32)
        osq = A.alloc([512], F32)
        rs = A.alloc([512], F32)
        fr = Reg()
        cst = Rot([A.alloc([n], BF16) for _ in range(2)])
        items = [(h, o, nb) for h in range(NH) for (o, nb) in blks]
        loaded = {}

        def load_head(h):
            q, qr = qb.next()
            k, kr = kb.next()
            v, vr = vb.next()
            S.add("sp", DMA(q, q_s[h * 128:(h + 1) * 128, 0:n]), writes=[qr], dma=True)
            S.add("sp", DMA(k, KT[l][h, :, k0:k0 + nk]), writes=[kr], dma=True)
            S.add("sp", DMA(v, VV[l][k0:k0 + nk, h * 128:(h + 1) * 128].rearrange("(c p) d -> p c d", p=128)), writes=[vr], dma=True)
            loaded[h] = (q, qr, k, kr, v, vr)

        def emit_S(h, o, nb, kc):
            q, qr, k, kr, v, vr = loaded[h]
            for i in range(2):
                bank = 2 * (kc % 2) + i
                S.add("pe", MM(PS[bank][:, 0:nb], k[i * 64:(i + 1) * 64, kc * 128:(kc + 1) * 128], q[i * 64:(i + 1) * 64, o:o + nb]),
                      reads=[kr, qr], writes=[PSR[bank]])

        load_head(0)
        cs_, csr = None, None
        for idx, (h, o, nb) in enumerate(items):
            if o == 0:
                if h + 1 < NH:
                    load_head(h + 1)
                cs_, csr = cst.next()
            q, qr, k, kr, v, vr = loaded[h]
            if idx == 0:
                emit_S(h, o, nb, 0)
            for kc in range(nkc):
                if kc + 1 < nkc:
                    emit_S(h, o, nb, kc + 1)
                ebs = []
                for i in range(2):
                    bank = 2 * (kc % 2) + i
                    e_, er = eb[i].next()
                    S.add("act", ACT(e_[:, 0:nb], PS[bank][:, 0:nb], AF.Exp, scale=0.125), reads=[PSR[bank]], writes=[er])
                    ebs.append((e_, er))
                for i in range(2):
                    e_, er = ebs[i]
                    S.add("pe", MM(PS[4 + i][:, 0:nb], v[:, kc, :], e_[:, 0:nb], kc == 0, kc == nkc - 1), reads=[vr, er], writes=[PSR[4 + i]])
                    S.add("pe", MM(PS[6 + i][:, 0:nb], onesbf, e_[:, 0:nb], kc == 0, kc == nkc - 1), reads=[cR, er], writes=[PSR[6 + i]])
            if idx + 1 < len(items):
                emit_S(items[idx + 1][0], items[idx + 1][1], items[idx + 1][2], 0)
            for i in range(2):
                S.add("dve", lambda e, a=rc[i][:, 0:nb], b=PS[6 + i][:, 0:nb]: e.reciprocal(out=a, in_=b), reads=[PSR[6 + i]], writes=[fr])
            S.add("dve", TT(o1[:, 0:nb], PS[4][:, 0:nb], rc[0][:, 0:nb], ALU.mult), reads=[PSR[4], fr], writes=[fr])
            S.add("dve", TT(o2[:, 0:nb], PS[5][:, 0:nb], rc[1][:, 0:nb], ALU.mult), reads=[PSR[5], fr], writes=[fr])
            S.add("dve", STT(o1[:, 0:nb], o2[:, 0:nb], lam_t[:, l, 0:1], o1[:, 0:nb], ALU.mult, ALU.add), reads=[fr, cR], writes=[fr])
            S.add("dve", TT(osq[:, 0:nb], o1[:, 0:nb], o1[:, 0:nb], ALU.mult), reads=[fr], writes=[fr])
            S.add("pe", MM(PS[6][:, 0:nb], ones32, osq[:, 0:nb]), reads=[fr, cR], writes=[PSR[6]])
            S.add("act", ACT(rs[:, 0:nb], PS[6][:, 0:nb], AF.Sqrt, bias=EPS_AP, scale=1.0 / 128), reads=[PSR[6], cR], writes=[fr])
            S.add("dve", lambda e, a=rs[:, 0:nb]: e.reciprocal(out=a, in_=a), reads=[fr], writes=[fr])
            S.add("dve", STT(cs_[:, o:o + nb], o1[:, 0:nb], subg_t[:, l:l + 1], rs[:, 0:nb], ALU.mult, ALU.mult), reads=[fr, cR], writes=[csr, fr])
            if o + nb >= n:
                S.add("sp", DMA(co_s[h * 128:(h + 1) * 128, 0:n], cs_), reads=[csr], dma=True)

    def stage_merge(l, tile):
        ti = tiles[tile]
        n = ti["n"]
        mc = ti["mc"]
        blks = blocks(n)
        stage()
        BR = []
        Rr = Reg()
        for src in (ao_s, bo_s, co_s):
            b = A.alloc([8, n], BF16)
            S.add("sp", DMA(b, src[:, 0:n].rearrange("(g p) t -> p g t", p=128)), writes=[Rr], dma=True)
            BR.append(b)
        M = A.alloc([KC, n], BF16)
        Mr = [Reg() for _ in range(KC)]
        wp = Rot([A.alloc([3, 8, 128], BF16) for _ in range(2)])
        gb = Rot([A.alloc([3, n], BF16) for _ in range(2)])
        tm = Rot([A.alloc([512], F32) for _ in range(2)])
        tm2 = Rot([A.alloc([512], F32) for _ in range(2)])
        bk = 0
        for j in range(KC):
            w, wr = wp.next()
            for xi, wsrc_ in enumerate((w_pa, w_pb, w_pc)):
                S.add("pool", DMA(w[:, xi], wsrc_[l].rearrange("(kc p) d -> p kc d", p=128)[:, :, j * 128:(j + 1) * 128]), writes=[wr], dma=True)
            g, gr = gb.next()
            S.add("sp", DMA(g, g_s[:, 0:n].rearrange("(x j p) t -> j p x t", x=3, p=128)[j]), writes=[gr], dma=True)
            for (o, nb) in blks:
                t, tr = tm.next()
                t2, t2r = tm2.next()
                for xi in range(3):
                    bank = bk % 6
                    bk += 1
                    for kc in range(8):
                        S.add("pe", MM(PS[bank][:, 0:nb], w[:, xi, kc, :], BR[xi][:, kc, o:o + nb], kc == 0, kc == 7), reads=[wr, Rr], writes=[PSR[bank]])
                    if xi == 0:
                        S.add("dve", TT(t[:, 0:nb], PS[bank][:, 0:nb], g[:, 0, o:o + nb], ALU.mult), reads=[PSR[bank], gr], writes=[tr])
                    elif xi == 1:
                        S.add("dve", TT(t2[:, 0:nb], PS[bank][:, 0:nb], g[:, 1, o:o + nb], ALU.mult), reads=[PSR[bank], gr], writes=[t2r])
                        S.add("dve", TT(t[:, 0:nb], t[:, 0:nb], t2[:, 0:nb], ALU.add), reads=[tr, t2r], writes=[tr])
                    else:
                        S.add("dve", TT(t2[:, 0:nb], PS[bank][:, 0:nb], g[:, 2, o:o + nb], ALU.mult), reads=[PSR[bank], gr, tr], writes=[t2r])
                        S.add("dve", TT(M[:, j, o:o + nb], t[:, 0:nb], t2[:, 0:nb], ALU.add), reads=[tr, t2r], writes=[Mr[j]])
        ws = Rot([A.alloc([KC, 512], BF16) for _ in range(2)])
        xr = Rot([A.alloc([n], F32) for _ in range(3)])
        gt = modv[:, l, mc, 2, :]
        src = xcur[tile]
        for bi in range(4):
            w, wr = ws.next()
            S.add("pool", DMA(w, wsrc(w_o[l], bi * 512, (bi + 1) * 512)), writes=[wr], dma=True)
            for c in range(4):
                jj = bi * 4 + c
                x, xreg = xr.next()
                S.add("sp", DMA(x, src[jj * 128:(jj + 1) * 128, :]), writes=[xreg], dma=True)
                for (o, nb) in blks:
                    bank = bk % 6
                    bk += 1
                    for kc in range(KC):
                        S.add("pe", MM(PS[bank][:, 0:nb], w[:, kc, c * 128:(c + 1) * 128], M[:, kc, o:o + nb], kc == 0, kc == KC - 1),
                              reads=[wr, Mr[kc]], writes=[PSR[bank]])
                    S.add("dve", STT(x[:, o:o + nb], PS[bank][:, 0:nb], gt[:, jj:jj + 1], x[:, o:o + nb], ALU.mult, ALU.add), reads=[PSR[bank], xreg, cR], writes=[xreg])
                S.add("sp", DMA(xs[tile][jj * 128:(jj + 1) * 128, :], x), reads=[xreg], dma=True)
        xcur[tile] = xs[tile]

    def stage_ffn(l, tile, moe, final, store_x=True):
        ti = tiles[tile]
        n = ti["n"]
        mc = ti["mc"]
        blks = blocks(n)
        stage()
        X = A.alloc([KC, n], F32)
        Xr = [[Reg() for _ in blks] for _ in range(KC)]
        for kc in range(KC):
            S.add("sp", DMA(X[:, kc, :], xcur[tile][kc * 128:(kc + 1) * 128, :]), writes=Xr[kc], dma=True)
        H2 = A.alloc([KC, n], BF16)
        Hr = Reg()
        S.add("sp", DMA(H2, h2_s[:, 0:n].rearrange("(kc p) t -> p kc t", p=128)), writes=[Hr], dma=True)
        FG = 2
        w1b = Rot([A.alloc([KC, FG * 128], BF16) for _ in range(2)])
        w3b = Rot([A.alloc([KC, FG * 128], BF16) for _ in range(2)])
        w2b = Rot([A.alloc([FG, D], BF16) for _ in range(3)])
        Gb = Rot([A.alloc([FG, n], BF16) for _ in range(2)])
        sb = Rot([A.alloc([512], F32) for _ in range(2)])
        cbuf = Rot([A.alloc([n], F32) for _ in range(2)]) if moe else None
        gt = modv[:, l, mc, 5, :]
        F = DFFE if moe else DFF
        nfc = F // 128
        assert nfc % FG == 0
        bk = [0]
        groups = [(e_, fg) for e_ in range(NE if moe else 1) for fg in range(nfc // FG)]
        wl = {}
        cbs = {}

        def load_w(gi):
            e_, fg = groups[gi]
            W1 = moe_w1[e_] if moe else ffn_w1[0]
            W3 = moe_w3[e_] if moe else ffn_w3[0]
            W2 = moe_w2[e_] if moe else ffn_w2[0]
            f0 = fg * FG * 128
            w1, w1r = w1b.next()
            w3, w3r = w3b.next()
            w2, w2r = w2b.next()
            S.add("pool", DMA(w1, wsrc(W1, f0, f0 + FG * 128)), writes=[w1r], dma=True)
            S.add("pool", DMA(w3, wsrc(W3, f0, f0 + FG * 128)), writes=[w3r], dma=True)
            S.add("pool", DMA(w2, W2[f0:f0 + FG * 128, :].rearrange("(fc p) d -> p fc d", p=128)), writes=[w2r], dma=True)
            wl[gi] = (w1, w1r, w3, w3r, w2, w2r)
            if moe and fg == 0:
                cb, cbr = cbuf.next()
                S.add("sp", DMA(cb, comb_s[e_, :, 0:n]), writes=[cbr], dma=True)
                cbs[e_] = (cb, cbr)

        Gs = {}

        def up(gi):
            e_, fg = groups[gi]
            w1, w1r, w3, w3r, w2, w2r = wl[gi]
            G, Gr = Gb.next()
            Gs[gi] = (G, Gr)
            for fc in range(FG):
                for (o, nb) in blks:
                    b1 = bk[0] % 4
                    b3 = (bk[0] + 1) % 4
                    bk[0] += 2
                    for kc in range(KC):
                        S.add("pe", MM(PS[b1][:, 0:nb], w1[:, kc, fc * 128:(fc + 1) * 128], H2[:, kc, o:o + nb], kc == 0, kc == KC - 1),
                              reads=[w1r, Hr], writes=[PSR[b1]])
                    for kc in range(KC):
                        S.add("pe", MM(PS[b3][:, 0:nb], w3[:, kc, fc * 128:(fc + 1) * 128], H2[:, kc, o:o + nb], kc == 0, kc == KC - 1),
                              reads=[w3r, Hr], writes=[PSR[b3]])
                    s_, sr = sb.next()
                    S.add("act", ACT(s_[:, 0:nb], PS[b1][:, 0:nb], AF.Silu), reads=[PSR[b1]], writes=[sr])
                    if moe:
                        cb, cbr = cbs[e_]
                        S.add("dve", TT(s_[:, 0:nb], s_[:, 0:nb], PS[b3][:, 0:nb], ALU.mult), reads=[sr, PSR[b3]], writes=[sr])
                        S.add("dve", TT(G[:, fc, o:o + nb], s_[:, 0:nb], cb[:, o:o + nb], ALU.mult), reads=[sr, cbr], writes=[Gr])
                    else:
                        S.add("dve", TT(G[:, fc, o:o + nb], s_[:, 0:nb], PS[b3][:, 0:nb], ALU.mult), reads=[sr, PSR[b3]], writes=[Gr])

        def down(gi):
            w1, w1r, w3, w3r, w2, w2r = wl[gi]
            G, Gr = Gs[gi]
            for jj in range(KC):
                for bi, (o, nb) in enumerate(blks):
                    bank = 4 + (jj * len(blks) + bi) % 4
                    for fc in range(FG):
                        S.add("pe", MM(PS[bank][:, 0:nb], w2[:, fc, jj * 128:(jj + 1) * 128], G[:, fc, o:o + nb], fc == 0, fc == FG - 1),
                              reads=[w2r, Gr], writes=[PSR[bank]])
                    S.add("dve", STT(X[:, jj, o:o + nb], PS[bank][:, 0:nb], gt[:, jj:jj + 1], X[:, jj, o:o + nb], ALU.mult, ALU.add),
                          reads=[PSR[bank], Xr[jj][bi], cR], writes=[Xr[jj][bi]])

        load_w(0)
        for gi in range(len(groups)):
            if gi + 1 < len(groups):
                load_w(gi + 1)
            up(gi)
            if gi > 0:
                down(gi - 1)
        down(len(groups) - 1)
        acc = A.alloc([n], F32)
        accr = [Reg() for _ in blks]
        if moe:
            rstd, creg0 = cbuf.items[0]
            extra = [creg0]
        else:
            rstd = A.alloc([n], F32)
            extra = []
        rr = [Reg() for _ in blks]
        for kc in range(KC):
            for bi, (o, nb) in enumerate(blks):
                if kc == 0:
                    S.add("act", ACT(acc[:, o:o + nb], X[:, kc, o:o + nb], AF.Square), reads=[Xr[kc][bi]], writes=[accr[bi]])
                else:
                    s_, sr = sb.next()
                    S.add("act", ACT(s_[:, 0:nb], X[:, kc, o:o + nb], AF.Square), reads=[Xr[kc][bi]], writes=[sr])
                    S.add("dve", TT(acc[:, o:o + nb], acc[:, o:o + nb], s_[:, 0:nb], ALU.add), reads=[sr, accr[bi]], writes=[accr[bi]])
        for bi, (o, nb) in enumerate(blks):
            S.add("pe", MM(PS[bi][:, 0:nb], ones32, acc[:, o:o + nb]), reads=[accr[bi], cR], writes=[PSR[bi]])
            s_, sr = sb.next()
            S.add("act", ACT(s_[:, 0:nb], PS[bi][:, 0:nb], AF.Sqrt, bias=EPS_AP, scale=1.0 / D), reads=[PSR[bi], cR], writes=[sr])
            S.add("dve", lambda e, a=rstd[:, o:o + nb], b=s_[:, 0:nb]: e.reciprocal(out=a, in_=b), reads=[sr], writes=[rr[bi]] + extra)
        if not final:
            if store_x:
                for kc in range(KC):
                    S.add("sp", DMA(xs[tile][kc * 128:(kc + 1) * 128, :], X[:, kc, :]), reads=Xr[kc], dma=True)
                xcur[tile] = xs[tile]
            Av = modv[:, l + 1, mc, 0, :]
            Bv = modv[:, l + 1, mc, 1, :]
            for kc in range(KC):
                for bi, (o, nb) in enumerate(blks):
                    s_, sr = sb.next()
                    S.add("dve", STT(s_[:, 0:nb], X[:, kc, o:o + nb], Av[:, kc:kc + 1], rstd[:, o:o + nb], ALU.mult, ALU.mult),
                          reads=[Xr[kc][bi], rr[bi], cR], writes=[sr])
                    S.add("act", ACT(H2[:, kc, o:o + nb], s_[:, 0:nb], AF.Identity, bias=Bv[:, kc:kc + 1]), reads=[sr, cR], writes=[Hr])
            S.add("sp", DMA(h_s[tile].rearrange("(kc p) t -> p kc t", p=128), H2), reads=[Hr], dma=True)
            return
        for kc in range(KC):
            for bi, (o, nb) in enumerate(blks):
                S.add("dve", STT(X[:, kc, o:o + nb], X[:, kc, o:o + nb], fng_t[:, kc:kc + 1], rstd[:, o:o + nb], ALU.mult, ALU.mult),
                      reads=[Xr[kc][bi], rr[bi], cR], writes=[Xr[kc][bi]])
            S.add("sp", DMA(outT[kc * 128:(kc + 1) * 128, :], X[:, kc, :]), reads=Xr[kc], dma=True)

    A.base = A.off
    eps_t = A.alloc([1], F32)
    S.add("dve", lambda e: e.memset(eps_t, EPS), writes=[cR])
    EPS_AP = eps_t
    A.base = A.off

    upto = cfg.get("upto", 99)
    stage_mod()
    step = [0]

    def go(fn, *a, **k):
        step[0] += 1
        if step[0] <= upto:
            fn(*a, **k)

    for t in ("oth", "ctx", "own"):
        go(stage_norm, 0, t, 0, h_s[t])
        go(stage_win, 0, t, ["K", "V", "A"])
    for t in ("oth", "ctx", "own"):
        go(stage_win, 0, t, ["U", "VS", "Q", "G"])
        go(stage_pool, 0, t)
        go(stage_sgu, 0, t)
        go(stage_att, 0, t)
        go(stage_merge, 0, t)
        go(stage_norm, 0, t, 1, h2_s[:, 0:tiles[t]["n"]])
        go(stage_ffn, 0, t, False, False, store_x=(t == "own"))
        go(stage_win, 1, t, ["K", "V", "A"] if t != "ctx" else ["K", "V"])
    t = "own"
    go(stage_win, 1, t, ["U", "VS", "Q", "G"])
    go(stage_pool, 1, t)
    go(stage_sgu, 1, t)
    go(stage_att, 1, t)
    go(stage_merge, 1, t)
    go(stage_norm, 1, t, 1, h2_s[:, 0:T], moe=True)
    go(stage_ffn, 1, t, True, True)
    S.barrier()

    S.finalize()
    with nc.Block() as block:
        @block.tensor
        def _(e):
            S.emit("pe", e, csem, dsem)

        @block.scalar
        def _(e):
            S.emit("act", e, csem, dsem)

        @block.vector
        def _(e):
            S.emit("dve", e, csem, dsem)

        @block.gpsimd
        def _(e):
            S.emit("pool", e, csem, dsem)

        @block.sync
        def _(e):
            S.emit("sp", e, csem, dsem)
    for cm in reversed(ctxs):
        cm.__exit__(None, None, None)
    return nc


def _fm(v):
    v = np.asarray(v)
    n = v.shape[-1] // 128
    return np.ascontiguousarray(np.swapaxes(v.reshape(v.shape[:-1] + (n, 128)), -1, -2))


def _consts(cfg, s):
    SEQ, CTX = cfg["SEQ"], cfg["CTX"]
    T = SEQ // 2
    half = 32
    inv = np.power(10000.0, -np.arange(0, half, 2, dtype=np.float32) / half).astype(np.float32)
    out = {}
    for name, t0 in (("rope_own", s * T), ("rope_oth", (1 - s) * T)):
        t = np.arange(t0, t0 + T)
        row = (t // 64).astype(np.float32)
        col = (t % 64).astype(np.float32)
        ar = row[:, None] * inv[None, :]
        ac = col[:, None] * inv[None, :]
        ang = np.concatenate([ar, ar, ac, ac], axis=-1).astype(np.float32)
        cs = np.stack([np.cos(ang), np.sin(ang)]).astype(np.float32)
        cs = np.transpose(cs, (0, 2, 1))
        out[name] = np.ascontiguousarray(np.concatenate([cs, cs], axis=1))
    cm = np.zeros((3, 128, 128), np.float32)
    cm[0] = 1.0
    cm[1] = np.eye(128, dtype=np.float32)
    for m in range(128):
        if m % 32 < 16:
            cm[2, m + 16, m] = -1.0
        else:
            cm[2, m - 16, m] = 1.0
    out["cmat"] = cm
    hm = np.zeros((128, 4), np.float32)
    hm[:, 0], hm[:, 1] = float(s == 1), float(s == 0)
    hm[:, 2], hm[:, 3] = float(s == 0), float(s == 1)
    out["hmask"] = hm

    def edges(t0, n, Sq):
        e = np.zeros((128, 4, 16), np.float32)
        for g, w in enumerate((2, 4, 8, 16)):
            for idx, t in enumerate(list(range(t0, t0 + 8)) + list(range(t0 + n - 8, t0 + n))):
                lo = min(max(t - w // 2, 0), Sq)
                hi = min(max(t + w - w // 2, 0), Sq)
                e[:, g, idx] = 1.0 / float(hi - lo)
        return e
    out["pedge_own"] = edges(s * T, T, SEQ)
    out["pedge_oth"] = edges((1 - s) * T, T, SEQ)
    out["pedge_ctx"] = edges(0, CTX, CTX)
    return out


def prep_inputs(inp, cfg):
    SEQ = cfg["SEQ"]
    T = SEQ // 2
    f32 = lambda a: np.ascontiguousarray(np.asarray(a, dtype=np.float32))
    shared = dict(
        w_mod=f32(inp["w_mod"]), b_mod=_fm(f32(inp["b_mod"])), n1g=_fm(f32(inp["norm1_g"])), n2g=_fm(f32(inp["norm2_g"])),
        fng=_fm(f32(inp["final_norm_g"])), w_in=f32(inp["w_in"]), pool_w=f32(inp["pool_w"]), pool_sc=_fm(f32(inp["pool_scale"])),
        sgu_ng=f32(inp["sgu_norm_g"]), sgu_wT=np.ascontiguousarray(np.swapaxes(f32(inp["sgu_w"]), -1, -2)),
        sgu_b=f32(inp["sgu_b"]).reshape(2, 1024),
        lamv=np.ascontiguousarray(np.stack([f32(inp["lambda_q1"]), f32(inp["lambda_k1"]), f32(inp["lambda_q2"]), f32(inp["lambda_k2"])], axis=1)),
        subln=f32(inp["attn_subln_g"]).reshape(2, 128, 1),
        w_pa=f32(inp["w_proj_a"]), w_pb=f32(inp["w_proj_b"]), w_pc=f32(inp["w_proj_c"]), w_o=f32(inp["w_o"]),
        ffn_w1=f32(inp["ffn_w1"]), ffn_w3=f32(inp["ffn_w3"]), ffn_w2=f32(inp["ffn_w2"]),
        router=np.ascontiguousarray(np.transpose(f32(inp["moe_router"])[0].reshape(KC, 128, NE), (1, 0, 2))),
        moe_w1=f32(inp["moe_w1"])[0], moe_w3=f32(inp["moe_w3"])[0], moe_w2=f32(inp["moe_w2"])[0],
    )
    x = f32(inp["x"])
    ctx = f32(inp["ctx"])
    c = f32(inp["c"])
    cc = f32(inp["c_ctx"])
    maps = []
    for core in range(8):
        b, s = core // 2, core % 2
        m = dict(shared)
        m["xT_own"] = np.ascontiguousarray(x[b, s * T:(s + 1) * T, :].T)
        m["xT_oth"] = np.ascontiguousarray(x[b, (1 - s) * T:(2 - s) * T, :].T)
        m["xT_ctx"] = np.ascontiguousarray(ctx[b].T)
        m["cvec"] = np.ascontiguousarray(np.stack([_fm(c[b]), _fm(cc)], axis=-1))
        m.update(_consts(cfg, s))
        maps.append(m)
    return maps


def assemble(results, cfg):
    SEQ = cfg["SEQ"]
    T = SEQ // 2
    out = np.zeros((4, SEQ, D), np.float32)
    for core in range(8):
        b, s = core // 2, core % 2
        out[b, s * T:(s + 1) * T, :] = np.asarray(results[core]["outT"]).T
    return out


def kernel(**inputs):
    cfg = FULL_CFG
    nc = build(cfg)
    maps = prep_inputs(inputs, cfg)
    res = run_bass_kernel_spmd(nc, maps, core_ids=list(range(8)))
    return assemble(res.results, cfg)
```

```python
import numpy as np
import ml_dtypes
import concourse.bass as bass
import concourse.mybir as mybir
from concourse.bass_utils import run_bass_kernel_spmd

F32 = mybir.dt.float32
BF16 = mybir.dt.bfloat16
AF = mybir.ActivationFunctionType
ALU = mybir.AluOpType
AX = mybir.AxisListType

D = 2048
KC = 16
NH = 8
OFF_A, OFF_B, OFF_Q, OFF_K, OFF_V, OFF_G, INW = 0, 1024, 3072, 4096, 5120, 6144, 12288
EPS = 1e-6
NE = 8
FULL_CFG = dict(SEQ=2048, CTX=256, DFF=5632, DFFE=7168)
SAME_ENGINE_SYNC = True
NDS = 6
ARENA32 = 46 * 1024


class Reg:
    __slots__ = ("w", "rs")

    def __init__(self):
        self.w = None
        self.rs = {}


class Op:
    __slots__ = ("eng", "fn", "deps", "dma", "idx", "inc", "cnt", "dslot", "dval")


class Sched:
    ENG = ("pe", "act", "dve", "pool", "sp")

    def __init__(self):
        self.ops = {e: [] for e in self.ENG}
        self.ndma = {e: 0 for e in self.ENG}
        self.lastdma = {}

    def add(self, eng, fn, reads=(), writes=(), dma=False):
        op = Op()
        op.eng, op.fn, op.dma, op.inc, op.cnt = eng, fn, dma, False, 0
        deps = []
        for r in reads:
            if r.w is not None:
                deps.append(r.w)
        for w in writes:
            if w.w is not None:
                deps.append(w.w)
            deps.extend(w.rs.values())
        op.deps = deps
        op.idx = len(self.ops[eng])
        self.ops[eng].append(op)
        if dma:
            n = self.ndma[eng]
            self.ndma[eng] += 1
            op.dslot = n % NDS
            op.dval = 16 * (n // NDS + 1)
            self.lastdma[(eng, op.dslot)] = op
            key = (eng, "d", op.idx)
        else:
            key = eng
        for r in reads:
            r.rs[key] = op
        for w in writes:
            w.w = op
            w.rs = {}
        return op

    def barrier(self):
        lasts = []
        for e in self.ENG:
            for o in reversed(self.ops[e]):
                if o.fn is not None and not o.dma:
                    lasts.append(o)
                    break
        lasts.extend(self.lastdma.values())
        for e in self.ENG:
            op = Op()
            op.eng, op.fn, op.dma, op.inc, op.cnt = e, None, False, False, 0
            op.deps = list(lasts)
            op.idx = len(self.ops[e])
            self.ops[e].append(op)

    def _skip(self, op, d):
        if d.dma or op.dma or op.fn is None:
            return False
        if d.eng != op.eng:
            return False
        return op.eng == "pe" or not SAME_ENGINE_SYNC

    def finalize(self):
        for e in self.ENG:
            for op in self.ops[e]:
                for d in op.deps:
                    if not d.dma and not self._skip(op, d):
                        d.inc = True
        for e in self.ENG:
            c = 0
            for op in self.ops[e]:
                if op.inc:
                    c += 1
                op.cnt = c

    def emit(self, e, eng, csem, dsem):
        waited = {}
        for op in self.ops[e]:
            need = {}
            for d in op.deps:
                if d.dma:
                    key, val = ("d", d.eng, d.dslot), d.dval
                else:
                    if self._skip(op, d):
                        continue
                    key, val = d.eng, d.cnt
                if waited.get(key, 0) < val and need.get(key, 0) < val:
                    need[key] = val
            if op.dma and op.dval > 16:
                key = ("d", e, op.dslot)
                val = op.dval - 16
                if waited.get(key, 0) < val and need.get(key, 0) < val:
                    need[key] = val
            for key, val in need.items():
                sem = dsem[key[1]][key[2]] if isinstance(key, tuple) else csem[key]
                eng.wait_ge(sem, val)
                waited[key] = val
            if op.fn is None:
                continue
            ins = op.fn(eng)
            if op.dma:
                ins.then_inc(dsem[e][op.dslot], 16)
            elif op.inc:
                ins.then_inc(csem[e], 1)
        for (qe, slot), o in self.lastdma.items():
            if qe == e:
                eng.wait_ge(dsem[e][slot], o.dval)


def MM(out, lhsT, rhs, start=True, stop=True):
    return lambda e: e.matmul(out, lhsT=lhsT, rhs=rhs, start=start, stop=stop)


def ACT(out, in_, func, bias=None, scale=None):
    kw = {}
    if bias is not None:
        kw["bias"] = bias
    if scale is not None:
        kw["scale"] = scale
    return lambda e: e.activation(out=out, in_=in_, func=func, **kw)


def TT(out, a, b, op):
    return lambda e: e.tensor_tensor(out=out, in0=a, in1=b, op=op)


def TS(out, a, s1, op0, s2=None, op1=None):
    if op1 is None:
        return lambda e: e.tensor_scalar(out=out, in0=a, scalar1=s1, scalar2=None, op0=op0)
    return lambda e: e.tensor_scalar(out=out, in0=a, scalar1=s1, scalar2=s2, op0=op0, op1=op1)


def STT(out, in0, scalar, in1, op0, op1):
    return lambda e: e.scalar_tensor_tensor(out=out, in0=in0, scalar=scalar, in1=in1, op0=op0, op1=op1)


def CP(out, in_):
    return lambda e: e.tensor_copy(out=out, in_=in_)


def DMA(out, in_):
    return lambda e: e.dma_start(out=out, in_=in_)


def blocks(n):
    return [(o, min(512, n - o)) for o in range(0, n, 512)]


class Rot:
    def __init__(self, items):
        self.items = [(it, Reg()) for it in items]
        self.i = 0

    def next(self):
        it = self.items[self.i % len(self.items)]
        self.i += 1
        return it


def build(cfg, debug_outs=()):
    SEQ, CTX, DFF, DFFE = cfg["SEQ"], cfg["CTX"], cfg["DFF"], cfg["DFFE"]
    T = SEQ // 2
    NK = SEQ + CTX
    nc = bass.Bass("TRN2", target_bir_lowering=False)
    S = Sched()

    def din(name, shape, dt=F32):
        return nc.dram_tensor(name, list(shape), dt, kind="ExternalInput").ap()

    def dscr(name, shape, dt):
        kind = "ExternalOutput" if name in debug_outs else "Internal"
        return nc.dram_tensor(name, list(shape), dt, kind=kind).ap()

    tiles = {
        "own": dict(n=T, koff=0, rope=True, mc=0),
        "oth": dict(n=T, koff=T, rope=True, mc=0),
        "ctx": dict(n=CTX, koff=2 * T, rope=False, mc=1),
    }
    xin = {t: din("xT_" + t, [D, tiles[t]["n"]]) for t in tiles}
    cvec = din("cvec", [128, KC, 2])
    w_mod = din("w_mod", [2, D, 6 * D])
    b_mod = din("b_mod", [2, 128, 96])
    n1g = din("n1g", [2, 128, KC])
    n2g = din("n2g", [2, 128, KC])
    fng = din("fng", [128, KC])
    w_in = din("w_in", [2, D, INW])
    pool_w = din("pool_w", [2, 4, 256, 256])
    pool_sc = din("pool_sc", [2, 128, 8])
    sgu_ng = din("sgu_ng", [2, 1024])
    sgu_wT = din("sgu_wT", [2, 8, 128, 128])
    sgu_b = din("sgu_b", [2, 1024])
    lamv = din("lamv", [2, 4, 64])
    subln = din("subln", [2, 128, 1])
    w_pa = din("w_pa", [2, 1024, D])
    w_pb = din("w_pb", [2, 1024, D])
    w_pc = din("w_pc", [2, 1024, D])
    w_o = din("w_o", [2, D, D])
    ffn_w1 = din("ffn_w1", [1, D, DFF])
    ffn_w3 = din("ffn_w3", [1, D, DFF])
    ffn_w2 = din("ffn_w2", [1, DFF, D])
    router = din("router", [128, KC, NE])
    moe_w1 = din("moe_w1", [NE, D, DFFE])
    moe_w3 = din("moe_w3", [NE, D, DFFE])
    moe_w2 = din("moe_w2", [NE, DFFE, D])
    rope_t = {"own": din("rope_own", [2, 128, T]), "oth": din("rope_oth", [2, 128, T])}
    cmat = din("cmat", [3, 128, 128])
    hmask = din("hmask", [128, 4])
    pedge = {t: din("pedge_" + t, [128, 4, 16]) for t in tiles}
    outT = nc.dram_tensor("outT", [D, T], F32, kind="ExternalOutput").ap()

    h_s = {t: dscr("h_" + t, [D, tiles[t]["n"]], BF16) for t in tiles}
    xs = {t: dscr("xs_" + t, [D, tiles[t]["n"]], F32) for t in tiles}
    a_s = [{t: dscr("a%d_%s" % (l, t), [1024, tiles[t]["n"]], F32) for t in tiles} for l in range(2)]
    KT = [dscr("KT%d" % l, [NH, 128, NK], BF16) for l in range(2)]
    VV = [dscr("VV%d" % l, [NK, 1024], BF16) for l in range(2)]
    u_s = dscr("u_s", [1024, T], BF16)
    vn_s = dscr("vn_s", [T, 1024], BF16)
    q_s = dscr("q_s", [1024, T], BF16)
    g_s = dscr("g_s", [6144, T], BF16)
    ao_s = dscr("ao_s", [1024, T], BF16)
    bo_s = dscr("bo_s", [1024, T], BF16)
    co_s = dscr("co_s", [1024, T], BF16)
    h2_s = dscr("h2_s", [D, T], BF16)
    comb_s = dscr("comb_s", [NE, 128, T], F32)
    xcur = dict(xin)

    ctxs = []

    def enter(cm):
        ctxs.append(cm)
        return cm.__enter__()

    arena_t = enter(nc.sbuf_tensor("arena", [128, ARENA32], F32))
    PS = [enter(nc.psum_tensor("ps%d" % i, [128, 512], F32))[:] for i in range(8)]
    PSR = [Reg() for _ in range(8)]
    csem = {e: enter(nc.semaphore("c_" + e)) for e in Sched.ENG}
    dsem = {e: [enter(nc.semaphore("d_%s%d" % (e, i))) for i in range(NDS)] for e in ("sp", "pool")}

    class Arena:
        def __init__(self):
            self.off = 0
            self.base = 0

        def alloc(self, shape, dt):
            ne = int(np.prod(shape))
            n32 = ne if dt == F32 else (ne + 1) // 2
            n32 = (n32 + 7) // 8 * 8
            a = arena_t[:, self.off:self.off + n32]
            self.off += n32
            assert self.off <= ARENA32, ("arena overflow", self.off)
            if dt != F32:
                a = a.bitcast(dt)
            a = a[:, 0:ne]
            if len(shape) == 2:
                a = a.rearrange("p (a b) -> p a b", a=shape[0])
            elif len(shape) == 3:
                a = a.rearrange("p (a b c) -> p a b c", a=shape[0], b=shape[1])
            elif len(shape) == 4:
                a = a.rearrange("p (a b c d) -> p a b c d", a=shape[0], b=shape[1], c=shape[2])
            return a

        def reset(self):
            self.off = self.base

    A = Arena()

    def stage():
        S.barrier()
        A.reset()

    ones32 = A.alloc([128], F32)
    ident32 = A.alloc([128], F32)
    rmat32 = A.alloc([128], F32)
    onesbf = A.alloc([128], BF16)
    hmask_t = A.alloc([4], F32)
    modv = A.alloc([2, 2, 6, KC], F32)
    lam_t = A.alloc([2, 2], F32)
    subg_t = A.alloc([2], F32)
    fng_t = A.alloc([KC], F32)
    cR = Reg()
    S.add("sp", DMA(ones32, cmat[0]), writes=[cR], dma=True)
    S.add("sp", DMA(ident32, cmat[1]), writes=[cR], dma=True)
    S.add("sp", DMA(rmat32, cmat[2]), writes=[cR], dma=True)
    S.add("sp", DMA(hmask_t, hmask), writes=[cR], dma=True)
    S.add("sp", DMA(fng_t, fng), writes=[cR], dma=True)
    S.add("dve", CP(onesbf, ones32), reads=[cR], writes=[cR])
    A.base = A.off

    def wsrc(w2d, c0, c1):
        return w2d.rearrange("(kc p) n -> p kc n", p=128)[:, :, c0:c1]

    def stage_mod():
        stage()
        cv = A.alloc([KC, 2], F32)
        sc = A.alloc([KC, 2], BF16)
        r0 = Reg()
        S.add("sp", DMA(cv, cvec), writes=[r0], dma=True)
        S.add("act", ACT(sc, cv, AF.Silu), reads=[r0], writes=[r0])
        ws = Rot([A.alloc([KC, 512], BF16) for _ in range(2)])
        lam_init = [0.8 - 0.6 * float(np.exp(-0.3 * l)) for l in range(2)]
        for l in range(2):
            bm = A.alloc([96], F32)
            mraw = A.alloc([96, 2], F32)
            g1 = A.alloc([KC], F32)
            g2 = A.alloc([KC], F32)
            rb = Reg()
            S.add("sp", DMA(bm, b_mod[l]), writes=[rb], dma=True)
            S.add("sp", DMA(g1, n1g[l]), writes=[rb], dma=True)
            S.add("sp", DMA(g2, n2g[l]), writes=[rb], dma=True)
            for bi in range(24):
                w, wr = ws.next()
                S.add("pool", DMA(w, wsrc(w_mod[l], bi * 512, (bi + 1) * 512)), writes=[wr], dma=True)
                bank = bi % 2
                for c in range(4):
                    j = bi * 4 + c
                    for kc in range(KC):
                        S.add("pe", MM(PS[bank][:, 2 * c:2 * c + 2], w[:, kc, c * 128:(c + 1) * 128], sc[:, kc, :],
                                       kc == 0, kc == KC - 1), reads=[wr, r0], writes=[PSR[bank]])
                S.add("dve", CP(mraw[:, bi * 4:(bi + 1) * 4, :], PS[bank][:, 0:8].rearrange("p (a b) -> p a b", a=4)),
                      reads=[PSR[bank]], writes=[rb])
            for col in range(2):
                S.add("dve", TT(mraw[:, :, col], mraw[:, :, col], bm, ALU.add), reads=[rb], writes=[rb])
            for col in range(2):
                mv = modv[:, l, col]
                for half, g in ((0, g1), (1, g2)):
                    sh = mraw[:, (3 * half + 0) * KC:(3 * half + 1) * KC, col]
                    scl = mraw[:, (3 * half + 1) * KC:(3 * half + 2) * KC, col]
                    gt = mraw[:, (3 * half + 2) * KC:(3 * half + 3) * KC, col]
                    S.add("dve", STT(mv[:, 3 * half + 0, :], scl, 1.0, g, ALU.add, ALU.mult), reads=[rb], writes=[rb])
                    S.add("dve", CP(mv[:, 3 * half + 1, :], sh), reads=[rb], writes=[rb])
                    S.add("dve", CP(mv[:, 3 * half + 2, :], gt), reads=[rb], writes=[rb])
            lv = A.alloc([4, 64], F32)
            lp = A.alloc([2, 64], F32)
            ls = A.alloc([2], F32)
            sg = A.alloc([1], F32)
            S.add("sp", DMA(lv, lamv[l].partition_broadcast(128)), writes=[rb], dma=True)
            S.add("sp", DMA(sg, subln[l]), writes=[rb], dma=True)
            S.add("dve", TT(lp[:, 0, :], lv[:, 0, :], lv[:, 1, :], ALU.mult), reads=[rb], writes=[rb])
            S.add("dve", TT(lp[:, 1, :], lv[:, 2, :], lv[:, 3, :], ALU.mult), reads=[rb], writes=[rb])
            S.add("dve", lambda e, lp=lp, ls=ls: e.tensor_reduce(out=ls, in_=lp, axis=AX.X, op=ALU.add), reads=[rb], writes=[rb])
            S.add("act", ACT(ls, ls, AF.Exp), reads=[rb], writes=[rb])
            S.add("dve", STT(lam_t[:, l, 0:1], ls[:, 1:2], -lam_init[l], ls[:, 0:1], ALU.add, ALU.subtract), reads=[rb], writes=[rb])
            S.add("dve", TS(subg_t[:, l:l + 1], sg, 1.0 - lam_init[l], ALU.mult), reads=[rb], writes=[rb])

    def stage_norm(l, tile, which, dst, moe=False):
        ti = tiles[tile]
        n = ti["n"]
        mc = ti["mc"]
        src = xcur[tile]
        stage()
        blks = blocks(n)
        X = A.alloc([KC, n], F32)
        Xr = [Reg() for _ in range(KC)]
        for kc in range(KC):
            S.add("sp", DMA(X[:, kc, :], src[kc * 128:(kc + 1) * 128, :]), writes=[Xr[kc]], dma=True)
        sq = Rot([A.alloc([n], F32) for _ in range(2)])
        acc = A.alloc([n], F32)
        accr = Reg()
        rstd = A.alloc([n], F32)
        rstd_r = [Reg() for _ in blks]
        tmp = Rot([A.alloc([n], F32) for _ in range(2)])
        hf = Rot([A.alloc([n], F32) for _ in range(2)])
        hst = Rot([A.alloc([n], BF16) for _ in range(3)])
        Av = modv[:, l, mc, 3 * which + 0, :]
        Bv = modv[:, l, mc, 3 * which + 1, :]
        for kc in range(KC):
            if kc == 0:
                S.add("act", ACT(acc, X[:, kc, :], AF.Square), reads=[Xr[kc]], writes=[accr])
            else:
                s_, sr = sq.next()
                S.add("act", ACT(s_, X[:, kc, :], AF.Square), reads=[Xr[kc]], writes=[sr])
                S.add("dve", TT(acc, acc, s_, ALU.add), reads=[sr, accr], writes=[accr])
        for bi, (o, nb) in enumerate(blks):
            S.add("pe", MM(PS[bi][:, 0:nb], ones32, acc[:, o:o + nb]), reads=[accr, cR], writes=[PSR[bi]])
            s_, sr = sq.next()
            S.add("act", ACT(s_[:, 0:nb], PS[bi][:, 0:nb], AF.Sqrt, bias=EPS_AP, scale=1.0 / D), reads=[PSR[bi], cR], writes=[sr])
            S.add("dve", lambda e, a=rstd[:, o:o + nb], b=s_[:, 0:nb]: e.reciprocal(out=a, in_=b), reads=[sr], writes=[rstd_r[bi]])
        if moe:
            wr_t = A.alloc([KC, NE], F32)
            wrr = Reg()
            S.add("sp", DMA(wr_t, router), writes=[wrr], dma=True)
        ntt = n // 128
        for kc in range(KC):
            x, xreg = X[:, kc, :], Xr[kc]
            t, tr = tmp.next()
            for bi, (o, nb) in enumerate(blks):
                S.add("dve", STT(t[:, o:o + nb], x[:, o:o + nb], Av[:, kc:kc + 1], rstd[:, o:o + nb], ALU.mult, ALU.mult),
                      reads=[xreg, rstd_r[bi]], writes=[tr])
            hs, hr = hst.next()
            if not moe:
                S.add("act", ACT(hs, t, AF.Identity, bias=Bv[:, kc:kc + 1]), reads=[tr], writes=[hr])
            else:
                f, fr = hf.next()
                S.add("act", ACT(f, t, AF.Identity, bias=Bv[:, kc:kc + 1]), reads=[tr], writes=[fr])
                S.add("dve", CP(hs, f), reads=[fr], writes=[hr])
                for tt in range(ntt):
                    S.add("pe", MM(PS[tt][:, 0:NE], f[:, tt * 128:(tt + 1) * 128], wr_t[:, kc, :], kc == 0, kc == KC - 1),
                          reads=[fr, wrr], writes=[PSR[tt]])
            S.add("sp", DMA(dst[kc * 128:(kc + 1) * 128, :], hs), reads=[hr], dma=True)
        if moe:
            comb = A.alloc([ntt, NE], F32)
            cr = [Reg() for _ in range(ntt)]
            sm = A.alloc([ntt, 24], F32)
            for tt in range(ntt):
                lg = sm[:, tt, 0:8]
                m8 = sm[:, tt, 8:16]
                ex = sm[:, tt, 16:24]
                r = cr[tt]
                S.add("dve", CP(lg, PS[tt][:, 0:NE]), reads=[PSR[tt]], writes=[r])
                S.add("dve", lambda e, a=m8, b=lg: e.max(out=a, in_=b), reads=[r], writes=[r])
                S.add("dve", TS(comb[:, tt, 0:1], m8[:, 0:1], -1.0, ALU.mult), reads=[r], writes=[r])
                S.add("act", ACT(ex, lg, AF.Exp, bias=comb[:, tt, 0:1]), reads=[r], writes=[r])
                S.add("dve", TS(lg, lg, m8[:, 1:2], ALU.is_ge), reads=[r], writes=[r])
                S.add("dve", TT(ex, ex, lg, ALU.mult), reads=[r], writes=[r])
                S.add("dve", lambda e, a=m8[:, 2:3], b=ex: e.tensor_reduce(out=a, in_=b, axis=AX.X, op=ALU.add), reads=[r], writes=[r])
                S.add("dve", lambda e, a=m8[:, 3:4], b=m8[:, 2:3]: e.reciprocal(out=a, in_=b), reads=[r], writes=[r])
                S.add("dve", TS(comb[:, tt, :], ex, m8[:, 3:4], ALU.mult), reads=[r], writes=[r])
            cb = Rot([A.alloc([128], F32) for _ in range(3)])
            cst = Rot([A.alloc([n], F32) for _ in range(2)])
            for e_ in range(NE):
                for tt in range(ntt):
                    c, creg = cb.next()
                    S.add("dve", CP(c, comb[:, tt, e_:e_ + 1].to_broadcast([128, 128])), reads=[cr[tt]], writes=[creg])
                    bank = 4 + 2 * (e_ % 2) + tt // 4
                    S.add("pe", MM(PS[bank][:, (tt % 4) * 128:(tt % 4 + 1) * 128], c, ident32), reads=[creg, cR], writes=[PSR[bank]])
                st, sr = cst.next()
                for bi, (o, nb) in enumerate(blks):
                    bank = 4 + 2 * (e_ % 2) + bi
                    S.add("act", ACT(st[:, o:o + nb], PS[bank][:, 0:nb], AF.Copy), reads=[PSR[bank]], writes=[sr])
                S.add("sp", DMA(comb_s[e_, :, 0:n], st), reads=[sr], dma=True)

    EPS_AP = None

    def stage_win(l, tile, kinds):
        ti = tiles[tile]
        n = ti["n"]
        koff = ti["koff"]
        rope = ti["rope"]
        blks = blocks(n)
        ntt = n // 128
        stage()
        H = A.alloc([KC, n], BF16)
        Hr = Reg()
        S.add("sp", DMA(H, h_s[tile].rearrange("(kc p) t -> p kc t", p=128)), writes=[Hr], dma=True)
        ws = Rot([A.alloc([KC, 512], BF16) for _ in range(2)])
        st32 = Rot([A.alloc([n], F32) for _ in range(2)])
        st16 = Rot([A.alloc([n], BF16) for _ in range(3)])
        stv = Rot([A.alloc([512], BF16) for _ in range(3)])
        if rope and ("Q" in kinds or "K" in kinds):
            cs = A.alloc([2, n], F32)
            csr = Reg()
            S.add("sp", DMA(cs, rope_t[tile].rearrange("a p t -> p a t")), writes=[csr], dma=True)
            t1 = Rot([A.alloc([512], F32) for _ in range(2)])
            t2 = Rot([A.alloc([512], F32) for _ in range(2)])
        if "VS" in kinds:
            vg = A.alloc([ntt, 1024], F32)
            vgr = [Reg() for _ in range(ntt)]
            ngt = A.alloc([1024], F32)
            ngr = Reg()
            S.add("sp", DMA(ngt, sgu_ng[l].partition_broadcast(128)), writes=[ngr], dma=True)
            bst = A.alloc([ntt, 2, 6], F32)
            mvv = A.alloc([ntt, 4], F32)
            stn = Rot([A.alloc([1024], BF16) for _ in range(2)])
        plan = {"A": (0, 2), "U": (2, 2), "VS": (4, 2), "Q": (6, 2), "K": (8, 2), "V": (10, 2), "G": (12, 12)}
        bank_i = [0]

        def nextbank():
            b = bank_i[0] % 6
            bank_i[0] += 1
            return b

        for kind in kinds:
            b0, nbk = plan[kind]
            for bb in range(nbk):
                bi = b0 + bb
                w, wr = ws.next()
                S.add("pool", DMA(w, wsrc(w_in[l], bi * 512, (bi + 1) * 512)), writes=[wr], dma=True)
                if kind in ("VS", "V"):
                    for tt in range(ntt):
                        bank = nextbank()
                        for kc in range(KC):
                            S.add("pe", MM(PS[bank], H[:, kc, tt * 128:(tt + 1) * 128], w[:, kc, :], kc == 0, kc == KC - 1),
                                  reads=[Hr, wr], writes=[PSR[bank]])
                        if kind == "V":
                            s, sr = stv.next()
                            S.add("act", ACT(s, PS[bank], AF.Copy), reads=[PSR[bank]], writes=[sr])
                            S.add("sp", DMA(VV[l][koff + tt * 128:koff + (tt + 1) * 128, bb * 512:(bb + 1) * 512], s), reads=[sr], dma=True)
                        else:
                            S.add("act", ACT(vg[:, tt, bb * 512:(bb + 1) * 512], PS[bank], AF.Gelu), reads=[PSR[bank]], writes=[vgr[tt]])
                            S.add("dve", lambda e, a=bst[:, tt, bb, :], b=vg[:, tt, bb * 512:(bb + 1) * 512]: e.bn_stats(out=a, in_=b),
                                  reads=[vgr[tt]], writes=[vgr[tt]])
                            if bb == 1:
                                r = vgr[tt]
                                S.add("dve", lambda e, a=mvv[:, tt, 0:2], b=bst[:, tt].rearrange("p a b -> p (a b)"): e.bn_aggr(out=a, in_=b),
                                      reads=[r], writes=[r])
                                S.add("act", ACT(mvv[:, tt, 2:3], mvv[:, tt, 1:2], AF.Sqrt, bias=EPS_AP), reads=[r, cR], writes=[r])
                                S.add("dve", lambda e, a=mvv[:, tt, 3:4], b=mvv[:, tt, 2:3]: e.reciprocal(out=a, in_=b), reads=[r], writes=[r])
                                S.add("dve", TS(vg[:, tt, :], vg[:, tt, :], mvv[:, tt, 0:1], ALU.subtract, mvv[:, tt, 3:4], ALU.mult),
                                      reads=[r], writes=[r])
                                s, sr = stn.next()
                                S.add("dve", TT(s, vg[:, tt, :], ngt, ALU.mult), reads=[r, ngr], writes=[sr])
                                S.add("sp", DMA(vn_s[tt * 128:(tt + 1) * 128, :], s), reads=[sr], dma=True)
                    continue
                for c in range(4):
                    gc = bb * 4 + c
                    if kind == "A":
                        s, sr = st32.next()
                    else:
                        s, sr = st16.next()
                    for (o, nb) in blks:
                        bank = nextbank()
                        for kc in range(KC):
                            S.add("pe", MM(PS[bank][:, 0:nb], w[:, kc, c * 128:(c + 1) * 128], H[:, kc, o:o + nb], kc == 0, kc == KC - 1),
                                  reads=[Hr, wr], writes=[PSR[bank]])
                        if kind == "A":
                            S.add("act", ACT(s[:, o:o + nb], PS[bank][:, 0:nb], AF.Copy), reads=[PSR[bank]], writes=[sr])
                        elif kind == "U":
                            S.add("act", ACT(s[:, o:o + nb], PS[bank][:, 0:nb], AF.Gelu), reads=[PSR[bank]], writes=[sr])
                        elif kind == "G":
                            S.add("act", ACT(s[:, o:o + nb], PS[bank][:, 0:nb], AF.Sigmoid), reads=[PSR[bank]], writes=[sr])
                        elif not rope:
                            S.add("act", ACT(s[:, o:o + nb], PS[bank][:, 0:nb], AF.Copy), reads=[PSR[bank]], writes=[sr])
                        else:
                            a1, r1 = t1.next()
                            a2, r2 = t2.next()
                            S.add("act", ACT(a1[:, 0:nb], PS[bank][:, 0:nb], AF.Copy), reads=[PSR[bank]], writes=[r1])
                            S.add("pe", MM(PS[6][:, 0:nb], rmat32, a1[:, 0:nb]), reads=[r1, cR], writes=[PSR[6]])
                            S.add("dve", TT(a2[:, 0:nb], PS[6][:, 0:nb], cs[:, 1, o:o + nb], ALU.mult), reads=[PSR[6], csr], writes=[r2])
                            S.add("dve", TT(a1[:, 0:nb], a1[:, 0:nb], cs[:, 0, o:o + nb], ALU.mult), reads=[r1, csr], writes=[r1])
                            S.add("dve", TT(s[:, o:o + nb], a1[:, 0:nb], a2[:, 0:nb], ALU.add), reads=[r1, r2], writes=[sr])
                    if kind == "A":
                        dst = a_s[l][tile][gc * 128:(gc + 1) * 128, :]
                    elif kind == "U":
                        dst = u_s[gc * 128:(gc + 1) * 128, 0:n]
                    elif kind == "G":
                        dst = g_s[gc * 128:(gc + 1) * 128, 0:n]
                    elif kind == "Q":
                        dst = q_s[gc * 128:(gc + 1) * 128, 0:n]
                    else:
                        dst = KT[l][gc, :, koff:koff + n]
                    S.add("sp", DMA(dst, s), reads=[sr], dma=True)

    def stage_pool(l, tile):
        ti = tiles[tile]
        n = ti["n"]
        L = n + 16
        blks = blocks(n)
        stage()
        P = A.alloc([8, n], BF16)
        Pr = [Reg() for _ in range(8)]
        pw = A.alloc([4, 2, 256], BF16)
        pwr = Reg()
        for g in range(4):
            S.add("pool", DMA(pw[:, g], pool_w[l, g].rearrange("(cc p) d -> p cc d", p=128)), writes=[pwr], dma=True)
        psc = A.alloc([8], F32)
        S.add("sp", DMA(psc, pool_sc[l]), writes=[pwr], dma=True)
        pe_t = A.alloc([4, 16], F32)
        S.add("sp", DMA(pe_t, pedge[tile]), writes=[pwr], dma=True)
        ab = Rot([A.alloc([L], F32) for _ in range(2)])
        wa = Rot([A.alloc([L], F32) for _ in range(2)])
        wb = Rot([A.alloc([L], F32) for _ in range(2)])
        ed = Rot([A.alloc([16], F32) for _ in range(2)])
        nb_tile = {"own": "oth", "oth": "own"}.get(tile)
        mi = {"own": 0, "oth": 2}.get(tile, 0)
        for c in range(8):
            g = c // 2
            a, ar = ab.next()
            src = a_s[l][tile][c * 128:(c + 1) * 128, :]
            S.add("sp", DMA(a[:, 8:8 + n], src), writes=[ar], dma=True)
            if nb_tile is None:
                S.add("dve", lambda e, x=a[:, 0:8]: e.memset(x, 0.0), writes=[ar])
                S.add("dve", lambda e, x=a[:, 8 + n:L]: e.memset(x, 0.0), writes=[ar])
            else:
                nsrc = a_s[l][nb_tile][c * 128:(c + 1) * 128, :]
                S.add("sp", DMA(a[:, 0:8], nsrc[:, n - 8:n]), writes=[ar], dma=True)
                S.add("sp", DMA(a[:, 8 + n:L], nsrc[:, 0:8]), writes=[ar], dma=True)
                S.add("dve", TS(a[:, 0:8], a[:, 0:8], hmask_t[:, mi:mi + 1], ALU.mult), reads=[ar, cR], writes=[ar])
                S.add("dve", TS(a[:, 8 + n:L], a[:, 8 + n:L], hmask_t[:, mi + 1:mi + 2], ALU.mult), reads=[ar, cR], writes=[ar])
            w1, w1r = wa.next()
            w2, w2r = wb.next()
            S.add("dve", TT(w1[:, 1:L], a[:, 0:L - 1], a[:, 1:L], ALU.add), reads=[ar], writes=[w1r])
            cur, curr = w1, w1r
            if g >= 1:
                S.add("dve", TT(w2[:, 2:L - 1], w1[:, 1:L - 2], w1[:, 3:L], ALU.add), reads=[w1r], writes=[w2r])
                cur, curr = w2, w2r
            if g >= 2:
                S.add("dve", TT(w1[:, 4:L - 3], w2[:, 2:L - 5], w2[:, 6:L - 1], ALU.add), reads=[w2r], writes=[w1r])
                cur, curr = w1, w1r
            if g >= 3:
                S.add("dve", TT(w2[:, 8:L - 7], w1[:, 4:L - 11], w1[:, 12:L - 3], ALU.add), reads=[w1r], writes=[w2r])
                cur, curr = w2, w2r
            wsz = float(2 ** (g + 1))
            S.add("dve", STT(P[:, c, :], cur[:, 8:8 + n], 1.0 / wsz, a[:, 8:8 + n], ALU.mult, ALU.subtract), reads=[curr, ar], writes=[Pr[c]])
            e_, er = ed.next()
            for (lo, eo) in ((0, 0), (n - 8, 8)):
                S.add("dve", TT(e_[:, eo:eo + 8], cur[:, 8 + lo:16 + lo], pe_t[:, g, eo:eo + 8], ALU.mult), reads=[curr, pwr], writes=[er])
                S.add("dve", TT(P[:, c, lo:lo + 8], e_[:, eo:eo + 8], a[:, 8 + lo:16 + lo], ALU.subtract), reads=[er, ar, Pr[c]], writes=[Pr[c]])
        st = Rot([A.alloc([n], BF16) for _ in range(2)])
        bk = 0
        for g in range(4):
            for dd in range(2):
                s, sr = st.next()
                for (o, nb) in blks:
                    bank = bk % 4
                    bk += 1
                    for cc in range(2):
                        S.add("pe", MM(PS[bank][:, 0:nb], pw[:, g, cc, dd * 128:(dd + 1) * 128], P[:, 2 * g + cc, o:o + nb], cc == 0, cc == 1),
                              reads=[pwr, Pr[2 * g + cc]], writes=[PSR[bank]])
                    S.add("act", ACT(s[:, o:o + nb], PS[bank][:, 0:nb], AF.Copy, scale=psc[:, 2 * g + dd:2 * g + dd + 1]), reads=[PSR[bank], pwr], writes=[sr])
                S.add("sp", DMA(ao_s[(2 * g + dd) * 128:(2 * g + dd + 1) * 128, 0:n], s), reads=[sr], dma=True)

    def stage_sgu(l, tile):
        n = tiles[tile]["n"]
        ntt = n // 128
        stage()
        U = A.alloc([8, n], BF16)
        Ur = Reg()
        S.add("sp", DMA(U, u_s[:, 0:n].rearrange("(g p) t -> p g t", p=128)), writes=[Ur], dma=True)
        wt = A.alloc([8, 128], BF16)
        S.add("pool", DMA(wt, sgu_wT[l].rearrange("g q p -> q g p")), writes=[Ur], dma=True)
        bt = A.alloc([8, 128], F32)
        S.add("sp", DMA(bt, sgu_b[l].partition_broadcast(128).rearrange("p (g q) -> p g q", g=8)), writes=[Ur], dma=True)
        vn = Rot([A.alloc([1024], BF16) for _ in range(2)])
        tp = Rot([A.alloc([4, 128], F32) for _ in range(2)])
        BO = A.alloc([8, n], BF16)
        BOr = Reg()
        for tt in range(ntt):
            v, vr = vn.next()
            S.add("sp", DMA(v, vn_s[tt * 128:(tt + 1) * 128, :]), writes=[vr], dma=True)
            for gh in range(2):
                bank = (2 * tt + gh) % 4
                for gg in range(4):
                    g = gh * 4 + gg
                    S.add("pe", MM(PS[bank][:, gg * 128:(gg + 1) * 128], v[:, g * 128:(g + 1) * 128], wt[:, g, :]), reads=[vr, Ur], writes=[PSR[bank]])
                t, tr = tp.next()
                S.add("dve", TT(t, PS[bank].rearrange("p (a b) -> p a b", a=4), bt[:, gh * 4:(gh + 1) * 4, :], ALU.add), reads=[PSR[bank], Ur], writes=[tr])
                S.add("dve", TT(BO[:, gh * 4:(gh + 1) * 4, tt * 128:(tt + 1) * 128], t, U[:, gh * 4:(gh + 1) * 4, tt * 128:(tt + 1) * 128], ALU.mult),
                      reads=[tr, Ur], writes=[BOr])
        S.add("sp", DMA(bo_s[:, 0:n].rearrange("(g p) t -> p g t", p=128), BO), reads=[BOr], dma=True)

    def stage_att(l, tile):
        ti = tiles[tile]
        n = ti["n"]
        blks = blocks(n)
        if tile == "ctx":
            k0, nk = 2 * T, CTX
        else:
            k0, nk = 0, NK
        nkc = nk // 128
        stage()
        qb = Rot([A.alloc([n], BF16) for _ in range(2)])
        kb = Rot([A.alloc([nk], BF16) for _ in range(2)])
        vb = Rot([A.alloc([nkc, 128], BF16) for _ in range(2)])
        eb = [Rot([A.alloc([512], BF16) for _ in range(3)]) for _ in range(2)]
        rc = [A.alloc([512], F32) for _ in range(2)]
        o1 = A.alloc([512], F32)
        o2 = A.alloc([512], F32)
        osq = A.alloc([512], F32)
        rs = A.alloc([512], F32)
        fr = Reg()
        cst = Rot([A.alloc([n], BF16) for _ in range(2)])
        items = [(h, o, nb) for h in range(NH) for (o, nb) in blks]
        loaded = {}

        def load_head(h):
            q, qr = qb.next()
            k, kr = kb.next()
            v, vr = vb.next()
            S.add("sp", DMA(q, q_s[h * 128:(h + 1) * 128, 0:n]), writes=[qr], dma=True)
            S.add("sp", DMA(k, KT[l][h, :, k0:k0 + nk]), writes=[kr], dma=True)
            S.add("sp", DMA(v, VV[l][k0:k0 + nk, h * 128:(h + 1) * 128].rearrange("(c p) d -> p c d", p=128)), writes=[vr], dma=True)
            loaded[h] = (q, qr, k, kr, v, vr)

        def emit_S(h, o, nb, kc):
            q, qr, k, kr, v, vr = loaded[h]
            for i in range(2):
                bank = 2 * (kc % 2) + i
                S.add("pe", MM(PS[bank][:, 0:nb], k[i * 64:(i + 1) * 64, kc * 128:(kc + 1) * 128], q[i * 64:(i + 1) * 64, o:o + nb]),
                      reads=[kr, qr], writes=[PSR[bank]])

        load_head(0)
        cs_, csr = None, None
        for idx, (h, o, nb) in enumerate(items):
            if o == 0:
                if h + 1 < NH:
                    load_head(h + 1)
                cs_, csr = cst.next()
            q, qr, k, kr, v, vr = loaded[h]
            if idx == 0:
                emit_S(h, o, nb, 0)
            for kc in range(nkc):
                if kc + 1 < nkc:
                    emit_S(h, o, nb, kc + 1)
                ebs = []
                for i in range(2):
                    bank = 2 * (kc % 2) + i
                    e_, er = eb[i].next()
                    S.add("act", ACT(e_[:, 0:nb], PS[bank][:, 0:nb], AF.Exp, scale=0.125), reads=[PSR[bank]], writes=[er])
                    ebs.append((e_, er))
                for i in range(2):
                    e_, er = ebs[i]
                    S.add("pe", MM(PS[4 + i][:, 0:nb], v[:, kc, :], e_[:, 0:nb], kc == 0, kc == nkc - 1), reads=[vr, er], writes=[PSR[4 + i]])
                    S.add("pe", MM(PS[6 + i][:, 0:nb], onesbf, e_[:, 0:nb], kc == 0, kc == nkc - 1), reads=[cR, er], writes=[PSR[6 + i]])
            if idx + 1 < len(items):
                emit_S(items[idx + 1][0], items[idx + 1][1], items[idx + 1][2], 0)
            for i in range(2):
                S.add("dve", lambda e, a=rc[i][:, 0:nb], b=PS[6 + i][:, 0:nb]: e.reciprocal(out=a, in_=b), reads=[PSR[6 + i]], writes=[fr])
            S.add("dve", TT(o1[:, 0:nb], PS[4][:, 0:nb], rc[0][:, 0:nb], ALU.mult), reads=[PSR[4], fr], writes=[fr])
            S.add("dve", TT(o2[:, 0:nb], PS[5][:, 0:nb], rc[1][:, 0:nb], ALU.mult), reads=[PSR[5], fr], writes=[fr])
            S.add("dve", STT(o1[:, 0:nb], o2[:, 0:nb], lam_t[:, l, 0:1], o1[:, 0:nb], ALU.mult, ALU.add), reads=[fr, cR], writes=[fr])
            S.add("dve", TT(osq[:, 0:nb], o1[:, 0:nb], o1[:, 0:nb], ALU.mult), reads=[fr], writes=[fr])
            S.add("pe", MM(PS[6][:, 0:nb], ones32, osq[:, 0:nb]), reads=[fr, cR], writes=[PSR[6]])
            S.add("act", ACT(rs[:, 0:nb], PS[6][:, 0:nb], AF.Sqrt, bias=EPS_AP, scale=1.0 / 128), reads=[PSR[6], cR], writes=[fr])
            S.add("dve", lambda e, a=rs[:, 0:nb]: e.reciprocal(out=a, in_=a), reads=[fr], writes=[fr])
            S.add("dve", STT(cs_[:, o:o + nb], o1[:, 0:nb], subg_t[:, l:l + 1], rs[:, 0:nb], ALU.mult, ALU.mult), reads=[fr, cR], writes=[csr, fr])
            if o + nb >= n:
                S.add("sp", DMA(co_s[h * 128:(h + 1) * 128, 0:n], cs_), reads=[csr], dma=True)

    def stage_merge(l, tile):
        ti = tiles[tile]
        n = ti["n"]
        mc = ti["mc"]
        blks = blocks(n)
        stage()
        BR = []
        Rr = Reg()
        for src in (ao_s, bo_s, co_s):
            b = A.alloc([8, n], BF16)
            S.add("sp", DMA(b, src[:, 0:n].rearrange("(g p) t -> p g t", p=128)), writes=[Rr], dma=True)
            BR.append(b)
        M = A.alloc([KC, n], BF16)
        Mr = [Reg() for _ in range(KC)]
        wp = Rot([A.alloc([3, 8, 128], BF16) for _ in range(3)])
        gb = Rot([A.alloc([3, n], BF16) for _ in range(3)])
        tm = Rot([A.alloc([512], F32) for _ in range(2)])
        tm2 = Rot([A.alloc([512], F32) for _ in range(2)])
        bk = 0
        for j in range(KC):
            w, wr = wp.next()
            for xi, wsrc_ in enumerate((w_pa, w_pb, w_pc)):
                S.add("pool", DMA(w[:, xi], wsrc_[l].rearrange("(kc p) d -> p kc d", p=128)[:, :, j * 128:(j + 1) * 128]), writes=[wr], dma=True)
            g, gr = gb.next()
            S.add("sp", DMA(g, g_s[:, 0:n].rearrange("(x j p) t -> j p x t", x=3, p=128)[j]), writes=[gr], dma=True)
            for (o, nb) in blks:
                t, tr = tm.next()
                t2, t2r = tm2.next()
                for xi in range(3):
                    bank = bk % 6
                    bk += 1
                    for kc in range(8):
                        S.add("pe", MM(PS[bank][:, 0:nb], w[:, xi, kc, :], BR[xi][:, kc, o:o + nb], kc == 0, kc == 7), reads=[wr, Rr], writes=[PSR[bank]])
                    if xi == 0:
                        S.add("dve", TT(t[:, 0:nb], PS[bank][:, 0:nb], g[:, 0, o:o + nb], ALU.mult), reads=[PSR[bank], gr], writes=[tr])
                    elif xi == 1:
                        S.add("dve", TT(t2[:, 0:nb], PS[bank][:, 0:nb], g[:, 1, o:o + nb], ALU.mult), reads=[PSR[bank], gr], writes=[t2r])
                        S.add("dve", TT(t[:, 0:nb], t[:, 0:nb], t2[:, 0:nb], ALU.add), reads=[tr, t2r], writes=[tr])
                    else:
                        S.add("dve", TT(t2[:, 0:nb], PS[bank][:, 0:nb], g[:, 2, o:o + nb], ALU.mult), reads=[PSR[bank], gr, tr], writes=[t2r])
                        S.add("dve", TT(M[:, j, o:o + nb], t[:, 0:nb], t2[:, 0:nb], ALU.add), reads=[tr, t2r], writes=[Mr[j]])
        ws = Rot([A.alloc([KC, 512], BF16) for _ in range(2)])
        xr = Rot([A.alloc([n], F32) for _ in range(3)])
        gt = modv[:, l, mc, 2, :]
        src = xcur[tile]
        for bi in range(4):
            w, wr = ws.next()
            S.add("pool", DMA(w, wsrc(w_o[l], bi * 512, (bi + 1) * 512)), writes=[wr], dma=True)
            for c in range(4):
                jj = bi * 4 + c
                x, xreg = xr.next()
                S.add("sp", DMA(x, src[jj * 128:(jj + 1) * 128, :]), writes=[xreg], dma=True)
                for (o, nb) in blks:
                    bank = bk % 6
                    bk += 1
                    for kc in range(KC):
                        S.add("pe", MM(PS[bank][:, 0:nb], w[:, kc, c * 128:(c + 1) * 128], M[:, kc, o:o + nb], kc == 0, kc == KC - 1),
                              reads=[wr, Mr[kc]], writes=[PSR[bank]])
                    S.add("dve", STT(x[:, o:o + nb], PS[bank][:, 0:nb], gt[:, jj:jj + 1], x[:, o:o + nb], ALU.mult, ALU.add), reads=[PSR[bank], xreg, cR], writes=[xreg])
                S.add("sp", DMA(xs[tile][jj * 128:(jj + 1) * 128, :], x), reads=[xreg], dma=True)
        xcur[tile] = xs[tile]

    def stage_ffn(l, tile, moe, final, store_x=True):
        ti = tiles[tile]
        n = ti["n"]
        mc = ti["mc"]
        blks = blocks(n)
        stage()
        X = A.alloc([KC, n], F32)
        Xr = [[Reg() for _ in blks] for _ in range(KC)]
        for kc in range(KC):
            S.add("sp", DMA(X[:, kc, :], xcur[tile][kc * 128:(kc + 1) * 128, :]), writes=Xr[kc], dma=True)
        H2 = A.alloc([KC, n], BF16)
        Hr = Reg()
        S.add("sp", DMA(H2, h2_s[:, 0:n].rearrange("(kc p) t -> p kc t", p=128)), writes=[Hr], dma=True)
        FG = 2
        w1b = Rot([A.alloc([KC, FG * 128], BF16) for _ in range(2)])
        w3b = Rot([A.alloc([KC, FG * 128], BF16) for _ in range(2)])
        w2b = Rot([A.alloc([FG, D], BF16) for _ in range(3)])
        Gb = Rot([A.alloc([FG, n], BF16) for _ in range(2)])
        sb = Rot([A.alloc([512], F32) for _ in range(2)])
        cbuf = Rot([A.alloc([n], F32) for _ in range(2)]) if moe else None
        gt = modv[:, l, mc, 5, :]
        F = DFFE if moe else DFF
        nfc = F // 128
        assert nfc % FG == 0
        bk = [0]
        groups = [(e_, fg) for e_ in range(NE if moe else 1) for fg in range(nfc // FG)]
        wl = {}
        cbs = {}

        def load_w(gi):
            e_, fg = groups[gi]
            W1 = moe_w1[e_] if moe else ffn_w1[0]
            W3 = moe_w3[e_] if moe else ffn_w3[0]
            W2 = moe_w2[e_] if moe else ffn_w2[0]
            f0 = fg * FG * 128
            w1, w1r = w1b.next()
            w3, w3r = w3b.next()
            w2, w2r = w2b.next()
            S.add("pool", DMA(w1, wsrc(W1, f0, f0 + FG * 128)), writes=[w1r], dma=True)
            S.add("pool", DMA(w3, wsrc(W3, f0, f0 + FG * 128)), writes=[w3r], dma=True)
            S.add("pool", DMA(w2, W2[f0:f0 + FG * 128, :].rearrange("(fc p) d -> p fc d", p=128)), writes=[w2r], dma=True)
            wl[gi] = (w1, w1r, w3, w3r, w2, w2r)
            if moe and fg == 0:
                cb, cbr = cbuf.next()
                S.add("sp", DMA(cb, comb_s[e_, :, 0:n]), writes=[cbr], dma=True)
                cbs[e_] = (cb, cbr)

        Gs = {}

        def up_parts(gi):
            e_, fg = groups[gi]
            w1, w1r, w3, w3r, w2, w2r = wl[gi]
            G, Gr = Gb.next()
            Gs[gi] = (G, Gr)
            parts = []
            for fc in range(FG):
                for (o, nb) in blks:
                    def part(fc=fc, o=o, nb=nb):
                        b1 = bk[0] % 4
                        b3 = (bk[0] + 1) % 4
                        bk[0] += 2
                        for kc in range(KC):
                            S.add("pe", MM(PS[b1][:, 0:nb], w1[:, kc, fc * 128:(fc + 1) * 128], H2[:, kc, o:o + nb], kc == 0, kc == KC - 1),
                                  reads=[w1r, Hr], writes=[PSR[b1]])
                        for kc in range(KC):
                            S.add("pe", MM(PS[b3][:, 0:nb], w3[:, kc, fc * 128:(fc + 1) * 128], H2[:, kc, o:o + nb], kc == 0, kc == KC - 1),
                                  reads=[w3r, Hr], writes=[PSR[b3]])
                        s_, sr = sb.next()
                        S.add("act", ACT(s_[:, 0:nb], PS[b1][:, 0:nb], AF.Silu), reads=[PSR[b1]], writes=[sr])
                        if moe:
                            cb, cbr = cbs[e_]
                            S.add("dve", TT(s_[:, 0:nb], s_[:, 0:nb], PS[b3][:, 0:nb], ALU.mult), reads=[sr, PSR[b3]], writes=[sr])
                            S.add("dve", TT(G[:, fc, o:o + nb], s_[:, 0:nb], cb[:, o:o + nb], ALU.mult), reads=[sr, cbr], writes=[Gr])
                        else:
                            S.add("dve", TT(G[:, fc, o:o + nb], s_[:, 0:nb], PS[b3][:, 0:nb], ALU.mult), reads=[sr, PSR[b3]], writes=[Gr])
                    parts.append(part)
            return parts

        def down_parts(gi, nparts):
            w1, w1r, w3, w3r, w2, w2r = wl[gi]
            G, Gr = Gs[gi]
            units = [(jj, bi, o, nb) for jj in range(KC) for bi, (o, nb) in enumerate(blks)]
            per = (len(units) + nparts - 1) // nparts
            parts = []
            for k in range(nparts):
                def part(us=units[k * per:(k + 1) * per]):
                    for (jj, bi, o, nb) in us:
                        bank = 4 + (jj * len(blks) + bi) % 4
                        for fc in range(FG):
                            S.add("pe", MM(PS[bank][:, 0:nb], w2[:, fc, jj * 128:(jj + 1) * 128], G[:, fc, o:o + nb], fc == 0, fc == FG - 1),
                                  reads=[w2r, Gr], writes=[PSR[bank]])
                        S.add("dve", STT(X[:, jj, o:o + nb], PS[bank][:, 0:nb], gt[:, jj:jj + 1], X[:, jj, o:o + nb], ALU.mult, ALU.add),
                              reads=[PSR[bank], Xr[jj][bi], cR], writes=[Xr[jj][bi]])
                parts.append(part)
            return parts

        load_w(0)
        for gi in range(len(groups)):
            if gi + 1 < len(groups):
                load_w(gi + 1)
            ups = up_parts(gi)
            downs = down_parts(gi - 1, len(ups)) if gi > 0 else [None] * len(ups)
            for k in range(len(ups)):
                ups[k]()
                if downs[k] is not None:
                    downs[k]()
        for p_ in down_parts(len(groups) - 1, 1):
            p_()
        acc = A.alloc([n], F32)
        accr = [Reg() for _ in blks]
        if moe:
            rstd, creg0 = cbuf.items[0]
            extra = [creg0]
        else:
            rstd = A.alloc([n], F32)
            extra = []
        rr = [Reg() for _ in blks]
        for kc in range(KC):
            for bi, (o, nb) in enumerate(blks):
                if kc == 0:
                    S.add("act", ACT(acc[:, o:o + nb], X[:, kc, o:o + nb], AF.Square), reads=[Xr[kc][bi]], writes=[accr[bi]])
                else:
                    s_, sr = sb.next()
                    S.add("act", ACT(s_[:, 0:nb], X[:, kc, o:o + nb], AF.Square), reads=[Xr[kc][bi]], writes=[sr])
                    S.add("dve", TT(acc[:, o:o + nb], acc[:, o:o + nb], s_[:, 0:nb], ALU.add), reads=[sr, accr[bi]], writes=[accr[bi]])
        for bi, (o, nb) in enumerate(blks):
            S.add("pe", MM(PS[bi][:, 0:nb], ones32, acc[:, o:o + nb]), reads=[accr[bi], cR], writes=[PSR[bi]])
            s_, sr = sb.next()
            S.add("act", ACT(s_[:, 0:nb], PS[bi][:, 0:nb], AF.Sqrt, bias=EPS_AP, scale=1.0 / D), reads=[PSR[bi], cR], writes=[sr])
            S.add("dve", lambda e, a=rstd[:, o:o + nb], b=s_[:, 0:nb]: e.reciprocal(out=a, in_=b), reads=[sr], writes=[rr[bi]] + extra)
        if not final:
            if store_x:
                for kc in range(KC):
                    S.add("sp", DMA(xs[tile][kc * 128:(kc + 1) * 128, :], X[:, kc, :]), reads=Xr[kc], dma=True)
                xcur[tile] = xs[tile]
            Av = modv[:, l + 1, mc, 0, :]
            Bv = modv[:, l + 1, mc, 1, :]
            for kc in range(KC):
                for bi, (o, nb) in enumerate(blks):
                    s_, sr = sb.next()
                    S.add("dve", STT(s_[:, 0:nb], X[:, kc, o:o + nb], Av[:, kc:kc + 1], rstd[:, o:o + nb], ALU.mult, ALU.mult),
                          reads=[Xr[kc][bi], rr[bi], cR], writes=[sr])
                    S.add("act", ACT(H2[:, kc, o:o + nb], s_[:, 0:nb], AF.Identity, bias=Bv[:, kc:kc + 1]), reads=[sr, cR], writes=[Hr])
            S.add("sp", DMA(h_s[tile].rearrange("(kc p) t -> p kc t", p=128), H2), reads=[Hr], dma=True)
            return
        for kc in range(KC):
            for bi, (o, nb) in enumerate(blks):
                S.add("dve", STT(X[:, kc, o:o + nb], X[:, kc, o:o + nb], fng_t[:, kc:kc + 1], rstd[:, o:o + nb], ALU.mult, ALU.mult),
                      reads=[Xr[kc][bi], rr[bi], cR], writes=[Xr[kc][bi]])
            S.add("sp", DMA(outT[kc * 128:(kc + 1) * 128, :], X[:, kc, :]), reads=Xr[kc], dma=True)

    A.base = A.off
    eps_t = A.alloc([1], F32)
    S.add("dve", lambda e: e.memset(eps_t, EPS), writes=[cR])
    EPS_AP = eps_t
    A.base = A.off

    upto = cfg.get("upto", 99)
    stage_mod()
    step = [0]

    def go(fn, *a, **k):
        step[0] += 1
        if step[0] <= upto:
            fn(*a, **k)

    for t in ("oth", "ctx", "own"):
        go(stage_norm, 0, t, 0, h_s[t])
        go(stage_win, 0, t, ["K", "V", "A"])
    for t in ("oth", "ctx", "own"):
        go(stage_win, 0, t, ["U", "VS", "Q", "G"])
        go(stage_pool, 0, t)
        go(stage_sgu, 0, t)
        go(stage_att, 0, t)
        go(stage_merge, 0, t)
        go(stage_norm, 0, t, 1, h2_s[:, 0:tiles[t]["n"]])
        go(stage_ffn, 0, t, False, False, store_x=(t == "own"))
        go(stage_win, 1, t, ["K", "V", "A"] if t != "ctx" else ["K", "V"])
    t = "own"
    go(stage_win, 1, t, ["U", "VS", "Q", "G"])
    go(stage_pool, 1, t)
    go(stage_sgu, 1, t)
    go(stage_att, 1, t)
    go(stage_merge, 1, t)
    go(stage_norm, 1, t, 1, h2_s[:, 0:T], moe=True)
    go(stage_ffn, 1, t, True, True)
    S.barrier()

    S.finalize()
    with nc.Block() as block:
        @block.tensor
        def _(e):
            S.emit("pe", e, csem, dsem)

        @block.scalar
        def _(e):
            S.emit("act", e, csem, dsem)

        @block.vector
        def _(e):
            S.emit("dve", e, csem, dsem)

        @block.gpsimd
        def _(e):
            S.emit("pool", e, csem, dsem)

        @block.sync
        def _(e):
            S.emit("sp", e, csem, dsem)
    for cm in reversed(ctxs):
        cm.__exit__(None, None, None)
    return nc


def _fm(v):
    v = np.asarray(v)
    n = v.shape[-1] // 128
    return np.ascontiguousarray(np.swapaxes(v.reshape(v.shape[:-1] + (n, 128)), -1, -2))


def _consts(cfg, s):
    SEQ, CTX = cfg["SEQ"], cfg["CTX"]
    T = SEQ // 2
    half = 32
    inv = np.power(10000.0, -np.arange(0, half, 2, dtype=np.float32) / half).astype(np.float32)
    out = {}
    for name, t0 in (("rope_own", s * T), ("rope_oth", (1 - s) * T)):
        t = np.arange(t0, t0 + T)
        row = (t // 64).astype(np.float32)
        col = (t % 64).astype(np.float32)
        ar = row[:, None] * inv[None, :]
        ac = col[:, None] * inv[None, :]
        ang = np.concatenate([ar, ar, ac, ac], axis=-1).astype(np.float32)
        cs = np.stack([np.cos(ang), np.sin(ang)]).astype(np.float32)
        cs = np.transpose(cs, (0, 2, 1))
        out[name] = np.ascontiguousarray(np.concatenate([cs, cs], axis=1))
    cm = np.zeros((3, 128, 128), np.float32)
    cm[0] = 1.0
    cm[1] = np.eye(128, dtype=np.float32)
    for m in range(128):
        if m % 32 < 16:
            cm[2, m + 16, m] = -1.0
        else:
            cm[2, m - 16, m] = 1.0
    out["cmat"] = cm
    hm = np.zeros((128, 4), np.float32)
    hm[:, 0], hm[:, 1] = float(s == 1), float(s == 0)
    hm[:, 2], hm[:, 3] = float(s == 0), float(s == 1)
    out["hmask"] = hm

    def edges(t0, n, Sq):
        e = np.zeros((128, 4, 16), np.float32)
        for g, w in enumerate((2, 4, 8, 16)):
            for idx, t in enumerate(list(range(t0, t0 + 8)) + list(range(t0 + n - 8, t0 + n))):
                lo = min(max(t - w // 2, 0), Sq)
                hi = min(max(t + w - w // 2, 0), Sq)
                e[:, g, idx] = 1.0 / float(hi - lo)
        return e
    out["pedge_own"] = edges(s * T, T, SEQ)
    out["pedge_oth"] = edges((1 - s) * T, T, SEQ)
    out["pedge_ctx"] = edges(0, CTX, CTX)
    return out


def prep_inputs(inp, cfg):
    SEQ = cfg["SEQ"]
    T = SEQ // 2
    f32 = lambda a: np.ascontiguousarray(np.asarray(a, dtype=np.float32))
    shared = dict(
        w_mod=f32(inp["w_mod"]), b_mod=_fm(f32(inp["b_mod"])), n1g=_fm(f32(inp["norm1_g"])), n2g=_fm(f32(inp["norm2_g"])),
        fng=_fm(f32(inp["final_norm_g"])), w_in=f32(inp["w_in"]), pool_w=f32(inp["pool_w"]), pool_sc=_fm(f32(inp["pool_scale"])),
        sgu_ng=f32(inp["sgu_norm_g"]), sgu_wT=np.ascontiguousarray(np.swapaxes(f32(inp["sgu_w"]), -1, -2)),
        sgu_b=f32(inp["sgu_b"]).reshape(2, 1024),
        lamv=np.ascontiguousarray(np.stack([f32(inp["lambda_q1"]), f32(inp["lambda_k1"]), f32(inp["lambda_q2"]), f32(inp["lambda_k2"])], axis=1)),
        subln=f32(inp["attn_subln_g"]).reshape(2, 128, 1),
        w_pa=f32(inp["w_proj_a"]), w_pb=f32(inp["w_proj_b"]), w_pc=f32(inp["w_proj_c"]), w_o=f32(inp["w_o"]),
        ffn_w1=f32(inp["ffn_w1"]), ffn_w3=f32(inp["ffn_w3"]), ffn_w2=f32(inp["ffn_w2"]),
        router=np.ascontiguousarray(np.transpose(f32(inp["moe_router"])[0].reshape(KC, 128, NE), (1, 0, 2))),
        moe_w1=f32(inp["moe_w1"])[0], moe_w3=f32(inp["moe_w3"])[0], moe_w2=f32(inp["moe_w2"])[0],
    )
    x = f32(inp["x"])
    ctx = f32(inp["ctx"])
    c = f32(inp["c"])
    cc = f32(inp["c_ctx"])
    maps = []
    for core in range(8):
        b, s = core // 2, core % 2
        m = dict(shared)
        m["xT_own"] = np.ascontiguousarray(x[b, s * T:(s + 1) * T, :].T)
        m["xT_oth"] = np.ascontiguousarray(x[b, (1 - s) * T:(2 - s) * T, :].T)
        m["xT_ctx"] = np.ascontiguousarray(ctx[b].T)
        m["cvec"] = np.ascontiguousarray(np.stack([_fm(c[b]), _fm(cc)], axis=-1))
        m.update(_consts(cfg, s))
        maps.append(m)
    return maps


def assemble(results, cfg):
    SEQ = cfg["SEQ"]
    T = SEQ // 2
    out = np.zeros((4, SEQ, D), np.float32)
    for core in range(8):
        b, s = core // 2, core % 2
        out[b, s * T:(s + 1) * T, :] = np.asarray(results[core]["outT"]).T
    return out


def kernel(**inputs):
    cfg = FULL_CFG
    nc = build(cfg)
    maps = prep_inputs(inputs, cfg)
    res = run_bass_kernel_spmd(nc, maps, core_ids=list(range(8)))
    return assemble(res.results, cfg)
```

```python
import numpy as np
import ml_dtypes
import concourse.bass as bass
import concourse.mybir as mybir
from concourse.bass_utils import run_bass_kernel_spmd

F32 = mybir.dt.float32
BF16 = mybir.dt.bfloat16
AF = mybir.ActivationFunctionType
ALU = mybir.AluOpType
AX = mybir.AxisListType

D = 2048
KC = 16
NH = 8
OFF_A, OFF_B, OFF_Q, OFF_K, OFF_V, OFF_G, INW = 0, 1024, 3072, 4096, 5120, 6144, 12288
EPS = 1e-6
NE = 8
FULL_CFG = dict(SEQ=2048, CTX=256, DFF=5632, DFFE=7168)
SAME_ENGINE_SYNC = True
NDS = 6
ARENA32 = 46 * 1024


class Reg:
    __slots__ = ("w", "rs")

    def __init__(self):
        self.w = None
        self.rs = {}


class Op:
    __slots__ = ("eng", "fn", "deps", "dma", "idx", "inc", "cnt", "dslot", "dval")


class Sched:
    ENG = ("pe", "act", "dve", "pool", "sp")

    def __init__(self):
        self.ops = {e: [] for e in self.ENG}
        self.ndma = {e: 0 for e in self.ENG}
        self.lastdma = {}

    def add(self, eng, fn, reads=(), writes=(), dma=False):
        op = Op()
        op.eng, op.fn, op.dma, op.inc, op.cnt = eng, fn, dma, False, 0
        deps = []
        for r in reads:
            if r.w is not None:
                deps.append(r.w)
        for w in writes:
            if w.w is not None:
                deps.append(w.w)
            deps.extend(w.rs.values())
        op.deps = deps
        op.idx = len(self.ops[eng])
        self.ops[eng].append(op)
        if dma:
            n = self.ndma[eng]
            self.ndma[eng] += 1
            op.dslot = n % NDS
            op.dval = 16 * (n // NDS + 1)
            self.lastdma[(eng, op.dslot)] = op
            key = (eng, "d", op.idx)
        else:
            key = eng
        for r in reads:
            r.rs[key] = op
        for w in writes:
            w.w = op
            w.rs = {}
        return op

    def barrier(self):
        lasts = []
        for e in self.ENG:
            for o in reversed(self.ops[e]):
                if o.fn is not None and not o.dma:
                    lasts.append(o)
                    break
        lasts.extend(self.lastdma.values())
        for e in self.ENG:
            op = Op()
            op.eng, op.fn, op.dma, op.inc, op.cnt = e, None, False, False, 0
            op.deps = list(lasts)
            op.idx = len(self.ops[e])
            self.ops[e].append(op)

    def _skip(self, op, d):
        if d.dma or op.dma or op.fn is None:
            return False
        if d.eng != op.eng:
            return False
        return op.eng == "pe" or not SAME_ENGINE_SYNC

    def finalize(self):
        for e in self.ENG:
            for op in self.ops[e]:
                for d in op.deps:
                    if not d.dma and not self._skip(op, d):
                        d.inc = True
        for e in self.ENG:
            c = 0
            for op in self.ops[e]:
                if op.inc:
                    c += 1
                op.cnt = c

    def emit(self, e, eng, csem, dsem):
        waited = {}
        for op in self.ops[e]:
            need = {}
            for d in op.deps:
                if d.dma:
                    key, val = ("d", d.eng, d.dslot), d.dval
                else:
                    if self._skip(op, d):
                        continue
                    key, val = d.eng, d.cnt
                if waited.get(key, 0) < val and need.get(key, 0) < val:
                    need[key] = val
            if op.dma and op.dval > 16:
                key = ("d", e, op.dslot)
                val = op.dval - 16
                if waited.get(key, 0) < val and need.get(key, 0) < val:
                    need[key] = val
            for key, val in need.items():
                sem = dsem[key[1]][key[2]] if isinstance(key, tuple) else csem[key]
                eng.wait_ge(sem, val)
                waited[key] = val
            if op.fn is None:
                continue
            ins = op.fn(eng)
            if op.dma:
                ins.then_inc(dsem[e][op.dslot], 16)
            elif op.inc:
                ins.then_inc(csem[e], 1)
        for (qe, slot), o in self.lastdma.items():
            if qe == e:
                eng.wait_ge(dsem[e][slot], o.dval)


def MM(out, lhsT, rhs, start=True, stop=True):
    return lambda e: e.matmul(out, lhsT=lhsT, rhs=rhs, start=start, stop=stop)


def ACT(out, in_, func, bias=None, scale=None):
    kw = {}
    if bias is not None:
        kw["bias"] = bias
    if scale is not None:
        kw["scale"] = scale
    return lambda e: e.activation(out=out, in_=in_, func=func, **kw)


def TT(out, a, b, op):
    return lambda e: e.tensor_tensor(out=out, in0=a, in1=b, op=op)


def TS(out, a, s1, op0, s2=None, op1=None):
    if op1 is None:
        return lambda e: e.tensor_scalar(out=out, in0=a, scalar1=s1, scalar2=None, op0=op0)
    return lambda e: e.tensor_scalar(out=out, in0=a, scalar1=s1, scalar2=s2, op0=op0, op1=op1)


def STT(out, in0, scalar, in1, op0, op1):
    return lambda e: e.scalar_tensor_tensor(out=out, in0=in0, scalar=scalar, in1=in1, op0=op0, op1=op1)


def CP(out, in_):
    return lambda e: e.tensor_copy(out=out, in_=in_)


def DMA(out, in_):
    return lambda e: e.dma_start(out=out, in_=in_)


def blocks(n):
    return [(o, min(512, n - o)) for o in range(0, n, 512)]


class Rot:
    def __init__(self, items):
        self.items = [(it, Reg()) for it in items]
        self.i = 0

    def next(self):
        it = self.items[self.i % len(self.items)]
        self.i += 1
        return it


def build(cfg, debug_outs=()):
    SEQ, CTX, DFF, DFFE = cfg["SEQ"], cfg["CTX"], cfg["DFF"], cfg["DFFE"]
    T = SEQ // 2
    NK = SEQ + CTX
    nc = bass.Bass("TRN2", target_bir_lowering=False)
    S = Sched()

    def din(name, shape, dt=F32):
        return nc.dram_tensor(name, list(shape), dt, kind="ExternalInput").ap()

    def dscr(name, shape, dt):
        kind = "ExternalOutput" if name in debug_outs else "Internal"
        return nc.dram_tensor(name, list(shape), dt, kind=kind).ap()

    tiles = {
        "own": dict(n=T, koff=0, rope=True, mc=0),
        "oth": dict(n=T, koff=T, rope=True, mc=0),
        "ctx": dict(n=CTX, koff=2 * T, rope=False, mc=1),
    }
    xin = {t: din("xT_" + t, [D, tiles[t]["n"]]) for t in tiles}
    cvec = din("cvec", [128, KC, 2])
    w_mod = din("w_mod", [2, D, 6 * D])
    b_mod = din("b_mod", [2, 128, 96])
    n1g = din("n1g", [2, 128, KC])
    n2g = din("n2g", [2, 128, KC])
    fng = din("fng", [128, KC])
    w_in = din("w_in", [2, D, INW])
    pool_w = din("pool_w", [2, 4, 256, 256])
    pool_sc = din("pool_sc", [2, 128, 8])
    sgu_ng = din("sgu_ng", [2, 1024])
    sgu_wT = din("sgu_wT", [2, 8, 128, 128])
    sgu_b = din("sgu_b", [2, 1024])
    lamv = din("lamv", [2, 4, 64])
    subln = din("subln", [2, 128, 1])
    w_pa = din("w_pa", [2, 1024, D])
    w_pb = din("w_pb", [2, 1024, D])
    w_pc = din("w_pc", [2, 1024, D])
    w_o = din("w_o", [2, D, D])
    ffn_w1 = din("ffn_w1", [1, D, DFF])
    ffn_w3 = din("ffn_w3", [1, D, DFF])
    ffn_w2 = din("ffn_w2", [1, DFF, D])
    router = din("router", [128, KC, NE])
    moe_w1 = din("moe_w1", [NE, D, DFFE])
    moe_w3 = din("moe_w3", [NE, D, DFFE])
    moe_w2 = din("moe_w2", [NE, DFFE, D])
    rope_t = {"own": din("rope_own", [2, 128, T]), "oth": din("rope_oth", [2, 128, T])}
    cmat = din("cmat", [3, 128, 128])
    hmask = din("hmask", [128, 4])
    pedge = {t: din("pedge_" + t, [128, 4, 16]) for t in tiles}
    outT = nc.dram_tensor("outT", [D, T], F32, kind="ExternalOutput").ap()

    h_s = {t: dscr("h_" + t, [D, tiles[t]["n"]], BF16) for t in tiles}
    xs = {t: dscr("xs_" + t, [D, tiles[t]["n"]], F32) for t in tiles}
    a_s = [{t: dscr("a%d_%s" % (l, t), [1024, tiles[t]["n"]], F32) for t in tiles} for l in range(2)]
    KT = [dscr("KT%d" % l, [NH, 128, NK], BF16) for l in range(2)]
    VV = [dscr("VV%d" % l, [NK, 1024], BF16) for l in range(2)]
    u_s = dscr("u_s", [1024, T], BF16)
    vn_s = dscr("vn_s", [T, 1024], BF16)
    q_s = dscr("q_s", [1024, T], BF16)
    g_s = dscr("g_s", [6144, T], BF16)
    ao_s = dscr("ao_s", [1024, T], BF16)
    bo_s = dscr("bo_s", [1024, T], BF16)
    co_s = dscr("co_s", [1024, T], BF16)
    h2_s = dscr("h2_s", [D, T], BF16)
    comb_s = dscr("comb_s", [NE, 128, T], F32)
    xcur = dict(xin)

    ctxs = []

    def enter(cm):
        ctxs.append(cm)
        return cm.__enter__()

    arena_t = enter(nc.sbuf_tensor("arena", [128, ARENA32], F32))
    psall = enter(nc.psum_tensor("psall", [128, 8 * 512], F32))
    PS = [psall[:, i * 512:(i + 1) * 512] for i in range(8)]
    PSR = [Reg() for _ in range(8)]
    csem = {e: enter(nc.semaphore("c_" + e)) for e in Sched.ENG}
    dsem = {e: [enter(nc.semaphore("d_%s%d" % (e, i))) for i in range(NDS)] for e in ("sp", "pool")}

    class Arena:
        def __init__(self):
            self.off = 0
            self.base = 0

        def alloc(self, shape, dt):
            ne = int(np.prod(shape))
            n32 = ne if dt == F32 else (ne + 1) // 2
            n32 = (n32 + 7) // 8 * 8
            a = arena_t[:, self.off:self.off + n32]
            self.off += n32
            assert self.off <= ARENA32, ("arena overflow", self.off)
            if dt != F32:
                a = a.bitcast(dt)
            a = a[:, 0:ne]
            if len(shape) == 2:
                a = a.rearrange("p (a b) -> p a b", a=shape[0])
            elif len(shape) == 3:
                a = a.rearrange("p (a b c) -> p a b c", a=shape[0], b=shape[1])
            elif len(shape) == 4:
                a = a.rearrange("p (a b c d) -> p a b c d", a=shape[0], b=shape[1], c=shape[2])
            return a

        def reset(self):
            self.off = self.base

    A = Arena()

    def stage():
        S.barrier()
        A.reset()

    ones32 = A.alloc([128], F32)
    ident32 = A.alloc([128], F32)
    rmat32 = A.alloc([128], F32)
    onesbf = A.alloc([128], BF16)
    hmask_t = A.alloc([4], F32)
    modv = A.alloc([2, 2, 6, KC], F32)
    lam_t = A.alloc([2, 2], F32)
    subg_t = A.alloc([2], F32)
    fng_t = A.alloc([KC], F32)
    cR = Reg()
    S.add("sp", DMA(ones32, cmat[0]), writes=[cR], dma=True)
    S.add("sp", DMA(ident32, cmat[1]), writes=[cR], dma=True)
    S.add("sp", DMA(rmat32, cmat[2]), writes=[cR], dma=True)
    S.add("sp", DMA(hmask_t, hmask), writes=[cR], dma=True)
    S.add("sp", DMA(fng_t, fng), writes=[cR], dma=True)
    S.add("dve", CP(onesbf, ones32), reads=[cR], writes=[cR])
    A.base = A.off

    def wsrc(w2d, c0, c1):
        return w2d.rearrange("(kc p) n -> p kc n", p=128)[:, :, c0:c1]

    def stage_mod():
        stage()
        cv = A.alloc([KC, 2], F32)
        sc = A.alloc([KC, 2], BF16)
        r0 = Reg()
        S.add("sp", DMA(cv, cvec), writes=[r0], dma=True)
        S.add("act", ACT(sc, cv, AF.Silu), reads=[r0], writes=[r0])
        ws = Rot([A.alloc([KC, 512], BF16) for _ in range(2)])
        lam_init = [0.8 - 0.6 * float(np.exp(-0.3 * l)) for l in range(2)]
        for l in range(2):
            bm = A.alloc([96], F32)
            mraw = A.alloc([96, 2], F32)
            g1 = A.alloc([KC], F32)
            g2 = A.alloc([KC], F32)
            rb = Reg()
            S.add("sp", DMA(bm, b_mod[l]), writes=[rb], dma=True)
            S.add("sp", DMA(g1, n1g[l]), writes=[rb], dma=True)
            S.add("sp", DMA(g2, n2g[l]), writes=[rb], dma=True)
            for bi in range(24):
                w, wr = ws.next()
                S.add("pool", DMA(w, wsrc(w_mod[l], bi * 512, (bi + 1) * 512)), writes=[wr], dma=True)
                bank = bi % 2
                for c in range(4):
                    j = bi * 4 + c
                    for kc in range(KC):
                        S.add("pe", MM(PS[bank][:, 2 * c:2 * c + 2], w[:, kc, c * 128:(c + 1) * 128], sc[:, kc, :],
                                       kc == 0, kc == KC - 1), reads=[wr, r0], writes=[PSR[bank]])
                S.add("dve", CP(mraw[:, bi * 4:(bi + 1) * 4, :], PS[bank][:, 0:8].rearrange("p (a b) -> p a b", a=4)),
                      reads=[PSR[bank]], writes=[rb])
            for col in range(2):
                S.add("dve", TT(mraw[:, :, col], mraw[:, :, col], bm, ALU.add), reads=[rb], writes=[rb])
            for col in range(2):
                mv = modv[:, l, col]
                for half, g in ((0, g1), (1, g2)):
                    sh = mraw[:, (3 * half + 0) * KC:(3 * half + 1) * KC, col]
                    scl = mraw[:, (3 * half + 1) * KC:(3 * half + 2) * KC, col]
                    gt = mraw[:, (3 * half + 2) * KC:(3 * half + 3) * KC, col]
                    S.add("dve", STT(mv[:, 3 * half + 0, :], scl, 1.0, g, ALU.add, ALU.mult), reads=[rb], writes=[rb])
                    S.add("dve", CP(mv[:, 3 * half + 1, :], sh), reads=[rb], writes=[rb])
                    S.add("dve", CP(mv[:, 3 * half + 2, :], gt), reads=[rb], writes=[rb])
            lv = A.alloc([4, 64], F32)
            lp = A.alloc([2, 64], F32)
            ls = A.alloc([2], F32)
            sg = A.alloc([1], F32)
            S.add("sp", DMA(lv, lamv[l].partition_broadcast(128)), writes=[rb], dma=True)
            S.add("sp", DMA(sg, subln[l]), writes=[rb], dma=True)
            S.add("dve", TT(lp[:, 0, :], lv[:, 0, :], lv[:, 1, :], ALU.mult), reads=[rb], writes=[rb])
            S.add("dve", TT(lp[:, 1, :], lv[:, 2, :], lv[:, 3, :], ALU.mult), reads=[rb], writes=[rb])
            S.add("dve", lambda e, lp=lp, ls=ls: e.tensor_reduce(out=ls, in_=lp, axis=AX.X, op=ALU.add), reads=[rb], writes=[rb])
            S.add("act", ACT(ls, ls, AF.Exp), reads=[rb], writes=[rb])
            S.add("dve", STT(lam_t[:, l, 0:1], ls[:, 1:2], -lam_init[l], ls[:, 0:1], ALU.add, ALU.subtract), reads=[rb], writes=[rb])
            S.add("dve", TS(subg_t[:, l:l + 1], sg, 1.0 - lam_init[l], ALU.mult), reads=[rb], writes=[rb])

    def stage_norm(l, tile, which, dst, moe=False):
        ti = tiles[tile]
        n = ti["n"]
        mc = ti["mc"]
        src = xcur[tile]
        stage()
        blks = blocks(n)
        X = A.alloc([KC, n], F32)
        Xr = [Reg() for _ in range(KC)]
        for kc in range(KC):
            S.add("sp", DMA(X[:, kc, :], src[kc * 128:(kc + 1) * 128, :]), writes=[Xr[kc]], dma=True)
        sq = Rot([A.alloc([n], F32) for _ in range(2)])
        acc = A.alloc([n], F32)
        accr = Reg()
        rstd = A.alloc([n], F32)
        rstd_r = [Reg() for _ in blks]
        tmp = Rot([A.alloc([n], F32) for _ in range(2)])
        hf = Rot([A.alloc([n], F32) for _ in range(2)])
        hst = Rot([A.alloc([n], BF16) for _ in range(3)])
        Av = modv[:, l, mc, 3 * which + 0, :]
        Bv = modv[:, l, mc, 3 * which + 1, :]
        for kc in range(KC):
            if kc == 0:
                S.add("act", ACT(acc, X[:, kc, :], AF.Square), reads=[Xr[kc]], writes=[accr])
            else:
                s_, sr = sq.next()
                S.add("act", ACT(s_, X[:, kc, :], AF.Square), reads=[Xr[kc]], writes=[sr])
                S.add("dve", TT(acc, acc, s_, ALU.add), reads=[sr, accr], writes=[accr])
        for bi, (o, nb) in enumerate(blks):
            S.add("pe", MM(PS[bi][:, 0:nb], ones32, acc[:, o:o + nb]), reads=[accr, cR], writes=[PSR[bi]])
            s_, sr = sq.next()
            S.add("act", ACT(s_[:, 0:nb], PS[bi][:, 0:nb], AF.Sqrt, bias=EPS_AP, scale=1.0 / D), reads=[PSR[bi], cR], writes=[sr])
            S.add("dve", lambda e, a=rstd[:, o:o + nb], b=s_[:, 0:nb]: e.reciprocal(out=a, in_=b), reads=[sr], writes=[rstd_r[bi]])
        if moe:
            wr_t = A.alloc([KC, NE], F32)
            wrr = Reg()
            S.add("sp", DMA(wr_t, router), writes=[wrr], dma=True)
        ntt = n // 128
        for kc in range(KC):
            x, xreg = X[:, kc, :], Xr[kc]
            t, tr = tmp.next()
            for bi, (o, nb) in enumerate(blks):
                S.add("dve", STT(t[:, o:o + nb], x[:, o:o + nb], Av[:, kc:kc + 1], rstd[:, o:o + nb], ALU.mult, ALU.mult),
                      reads=[xreg, rstd_r[bi]], writes=[tr])
            hs, hr = hst.next()
            if not moe:
                S.add("act", ACT(hs, t, AF.Identity, bias=Bv[:, kc:kc + 1]), reads=[tr], writes=[hr])
            else:
                f, fr = hf.next()
                S.add("act", ACT(f, t, AF.Identity, bias=Bv[:, kc:kc + 1]), reads=[tr], writes=[fr])
                S.add("dve", CP(hs, f), reads=[fr], writes=[hr])
                for tt in range(ntt):
                    S.add("pe", MM(PS[tt][:, 0:NE], f[:, tt * 128:(tt + 1) * 128], wr_t[:, kc, :], kc == 0, kc == KC - 1),
                          reads=[fr, wrr], writes=[PSR[tt]])
            S.add("sp", DMA(dst[kc * 128:(kc + 1) * 128, :], hs), reads=[hr], dma=True)
        if moe:
            comb = A.alloc([ntt, NE], F32)
            cr = [Reg() for _ in range(ntt)]
            sm = A.alloc([ntt, 24], F32)
            for tt in range(ntt):
                lg = sm[:, tt, 0:8]
                m8 = sm[:, tt, 8:16]
                ex = sm[:, tt, 16:24]
                r = cr[tt]
                S.add("dve", CP(lg, PS[tt][:, 0:NE]), reads=[PSR[tt]], writes=[r])
                S.add("dve", lambda e, a=m8, b=lg: e.max(out=a, in_=b), reads=[r], writes=[r])
                S.add("dve", TS(comb[:, tt, 0:1], m8[:, 0:1], -1.0, ALU.mult), reads=[r], writes=[r])
                S.add("act", ACT(ex, lg, AF.Exp, bias=comb[:, tt, 0:1]), reads=[r], writes=[r])
                S.add("dve", TS(lg, lg, m8[:, 1:2], ALU.is_ge), reads=[r], writes=[r])
                S.add("dve", TT(ex, ex, lg, ALU.mult), reads=[r], writes=[r])
                S.add("dve", lambda e, a=m8[:, 2:3], b=ex: e.tensor_reduce(out=a, in_=b, axis=AX.X, op=ALU.add), reads=[r], writes=[r])
                S.add("dve", lambda e, a=m8[:, 3:4], b=m8[:, 2:3]: e.reciprocal(out=a, in_=b), reads=[r], writes=[r])
                S.add("dve", TS(comb[:, tt, :], ex, m8[:, 3:4], ALU.mult), reads=[r], writes=[r])
            cb = Rot([A.alloc([128], F32) for _ in range(3)])
            cst = Rot([A.alloc([n], F32) for _ in range(2)])
            for e_ in range(NE):
                for tt in range(ntt):
                    c, creg = cb.next()
                    S.add("dve", CP(c, comb[:, tt, e_:e_ + 1].to_broadcast([128, 128])), reads=[cr[tt]], writes=[creg])
                    bank = 4 + 2 * (e_ % 2) + tt // 4
                    S.add("pe", MM(PS[bank][:, (tt % 4) * 128:(tt % 4 + 1) * 128], c, ident32), reads=[creg, cR], writes=[PSR[bank]])
                st, sr = cst.next()
                for bi, (o, nb) in enumerate(blks):
                    bank = 4 + 2 * (e_ % 2) + bi
                    S.add("act", ACT(st[:, o:o + nb], PS[bank][:, 0:nb], AF.Copy), reads=[PSR[bank]], writes=[sr])
                S.add("sp", DMA(comb_s[e_, :, 0:n], st), reads=[sr], dma=True)

    EPS_AP = None

    def stage_win(l, tile, kinds):
        ti = tiles[tile]
        n = ti["n"]
        koff = ti["koff"]
        rope = ti["rope"]
        blks = blocks(n)
        ntt = n // 128
        stage()
        H = A.alloc([KC, n], BF16)
        Hr = Reg()
        S.add("sp", DMA(H, h_s[tile].rearrange("(kc p) t -> p kc t", p=128)), writes=[Hr], dma=True)
        ws = Rot([A.alloc([KC, 512], BF16) for _ in range(2)])
        st32 = Rot([A.alloc([n], F32) for _ in range(2)])
        st16 = Rot([A.alloc([n], BF16) for _ in range(3)])
        stv = Rot([A.alloc([512], BF16) for _ in range(3)])
        if rope and ("Q" in kinds or "K" in kinds):
            cs = A.alloc([2, n], F32)
            csr = Reg()
            S.add("sp", DMA(cs, rope_t[tile].rearrange("a p t -> p a t")), writes=[csr], dma=True)
            t1 = Rot([A.alloc([512], F32) for _ in range(2)])
            t2 = Rot([A.alloc([512], F32) for _ in range(2)])
        if "VS" in kinds:
            vg = A.alloc([ntt, 1024], F32)
            vgr = [Reg() for _ in range(ntt)]
            ngt = A.alloc([1024], F32)
            ngr = Reg()
            S.add("sp", DMA(ngt, sgu_ng[l].partition_broadcast(128)), writes=[ngr], dma=True)
            bst = A.alloc([ntt, 2, 6], F32)
            mvv = A.alloc([ntt, 4], F32)
            stn = Rot([A.alloc([1024], BF16) for _ in range(2)])
        plan = {"A": (0, 2), "U": (2, 2), "VS": (4, 2), "Q": (6, 2), "K": (8, 2), "V": (10, 2), "G": (12, 12)}
        bank_i = [0]

        def nextbank():
            b = bank_i[0] % 6
            bank_i[0] += 1
            return b

        for kind in kinds:
            b0, nbk = plan[kind]
            for bb in range(nbk):
                bi = b0 + bb
                w, wr = ws.next()
                S.add("pool", DMA(w, wsrc(w_in[l], bi * 512, (bi + 1) * 512)), writes=[wr], dma=True)
                if kind in ("VS", "V"):
                    for tt in range(ntt):
                        bank = nextbank()
                        for kc in range(KC):
                            S.add("pe", MM(PS[bank], H[:, kc, tt * 128:(tt + 1) * 128], w[:, kc, :], kc == 0, kc == KC - 1),
                                  reads=[Hr, wr], writes=[PSR[bank]])
                        if kind == "V":
                            s, sr = stv.next()
                            S.add("act", ACT(s, PS[bank], AF.Copy), reads=[PSR[bank]], writes=[sr])
                            S.add("sp", DMA(VV[l][koff + tt * 128:koff + (tt + 1) * 128, bb * 512:(bb + 1) * 512], s), reads=[sr], dma=True)
                        else:
                            S.add("act", ACT(vg[:, tt, bb * 512:(bb + 1) * 512], PS[bank], AF.Gelu), reads=[PSR[bank]], writes=[vgr[tt]])
                            S.add("dve", lambda e, a=bst[:, tt, bb, :], b=vg[:, tt, bb * 512:(bb + 1) * 512]: e.bn_stats(out=a, in_=b),
                                  reads=[vgr[tt]], writes=[vgr[tt]])
                            if bb == 1:
                                r = vgr[tt]
                                S.add("dve", lambda e, a=mvv[:, tt, 0:2], b=bst[:, tt].rearrange("p a b -> p (a b)"): e.bn_aggr(out=a, in_=b),
                                      reads=[r], writes=[r])
                                S.add("act", ACT(mvv[:, tt, 2:3], mvv[:, tt, 1:2], AF.Sqrt, bias=EPS_AP), reads=[r, cR], writes=[r])
                                S.add("dve", lambda e, a=mvv[:, tt, 3:4], b=mvv[:, tt, 2:3]: e.reciprocal(out=a, in_=b), reads=[r], writes=[r])
                                S.add("dve", TS(vg[:, tt, :], vg[:, tt, :], mvv[:, tt, 0:1], ALU.subtract, mvv[:, tt, 3:4], ALU.mult),
                                      reads=[r], writes=[r])
                                s, sr = stn.next()
                                S.add("dve", TT(s, vg[:, tt, :], ngt, ALU.mult), reads=[r, ngr], writes=[sr])
                                S.add("sp", DMA(vn_s[tt * 128:(tt + 1) * 128, :], s), reads=[sr], dma=True)
                    continue
                for c in range(4):
                    gc = bb * 4 + c
                    if kind == "A":
                        s, sr = st32.next()
                    else:
                        s, sr = st16.next()
                    for (o, nb) in blks:
                        bank = nextbank()
                        for kc in range(KC):
                            S.add("pe", MM(PS[bank][:, 0:nb], w[:, kc, c * 128:(c + 1) * 128], H[:, kc, o:o + nb], kc == 0, kc == KC - 1),
                                  reads=[Hr, wr], writes=[PSR[bank]])
                        if kind == "A":
                            S.add("act", ACT(s[:, o:o + nb], PS[bank][:, 0:nb], AF.Copy), reads=[PSR[bank]], writes=[sr])
                        elif kind == "U":
                            S.add("act", ACT(s[:, o:o + nb], PS[bank][:, 0:nb], AF.Gelu), reads=[PSR[bank]], writes=[sr])
                        elif kind == "G":
                            S.add("act", ACT(s[:, o:o + nb], PS[bank][:, 0:nb], AF.Sigmoid), reads=[PSR[bank]], writes=[sr])
                        elif not rope:
                            S.add("act", ACT(s[:, o:o + nb], PS[bank][:, 0:nb], AF.Copy), reads=[PSR[bank]], writes=[sr])
                        else:
                            a1, r1 = t1.next()
                            a2, r2 = t2.next()
                            S.add("act", ACT(a1[:, 0:nb], PS[bank][:, 0:nb], AF.Copy), reads=[PSR[bank]], writes=[r1])
                            S.add("pe", MM(PS[6][:, 0:nb], rmat32, a1[:, 0:nb]), reads=[r1, cR], writes=[PSR[6]])
                            S.add("dve", TT(a2[:, 0:nb], PS[6][:, 0:nb], cs[:, 1, o:o + nb], ALU.mult), reads=[PSR[6], csr], writes=[r2])
                            S.add("dve", TT(a1[:, 0:nb], a1[:, 0:nb], cs[:, 0, o:o + nb], ALU.mult), reads=[r1, csr], writes=[r1])
                            S.add("dve", TT(s[:, o:o + nb], a1[:, 0:nb], a2[:, 0:nb], ALU.add), reads=[r1, r2], writes=[sr])
                    if kind == "A":
                        dst = a_s[l][tile][gc * 128:(gc + 1) * 128, :]
                    elif kind == "U":
                        dst = u_s[gc * 128:(gc + 1) * 128, 0:n]
                    elif kind == "G":
                        dst = g_s[gc * 128:(gc + 1) * 128, 0:n]
                    elif kind == "Q":
                        dst = q_s[gc * 128:(gc + 1) * 128, 0:n]
                    else:
                        dst = KT[l][gc, :, koff:koff + n]
                    S.add("sp", DMA(dst, s), reads=[sr], dma=True)

    def stage_pool(l, tile):
        ti = tiles[tile]
        n = ti["n"]
        L = n + 16
        blks = blocks(n)
        stage()
        P = A.alloc([8, n], BF16)
        Pr = [Reg() for _ in range(8)]
        pw = A.alloc([4, 2, 256], BF16)
        pwr = Reg()
        for g in range(4):
            S.add("pool", DMA(pw[:, g], pool_w[l, g].rearrange("(cc p) d -> p cc d", p=128)), writes=[pwr], dma=True)
        psc = A.alloc([8], F32)
        S.add("sp", DMA(psc, pool_sc[l]), writes=[pwr], dma=True)
        pe_t = A.alloc([4, 16], F32)
        S.add("sp", DMA(pe_t, pedge[tile]), writes=[pwr], dma=True)
        ab = Rot([A.alloc([L], F32) for _ in range(2)])
        wa = Rot([A.alloc([L], F32) for _ in range(2)])
        wb = Rot([A.alloc([L], F32) for _ in range(2)])
        ed = Rot([A.alloc([16], F32) for _ in range(2)])
        nb_tile = {"own": "oth", "oth": "own"}.get(tile)
        mi = {"own": 0, "oth": 2}.get(tile, 0)
        for c in range(8):
            g = c // 2
            a, ar = ab.next()
            src = a_s[l][tile][c * 128:(c + 1) * 128, :]
            S.add("sp", DMA(a[:, 8:8 + n], src), writes=[ar], dma=True)
            if nb_tile is None:
                S.add("dve", lambda e, x=a[:, 0:8]: e.memset(x, 0.0), writes=[ar])
                S.add("dve", lambda e, x=a[:, 8 + n:L]: e.memset(x, 0.0), writes=[ar])
            else:
                nsrc = a_s[l][nb_tile][c * 128:(c + 1) * 128, :]
                S.add("sp", DMA(a[:, 0:8], nsrc[:, n - 8:n]), writes=[ar], dma=True)
                S.add("sp", DMA(a[:, 8 + n:L], nsrc[:, 0:8]), writes=[ar], dma=True)
                S.add("dve", TS(a[:, 0:8], a[:, 0:8], hmask_t[:, mi:mi + 1], ALU.mult), reads=[ar, cR], writes=[ar])
                S.add("dve", TS(a[:, 8 + n:L], a[:, 8 + n:L], hmask_t[:, mi + 1:mi + 2], ALU.mult), reads=[ar, cR], writes=[ar])
            w1, w1r = wa.next()
            w2, w2r = wb.next()
            S.add("dve", TT(w1[:, 1:L], a[:, 0:L - 1], a[:, 1:L], ALU.add), reads=[ar], writes=[w1r])
            cur, curr = w1, w1r
            if g >= 1:
                S.add("dve", TT(w2[:, 2:L - 1], w1[:, 1:L - 2], w1[:, 3:L], ALU.add), reads=[w1r], writes=[w2r])
                cur, curr = w2, w2r
            if g >= 2:
                S.add("dve", TT(w1[:, 4:L - 3], w2[:, 2:L - 5], w2[:, 6:L - 1], ALU.add), reads=[w2r], writes=[w1r])
                cur, curr = w1, w1r
            if g >= 3:
                S.add("dve", TT(w2[:, 8:L - 7], w1[:, 4:L - 11], w1[:, 12:L - 3], ALU.add), reads=[w1r], writes=[w2r])
                cur, curr = w2, w2r
            wsz = float(2 ** (g + 1))
            S.add("dve", STT(P[:, c, :], cur[:, 8:8 + n], 1.0 / wsz, a[:, 8:8 + n], ALU.mult, ALU.subtract), reads=[curr, ar], writes=[Pr[c]])
            e_, er = ed.next()
            for (lo, eo) in ((0, 0), (n - 8, 8)):
                S.add("dve", TT(e_[:, eo:eo + 8], cur[:, 8 + lo:16 + lo], pe_t[:, g, eo:eo + 8], ALU.mult), reads=[curr, pwr], writes=[er])
                S.add("dve", TT(P[:, c, lo:lo + 8], e_[:, eo:eo + 8], a[:, 8 + lo:16 + lo], ALU.subtract), reads=[er, ar, Pr[c]], writes=[Pr[c]])
        st = Rot([A.alloc([n], BF16) for _ in range(2)])
        bk = 0
        for g in range(4):
            for dd in range(2):
                s, sr = st.next()
                for (o, nb) in blks:
                    bank = bk % 4
                    bk += 1
                    for cc in range(2):
                        S.add("pe", MM(PS[bank][:, 0:nb], pw[:, g, cc, dd * 128:(dd + 1) * 128], P[:, 2 * g + cc, o:o + nb], cc == 0, cc == 1),
                              reads=[pwr, Pr[2 * g + cc]], writes=[PSR[bank]])
                    S.add("act", ACT(s[:, o:o + nb], PS[bank][:, 0:nb], AF.Copy, scale=psc[:, 2 * g + dd:2 * g + dd + 1]), reads=[PSR[bank], pwr], writes=[sr])
                S.add("sp", DMA(ao_s[(2 * g + dd) * 128:(2 * g + dd + 1) * 128, 0:n], s), reads=[sr], dma=True)

    def stage_sgu(l, tile):
        n = tiles[tile]["n"]
        ntt = n // 128
        stage()
        U = A.alloc([8, n], BF16)
        Ur = Reg()
        S.add("sp", DMA(U, u_s[:, 0:n].rearrange("(g p) t -> p g t", p=128)), writes=[Ur], dma=True)
        wt = A.alloc([8, 128], BF16)
        S.add("pool", DMA(wt, sgu_wT[l].rearrange("g q p -> q g p")), writes=[Ur], dma=True)
        bt = A.alloc([8, 128], F32)
        S.add("sp", DMA(bt, sgu_b[l].partition_broadcast(128).rearrange("p (g q) -> p g q", g=8)), writes=[Ur], dma=True)
        vn = Rot([A.alloc([1024], BF16) for _ in range(2)])
        tp = Rot([A.alloc([4, 128], F32) for _ in range(2)])
        BO = A.alloc([8, n], BF16)
        BOr = Reg()
        for tt in range(ntt):
            v, vr = vn.next()
            S.add("sp", DMA(v, vn_s[tt * 128:(tt + 1) * 128, :]), writes=[vr], dma=True)
            for gh in range(2):
                bank = (2 * tt + gh) % 4
                for gg in range(4):
                    g = gh * 4 + gg
                    S.add("pe", MM(PS[bank][:, gg * 128:(gg + 1) * 128], v[:, g * 128:(g + 1) * 128], wt[:, g, :]), reads=[vr, Ur], writes=[PSR[bank]])
                t, tr = tp.next()
                S.add("dve", TT(t, PS[bank].rearrange("p (a b) -> p a b", a=4), bt[:, gh * 4:(gh + 1) * 4, :], ALU.add), reads=[PSR[bank], Ur], writes=[tr])
                S.add("dve", TT(BO[:, gh * 4:(gh + 1) * 4, tt * 128:(tt + 1) * 128], t, U[:, gh * 4:(gh + 1) * 4, tt * 128:(tt + 1) * 128], ALU.mult),
                      reads=[tr, Ur], writes=[BOr])
        S.add("sp", DMA(bo_s[:, 0:n].rearrange("(g p) t -> p g t", p=128), BO), reads=[BOr], dma=True)

    def stage_att(l, tile):
        ti = tiles[tile]
        n = ti["n"]
        blks = blocks(n)
        if tile == "ctx":
            k0, nk = 2 * T, CTX
        else:
            k0, nk = 0, NK
        nkc = nk // 128
        stage()
        qb = Rot([A.alloc([n], BF16) for _ in range(2)])
        kb = Rot([A.alloc([nk], BF16) for _ in range(2)])
        vb = Rot([A.alloc([nkc, 128], BF16) for _ in range(2)])
        e2 = Rot([A.alloc([2, 512], BF16) for _ in range(3)])
        rc = A.alloc([2, 512], F32)
        oc = A.alloc([2, 512], F32)
        o1 = A.alloc([512], F32)
        osq = A.alloc([512], F32)
        rs = A.alloc([512], F32)
        fr = Reg()
        cst = Rot([A.alloc([n], BF16) for _ in range(2)])
        items = [(h, o, nb) for h in range(NH) for (o, nb) in blks]
        loaded = {}

        def pair(p, nb):
            return psall[:, 2 * p * 512:(2 * p + 2) * 512].rearrange("p (a b) -> p a b", a=2)[:, :, 0:nb]

        def load_head(h):
            q, qr = qb.next()
            k, kr = kb.next()
            v, vr = vb.next()
            S.add("sp", DMA(q, q_s[h * 128:(h + 1) * 128, 0:n]), writes=[qr], dma=True)
            S.add("sp", DMA(k, KT[l][h, :, k0:k0 + nk]), writes=[kr], dma=True)
            S.add("sp", DMA(v, VV[l][k0:k0 + nk, h * 128:(h + 1) * 128].rearrange("(c p) d -> p c d", p=128)), writes=[vr], dma=True)
            loaded[h] = (q, qr, k, kr, v, vr)

        def emit_S(h, o, nb, kc):
            q, qr, k, kr, v, vr = loaded[h]
            for i in range(2):
                bank = 2 * (kc % 2) + i
                S.add("pe", MM(PS[bank][:, 0:nb], k[i * 64:(i + 1) * 64, kc * 128:(kc + 1) * 128], q[i * 64:(i + 1) * 64, o:o + nb]),
                      reads=[kr, qr], writes=[PSR[bank]])

        load_head(0)
        cs_, csr = None, None
        for idx, (h, o, nb) in enumerate(items):
            if o == 0:
                if h + 1 < NH:
                    load_head(h + 1)
                cs_, csr = cst.next()
            q, qr, k, kr, v, vr = loaded[h]
            if idx == 0:
                emit_S(h, o, nb, 0)
            for kc in range(nkc):
                if kc + 1 < nkc:
                    emit_S(h, o, nb, kc + 1)
                p = kc % 2
                e_, er = e2.next()
                S.add("act", ACT(e_[:, :, 0:nb], pair(p, nb), AF.Exp, scale=0.125), reads=[PSR[2 * p], PSR[2 * p + 1]], writes=[er])
                for i in range(2):
                    S.add("pe", MM(PS[4 + i][:, 0:nb], v[:, kc, :], e_[:, i, 0:nb], kc == 0, kc == nkc - 1), reads=[vr, er], writes=[PSR[4 + i]])
                    S.add("pe", MM(PS[6 + i][:, 0:nb], onesbf, e_[:, i, 0:nb], kc == 0, kc == nkc - 1), reads=[cR, er], writes=[PSR[6 + i]])
            if idx + 1 < len(items):
                emit_S(items[idx + 1][0], items[idx + 1][1], items[idx + 1][2], 0)
            S.add("act", ACT(rc[:, :, 0:nb], pair(3, nb), AF.Ln), reads=[PSR[6], PSR[7]], writes=[fr])
            S.add("act", ACT(rc[:, :, 0:nb], rc[:, :, 0:nb], AF.Exp, scale=-1.0), reads=[fr], writes=[fr])
            S.add("dve", TT(oc[:, :, 0:nb], pair(2, nb), rc[:, :, 0:nb], ALU.mult), reads=[PSR[4], PSR[5], fr], writes=[fr])
            S.add("dve", STT(o1[:, 0:nb], oc[:, 1, 0:nb], lam_t[:, l, 0:1], oc[:, 0, 0:nb], ALU.mult, ALU.add), reads=[fr, cR], writes=[fr])
            S.add("dve", TT(osq[:, 0:nb], o1[:, 0:nb], o1[:, 0:nb], ALU.mult), reads=[fr], writes=[fr])
            S.add("pe", MM(PS[6][:, 0:nb], ones32, osq[:, 0:nb]), reads=[fr, cR], writes=[PSR[6]])
            S.add("act", ACT(rs[:, 0:nb], PS[6][:, 0:nb], AF.Ln, bias=EPS_AP, scale=1.0 / 128), reads=[PSR[6], cR], writes=[fr])
            S.add("act", ACT(rs[:, 0:nb], rs[:, 0:nb], AF.Exp, scale=-0.5), reads=[fr], writes=[fr])
            S.add("dve", STT(cs_[:, o:o + nb], o1[:, 0:nb], subg_t[:, l:l + 1], rs[:, 0:nb], ALU.mult, ALU.mult), reads=[fr, cR], writes=[csr, fr])
            if o + nb >= n:
                S.add("sp", DMA(co_s[h * 128:(h + 1) * 128, 0:n], cs_), reads=[csr], dma=True)

    def stage_merge(l, tile):
        ti = tiles[tile]
        n = ti["n"]
        mc = ti["mc"]
        blks = blocks(n)
        stage()
        BR = []
        Rr = Reg()
        for src in (ao_s, bo_s, co_s):
            b = A.alloc([8, n], BF16)
            S.add("sp", DMA(b, src[:, 0:n].rearrange("(g p) t -> p g t", p=128)), writes=[Rr], dma=True)
            BR.append(b)
        M = A.alloc([KC, n], BF16)
        Mr = [Reg() for _ in range(KC)]
        wp = Rot([A.alloc([3, 8, 128], BF16) for _ in range(3)])
        gb = Rot([A.alloc([3, n], BF16) for _ in range(3)])
        tm = Rot([A.alloc([512], F32) for _ in range(2)])
        tm2 = Rot([A.alloc([512], F32) for _ in range(2)])
        bk = 0
        for j in range(KC):
            w, wr = wp.next()
            for xi, wsrc_ in enumerate((w_pa, w_pb, w_pc)):
                S.add("pool", DMA(w[:, xi], wsrc_[l].rearrange("(kc p) d -> p kc d", p=128)[:, :, j * 128:(j + 1) * 128]), writes=[wr], dma=True)
            g, gr = gb.next()
            S.add("sp", DMA(g, g_s[:, 0:n].rearrange("(x j p) t -> j p x t", x=3, p=128)[j]), writes=[gr], dma=True)
            for (o, nb) in blks:
                t, tr = tm.next()
                t2, t2r = tm2.next()
                for xi in range(3):
                    bank = bk % 6
                    bk += 1
                    for kc in range(8):
                        S.add("pe", MM(PS[bank][:, 0:nb], w[:, xi, kc, :], BR[xi][:, kc, o:o + nb], kc == 0, kc == 7), reads=[wr, Rr], writes=[PSR[bank]])
                    if xi == 0:
                        S.add("dve", TT(t[:, 0:nb], PS[bank][:, 0:nb], g[:, 0, o:o + nb], ALU.mult), reads=[PSR[bank], gr], writes=[tr])
                    elif xi == 1:
                        S.add("dve", TT(t2[:, 0:nb], PS[bank][:, 0:nb], g[:, 1, o:o + nb], ALU.mult), reads=[PSR[bank], gr], writes=[t2r])
                        S.add("dve", TT(t[:, 0:nb], t[:, 0:nb], t2[:, 0:nb], ALU.add), reads=[tr, t2r], writes=[tr])
                    else:
                        S.add("dve", TT(t2[:, 0:nb], PS[bank][:, 0:nb], g[:, 2, o:o + nb], ALU.mult), reads=[PSR[bank], gr, tr], writes=[t2r])
                        S.add("dve", TT(M[:, j, o:o + nb], t[:, 0:nb], t2[:, 0:nb], ALU.add), reads=[tr, t2r], writes=[Mr[j]])
        ws = Rot([A.alloc([KC, 512], BF16) for _ in range(2)])
        xr = Rot([A.alloc([n], F32) for _ in range(3)])
        gt = modv[:, l, mc, 2, :]
        src = xcur[tile]
        for bi in range(4):
            w, wr = ws.next()
            S.add("pool", DMA(w, wsrc(w_o[l], bi * 512, (bi + 1) * 512)), writes=[wr], dma=True)
            for c in range(4):
                jj = bi * 4 + c
                x, xreg = xr.next()
                S.add("sp", DMA(x, src[jj * 128:(jj + 1) * 128, :]), writes=[xreg], dma=True)
                for (o, nb) in blks:
                    bank = bk % 6
                    bk += 1
                    for kc in range(KC):
                        S.add("pe", MM(PS[bank][:, 0:nb], w[:, kc, c * 128:(c + 1) * 128], M[:, kc, o:o + nb], kc == 0, kc == KC - 1),
                              reads=[wr, Mr[kc]], writes=[PSR[bank]])
                    S.add("dve", STT(x[:, o:o + nb], PS[bank][:, 0:nb], gt[:, jj:jj + 1], x[:, o:o + nb], ALU.mult, ALU.add), reads=[PSR[bank], xreg, cR], writes=[xreg])
                S.add("sp", DMA(xs[tile][jj * 128:(jj + 1) * 128, :], x), reads=[xreg], dma=True)
        xcur[tile] = xs[tile]

    def stage_ffn(l, tile, moe, final, store_x=True):
        ti = tiles[tile]
        n = ti["n"]
        mc = ti["mc"]
        blks = blocks(n)
        stage()
        X = A.alloc([KC, n], F32)
        Xr = [[Reg() for _ in blks] for _ in range(KC)]
        for kc in range(KC):
            S.add("sp", DMA(X[:, kc, :], xcur[tile][kc * 128:(kc + 1) * 128, :]), writes=Xr[kc], dma=True)
        H2 = A.alloc([KC, n], BF16)
        Hr = Reg()
        S.add("sp", DMA(H2, h2_s[:, 0:n].rearrange("(kc p) t -> p kc t", p=128)), writes=[Hr], dma=True)
        FG = 2
        w1b = Rot([A.alloc([KC, FG * 128], BF16) for _ in range(2)])
        w3b = Rot([A.alloc([KC, FG * 128], BF16) for _ in range(2)])
        w2b = Rot([A.alloc([FG, D], BF16) for _ in range(3)])
        Gb = Rot([A.alloc([FG, n], BF16) for _ in range(2)])
        sb = Rot([A.alloc([512], F32) for _ in range(2)])
        cbuf = Rot([A.alloc([n], F32) for _ in range(2)]) if moe else None
        gt = modv[:, l, mc, 5, :]
        F = DFFE if moe else DFF
        nfc = F // 128
        assert nfc % FG == 0
        bk = [0]
        groups = [(e_, fg) for e_ in range(NE if moe else 1) for fg in range(nfc // FG)]
        wl = {}
        cbs = {}

        def load_w(gi):
            e_, fg = groups[gi]
            W1 = moe_w1[e_] if moe else ffn_w1[0]
            W3 = moe_w3[e_] if moe else ffn_w3[0]
            W2 = moe_w2[e_] if moe else ffn_w2[0]
            f0 = fg * FG * 128
            w1, w1r = w1b.next()
            w3, w3r = w3b.next()
            w2, w2r = w2b.next()
            S.add("pool", DMA(w1, wsrc(W1, f0, f0 + FG * 128)), writes=[w1r], dma=True)
            S.add("pool", DMA(w3, wsrc(W3, f0, f0 + FG * 128)), writes=[w3r], dma=True)
            S.add("pool", DMA(w2, W2[f0:f0 + FG * 128, :].rearrange("(fc p) d -> p fc d", p=128)), writes=[w2r], dma=True)
            wl[gi] = (w1, w1r, w3, w3r, w2, w2r)
            if moe and fg == 0:
                cb, cbr = cbuf.next()
                S.add("sp", DMA(cb, comb_s[e_, :, 0:n]), writes=[cbr], dma=True)
                cbs[e_] = (cb, cbr)

        Gs = {}

        def up_parts(gi):
            e_, fg = groups[gi]
            w1, w1r, w3, w3r, w2, w2r = wl[gi]
            G, Gr = Gb.next()
            Gs[gi] = (G, Gr)
            parts = []
            for fc in range(FG):
                for (o, nb) in blks:
                    def part(fc=fc, o=o, nb=nb):
                        b1 = bk[0] % 4
                        b3 = (bk[0] + 1) % 4
                        bk[0] += 2
                        for kc in range(KC):
                            S.add("pe", MM(PS[b1][:, 0:nb], w1[:, kc, fc * 128:(fc + 1) * 128], H2[:, kc, o:o + nb], kc == 0, kc == KC - 1),
                                  reads=[w1r, Hr], writes=[PSR[b1]])
                        for kc in range(KC):
                            S.add("pe", MM(PS[b3][:, 0:nb], w3[:, kc, fc * 128:(fc + 1) * 128], H2[:, kc, o:o + nb], kc == 0, kc == KC - 1),
                                  reads=[w3r, Hr], writes=[PSR[b3]])
                        s_, sr = sb.next()
                        S.add("act", ACT(s_[:, 0:nb], PS[b1][:, 0:nb], AF.Silu), reads=[PSR[b1]], writes=[sr])
                        if moe:
                            cb, cbr = cbs[e_]
                            S.add("dve", TT(s_[:, 0:nb], s_[:, 0:nb], PS[b3][:, 0:nb], ALU.mult), reads=[sr, PSR[b3]], writes=[sr])
                            S.add("dve", TT(G[:, fc, o:o + nb], s_[:, 0:nb], cb[:, o:o + nb], ALU.mult), reads=[sr, cbr], writes=[Gr])
                        else:
                            S.add("dve", TT(G[:, fc, o:o + nb], s_[:, 0:nb], PS[b3][:, 0:nb], ALU.mult), reads=[sr, PSR[b3]], writes=[Gr])
                    parts.append(part)
            return parts

        def down_parts(gi, nparts):
            w1, w1r, w3, w3r, w2, w2r = wl[gi]
            G, Gr = Gs[gi]
            units = [(jj, bi, o, nb) for jj in range(KC) for bi, (o, nb) in enumerate(blks)]
            per = (len(units) + nparts - 1) // nparts
            parts = []
            for k in range(nparts):
                def part(us=units[k * per:(k + 1) * per]):
                    for (jj, bi, o, nb) in us:
                        bank = 4 + (jj * len(blks) + bi) % 4
                        for fc in range(FG):
                            S.add("pe", MM(PS[bank][:, 0:nb], w2[:, fc, jj * 128:(jj + 1) * 128], G[:, fc, o:o + nb], fc == 0, fc == FG - 1),
                                  reads=[w2r, Gr], writes=[PSR[bank]])
                        S.add("dve", STT(X[:, jj, o:o + nb], PS[bank][:, 0:nb], gt[:, jj:jj + 1], X[:, jj, o:o + nb], ALU.mult, ALU.add),
                              reads=[PSR[bank], Xr[jj][bi], cR], writes=[Xr[jj][bi]])
                parts.append(part)
            return parts

        load_w(0)
        for gi in range(len(groups)):
            if gi + 1 < len(groups):
                load_w(gi + 1)
            ups = up_parts(gi)
            downs = down_parts(gi - 1, len(ups)) if gi > 0 else [None] * len(ups)
            for k in range(len(ups)):
                ups[k]()
                if downs[k] is not None:
                    downs[k]()
        for p_ in down_parts(len(groups) - 1, 1):
            p_()
        acc = A.alloc([n], F32)
        accr = [Reg() for _ in blks]
        if moe:
            rstd, creg0 = cbuf.items[0]
            extra = [creg0]
        else:
            rstd = A.alloc([n], F32)
            extra = []
        rr = [Reg() for _ in blks]
        for kc in range(KC):
            for bi, (o, nb) in enumerate(blks):
                if kc == 0:
                    S.add("act", ACT(acc[:, o:o + nb], X[:, kc, o:o + nb], AF.Square), reads=[Xr[kc][bi]], writes=[accr[bi]])
                else:
                    s_, sr = sb.next()
                    S.add("act", ACT(s_[:, 0:nb], X[:, kc, o:o + nb], AF.Square), reads=[Xr[kc][bi]], writes=[sr])
                    S.add("dve", TT(acc[:, o:o + nb], acc[:, o:o + nb], s_[:, 0:nb], ALU.add), reads=[sr, accr[bi]], writes=[accr[bi]])
        for bi, (o, nb) in enumerate(blks):
            S.add("pe", MM(PS[bi][:, 0:nb], ones32, acc[:, o:o + nb]), reads=[accr[bi], cR], writes=[PSR[bi]])
            s_, sr = sb.next()
            S.add("act", ACT(s_[:, 0:nb], PS[bi][:, 0:nb], AF.Sqrt, bias=EPS_AP, scale=1.0 / D), reads=[PSR[bi], cR], writes=[sr])
            S.add("dve", lambda e, a=rstd[:, o:o + nb], b=s_[:, 0:nb]: e.reciprocal(out=a, in_=b), reads=[sr], writes=[rr[bi]] + extra)
        if not final:
            if store_x:
                for kc in range(KC):
                    S.add("sp", DMA(xs[tile][kc * 128:(kc + 1) * 128, :], X[:, kc, :]), reads=Xr[kc], dma=True)
                xcur[tile] = xs[tile]
            Av = modv[:, l + 1, mc, 0, :]
            Bv = modv[:, l + 1, mc, 1, :]
            for kc in range(KC):
                for bi, (o, nb) in enumerate(blks):
                    s_, sr = sb.next()
                    S.add("dve", STT(s_[:, 0:nb], X[:, kc, o:o + nb], Av[:, kc:kc + 1], rstd[:, o:o + nb], ALU.mult, ALU.mult),
                          reads=[Xr[kc][bi], rr[bi], cR], writes=[sr])
                    S.add("act", ACT(H2[:, kc, o:o + nb], s_[:, 0:nb], AF.Identity, bias=Bv[:, kc:kc + 1]), reads=[sr, cR], writes=[Hr])
            S.add("sp", DMA(h_s[tile].rearrange("(kc p) t -> p kc t", p=128), H2), reads=[Hr], dma=True)
            return
        for kc in range(KC):
            for bi, (o, nb) in enumerate(blks):
                S.add("dve", STT(X[:, kc, o:o + nb], X[:, kc, o:o + nb], fng_t[:, kc:kc + 1], rstd[:, o:o + nb], ALU.mult, ALU.mult),
                      reads=[Xr[kc][bi], rr[bi], cR], writes=[Xr[kc][bi]])
            S.add("sp", DMA(outT[kc * 128:(kc + 1) * 128, :], X[:, kc, :]), reads=Xr[kc], dma=True)

    A.base = A.off
    eps_t = A.alloc([1], F32)
    S.add("dve", lambda e: e.memset(eps_t, EPS), writes=[cR])
    EPS_AP = eps_t
    A.base = A.off

    upto = cfg.get("upto", 99)
    stage_mod()
    step = [0]

    def go(fn, *a, **k):
        step[0] += 1
        if step[0] <= upto:
            fn(*a, **k)

    for t in ("oth", "ctx", "own"):
        go(stage_norm, 0, t, 0, h_s[t])
        go(stage_win, 0, t, ["K", "V", "A"])
    for t in ("oth", "ctx", "own"):
        go(stage_win, 0, t, ["U", "VS", "Q", "G"])
        go(stage_pool, 0, t)
        go(stage_sgu, 0, t)
        go(stage_att, 0, t)
        go(stage_merge, 0, t)
        go(stage_norm, 0, t, 1, h2_s[:, 0:tiles[t]["n"]])
        go(stage_ffn, 0, t, False, False, store_x=(t == "own"))
        go(stage_win, 1, t, ["K", "V", "A"] if t != "ctx" else ["K", "V"])
    t = "own"
    go(stage_win, 1, t, ["U", "VS", "Q", "G"])
    go(stage_pool, 1, t)
    go(stage_sgu, 1, t)
    go(stage_att, 1, t)
    go(stage_merge, 1, t)
    go(stage_norm, 1, t, 1, h2_s[:, 0:T], moe=True)
    go(stage_ffn, 1, t, True, True)
    S.barrier()

    S.finalize()
    with nc.Block() as block:
        @block.tensor
        def _(e):
            S.emit("pe", e, csem, dsem)

        @block.scalar
        def _(e):
            S.emit("act", e, csem, dsem)

        @block.vector
        def _(e):
            S.emit("dve", e, csem, dsem)

        @block.gpsimd
        def _(e):
            S.emit("pool", e, csem, dsem)

        @block.sync
        def _(e):
            S.emit("sp", e, csem, dsem)
    for cm in reversed(ctxs):
        cm.__exit__(None, None, None)
    return nc


def _fm(v):
    v = np.asarray(v)
    n = v.shape[-1] // 128
    return np.ascontiguousarray(np.swapaxes(v.reshape(v.shape[:-1] + (n, 128)), -1, -2))


def _consts(cfg, s):
    SEQ, CTX = cfg["SEQ"], cfg["CTX"]
    T = SEQ // 2
    half = 32
    inv = np.power(10000.0, -np.arange(0, half, 2, dtype=np.float32) / half).astype(np.float32)
    out = {}
    for name, t0 in (("rope_own", s * T), ("rope_oth", (1 - s) * T)):
        t = np.arange(t0, t0 + T)
        row = (t // 64).astype(np.float32)
        col = (t % 64).astype(np.float32)
        ar = row[:, None] * inv[None, :]
        ac = col[:, None] * inv[None, :]
        ang = np.concatenate([ar, ar, ac, ac], axis=-1).astype(np.float32)
        cs = np.stack([np.cos(ang), np.sin(ang)]).astype(np.float32)
        cs = np.transpose(cs, (0, 2, 1))
        out[name] = np.ascontiguousarray(np.concatenate([cs, cs], axis=1))
    cm = np.zeros((3, 128, 128), np.float32)
    cm[0] = 1.0
    cm[1] = np.eye(128, dtype=np.float32)
    for m in range(128):
        if m % 32 < 16:
            cm[2, m + 16, m] = -1.0
        else:
            cm[2, m - 16, m] = 1.0
    out["cmat"] = cm
    hm = np.zeros((128, 4), np.float32)
    hm[:, 0], hm[:, 1] = float(s == 1), float(s == 0)
    hm[:, 2], hm[:, 3] = float(s == 0), float(s == 1)
    out["hmask"] = hm

    def edges(t0, n, Sq):
        e = np.zeros((128, 4, 16), np.float32)
        for g, w in enumerate((2, 4, 8, 16)):
            for idx, t in enumerate(list(range(t0, t0 + 8)) + list(range(t0 + n - 8, t0 + n))):
                lo = min(max(t - w // 2, 0), Sq)
                hi = min(max(t + w - w // 2, 0), Sq)
                e[:, g, idx] = 1.0 / float(hi - lo)
        return e
    out["pedge_own"] = edges(s * T, T, SEQ)
    out["pedge_oth"] = edges((1 - s) * T, T, SEQ)
    out["pedge_ctx"] = edges(0, CTX, CTX)
    return out


def prep_inputs(inp, cfg):
    SEQ = cfg["SEQ"]
    T = SEQ // 2
    f32 = lambda a: np.ascontiguousarray(np.asarray(a, dtype=np.float32))
    shared = dict(
        w_mod=f32(inp["w_mod"]), b_mod=_fm(f32(inp["b_mod"])), n1g=_fm(f32(inp["norm1_g"])), n2g=_fm(f32(inp["norm2_g"])),
        fng=_fm(f32(inp["final_norm_g"])), w_in=f32(inp["w_in"]), pool_w=f32(inp["pool_w"]), pool_sc=_fm(f32(inp["pool_scale"])),
        sgu_ng=f32(inp["sgu_norm_g"]), sgu_wT=np.ascontiguousarray(np.swapaxes(f32(inp["sgu_w"]), -1, -2)),
        sgu_b=f32(inp["sgu_b"]).reshape(2, 1024),
        lamv=np.ascontiguousarray(np.stack([f32(inp["lambda_q1"]), f32(inp["lambda_k1"]), f32(inp["lambda_q2"]), f32(inp["lambda_k2"])], axis=1)),
        subln=f32(inp["attn_subln_g"]).reshape(2, 128, 1),
        w_pa=f32(inp["w_proj_a"]), w_pb=f32(inp["w_proj_b"]), w_pc=f32(inp["w_proj_c"]), w_o=f32(inp["w_o"]),
        ffn_w1=f32(inp["ffn_w1"]), ffn_w3=f32(inp["ffn_w3"]), ffn_w2=f32(inp["ffn_w2"]),
        router=np.ascontiguousarray(np.transpose(f32(inp["moe_router"])[0].reshape(KC, 128, NE), (1, 0, 2))),
        moe_w1=f32(inp["moe_w1"])[0], moe_w3=f32(inp["moe_w3"])[0], moe_w2=f32(inp["moe_w2"])[0],
    )
    x = f32(inp["x"])
    ctx = f32(inp["ctx"])
    c = f32(inp["c"])
    cc = f32(inp["c_ctx"])
    maps = []
    for core in range(8):
        b, s = core // 2, core % 2
        m = dict(shared)
        m["xT_own"] = np.ascontiguousarray(x[b, s * T:(s + 1) * T, :].T)
        m["xT_oth"] = np.ascontiguousarray(x[b, (1 - s) * T:(2 - s) * T, :].T)
        m["xT_ctx"] = np.ascontiguousarray(ctx[b].T)
        m["cvec"] = np.ascontiguousarray(np.stack([_fm(c[b]), _fm(cc)], axis=-1))
        m.update(_consts(cfg, s))
        maps.append(m)
    return maps


def assemble(results, cfg):
    SEQ = cfg["SEQ"]
    T = SEQ // 2
    out = np.zeros((4, SEQ, D), np.float32)
    for core in range(8):
        b, s = core // 2, core % 2
        out[b, s * T:(s + 1) * T, :] = np.asarray(results[core]["outT"]).T
    return out


def kernel(**inputs):
    cfg = FULL_CFG
    nc = build(cfg)
    maps = prep_inputs(inputs, cfg)
    res = run_bass_kernel_spmd(nc, maps, core_ids=list(range(8)))
    return assemble(res.results, cfg)
```

```python
import numpy as np
import ml_dtypes
import concourse.bass as bass
import concourse.mybir as mybir
from concourse.bass_utils import run_bass_kernel_spmd

F32 = mybir.dt.float32
BF16 = mybir.dt.bfloat16
AF = mybir.ActivationFunctionType
ALU = mybir.AluOpType
AX = mybir.AxisListType

D = 2048
KC = 16
NH = 8
OFF_A, OFF_B, OFF_Q, OFF_K, OFF_V, OFF_G, INW = 0, 1024, 3072, 4096, 5120, 6144, 12288
EPS = 1e-6
NE = 8
FULL_CFG = dict(SEQ=2048, CTX=256, DFF=5632, DFFE=7168)
SAME_ENGINE_SYNC = True
NDS = 6
ARENA32 = 46 * 1024


class Reg:
    __slots__ = ("w", "rs")

    def __init__(self):
        self.w = None
        self.rs = {}


class Op:
    __slots__ = ("eng", "fn", "deps", "dma", "idx", "inc", "cnt", "dslot", "dval")


class Sched:
    ENG = ("pe", "act", "dve", "pool", "sp")

    def __init__(self):
        self.ops = {e: [] for e in self.ENG}
        self.ndma = {e: 0 for e in self.ENG}
        self.lastdma = {}

    def add(self, eng, fn, reads=(), writes=(), dma=False):
        op = Op()
        op.eng, op.fn, op.dma, op.inc, op.cnt = eng, fn, dma, False, 0
        deps = []
        for r in reads:
            if r.w is not None:
                deps.append(r.w)
        for w in writes:
            if w.w is not None:
                deps.append(w.w)
            deps.extend(w.rs.values())
        op.deps = deps
        op.idx = len(self.ops[eng])
        self.ops[eng].append(op)
        if dma:
            n = self.ndma[eng]
            self.ndma[eng] += 1
            op.dslot = n % NDS
            op.dval = 16 * (n // NDS + 1)
            self.lastdma[(eng, op.dslot)] = op
            key = (eng, "d", op.idx)
        else:
            key = eng
        for r in reads:
            r.rs[key] = op
        for w in writes:
            w.w = op
            w.rs = {}
        return op

    def barrier(self):
        lasts = []
        for e in self.ENG:
            for o in reversed(self.ops[e]):
                if o.fn is not None and not o.dma:
                    lasts.append(o)
                    break
        lasts.extend(self.lastdma.values())
        for e in self.ENG:
            op = Op()
            op.eng, op.fn, op.dma, op.inc, op.cnt = e, None, False, False, 0
            op.deps = list(lasts)
            op.idx = len(self.ops[e])
            self.ops[e].append(op)

    def _skip(self, op, d):
        if d.dma or op.dma or op.fn is None:
            return False
        if d.eng != op.eng:
            return False
        return op.eng == "pe" or not SAME_ENGINE_SYNC

    def finalize(self):
        for e in self.ENG:
            for op in self.ops[e]:
                for d in op.deps:
                    if not d.dma and not self._skip(op, d):
                        d.inc = True
        for e in self.ENG:
            c = 0
            for op in self.ops[e]:
                if op.inc:
                    c += 1
                op.cnt = c

    def emit(self, e, eng, csem, dsem):
        waited = {}
        for op in self.ops[e]:
            need = {}
            for d in op.deps:
                if d.dma:
                    key, val = ("d", d.eng, d.dslot), d.dval
                else:
                    if self._skip(op, d):
                        continue
                    key, val = d.eng, d.cnt
                if waited.get(key, 0) < val and need.get(key, 0) < val:
                    need[key] = val
            if op.dma and op.dval > 16:
                key = ("d", e, op.dslot)
                val = op.dval - 16
                if waited.get(key, 0) < val and need.get(key, 0) < val:
                    need[key] = val
            for key, val in need.items():
                sem = dsem[key[1]][key[2]] if isinstance(key, tuple) else csem[key]
                eng.wait_ge(sem, val)
                waited[key] = val
            if op.fn is None:
                continue
            ins = op.fn(eng)
            if op.dma:
                ins.then_inc(dsem[e][op.dslot], 16)
            elif op.inc:
                ins.then_inc(csem[e], 1)
        for (qe, slot), o in self.lastdma.items():
            if qe == e:
                eng.wait_ge(dsem[e][slot], o.dval)


def MM(out, lhsT, rhs, start=True, stop=True):
    return lambda e: e.matmul(out, lhsT=lhsT, rhs=rhs, start=start, stop=stop)


def ACT(out, in_, func, bias=None, scale=None):
    kw = {}
    if bias is not None:
        kw["bias"] = bias
    if scale is not None:
        kw["scale"] = scale
    return lambda e: e.activation(out=out, in_=in_, func=func, **kw)


def TT(out, a, b, op):
    return lambda e: e.tensor_tensor(out=out, in0=a, in1=b, op=op)


def TS(out, a, s1, op0, s2=None, op1=None):
    if op1 is None:
        return lambda e: e.tensor_scalar(out=out, in0=a, scalar1=s1, scalar2=None, op0=op0)
    return lambda e: e.tensor_scalar(out=out, in0=a, scalar1=s1, scalar2=s2, op0=op0, op1=op1)


def STT(out, in0, scalar, in1, op0, op1):
    return lambda e: e.scalar_tensor_tensor(out=out, in0=in0, scalar=scalar, in1=in1, op0=op0, op1=op1)


def CP(out, in_):
    return lambda e: e.tensor_copy(out=out, in_=in_)


def DMA(out, in_):
    return lambda e: e.dma_start(out=out, in_=in_)


def blocks(n):
    return [(o, min(512, n - o)) for o in range(0, n, 512)]


class Rot:
    def __init__(self, items):
        self.items = [(it, Reg()) for it in items]
        self.i = 0

    def next(self):
        it = self.items[self.i % len(self.items)]
        self.i += 1
        return it


def build(cfg, debug_outs=()):
    SEQ, CTX, DFF, DFFE = cfg["SEQ"], cfg["CTX"], cfg["DFF"], cfg["DFFE"]
    T = SEQ // 2
    NK = SEQ + CTX
    nc = bass.Bass("TRN2", target_bir_lowering=False)
    S = Sched()

    def din(name, shape, dt=F32):
        return nc.dram_tensor(name, list(shape), dt, kind="ExternalInput").ap()

    def dscr(name, shape, dt):
        kind = "ExternalOutput" if name in debug_outs else "Internal"
        return nc.dram_tensor(name, list(shape), dt, kind=kind).ap()

    tiles = {
        "own": dict(n=T, koff=0, rope=True, mc=0),
        "oth": dict(n=T, koff=T, rope=True, mc=0),
        "ctx": dict(n=CTX, koff=2 * T, rope=False, mc=1),
    }
    xin = {t: din("xT_" + t, [D, tiles[t]["n"]]) for t in tiles}
    cvec = din("cvec", [128, KC, 2])
    w_mod = din("w_mod", [2, D, 6 * D])
    b_mod = din("b_mod", [2, 128, 96])
    n1g = din("n1g", [2, 128, KC])
    n2g = din("n2g", [2, 128, KC])
    fng = din("fng", [128, KC])
    w_in = din("w_in", [2, D, INW])
    pool_w = din("pool_w", [2, 4, 256, 256])
    pool_sc = din("pool_sc", [2, 128, 8])
    sgu_ng = din("sgu_ng", [2, 1024])
    sgu_wT = din("sgu_wT", [2, 8, 128, 128])
    sgu_b = din("sgu_b", [2, 1024])
    lamv = din("lamv", [2, 4, 64])
    subln = din("subln", [2, 128, 1])
    w_pa = din("w_pa", [2, 1024, D])
    w_pb = din("w_pb", [2, 1024, D])
    w_pc = din("w_pc", [2, 1024, D])
    w_o = din("w_o", [2, D, D])
    ffn_w1 = din("ffn_w1", [1, D, DFF])
    ffn_w3 = din("ffn_w3", [1, D, DFF])
    ffn_w2 = din("ffn_w2", [1, DFF, D])
    router = din("router", [128, KC, NE])
    moe_w1 = din("moe_w1", [NE, D, DFFE])
    moe_w3 = din("moe_w3", [NE, D, DFFE])
    moe_w2 = din("moe_w2", [NE, DFFE, D])
    rope_t = {"own": din("rope_own", [2, 128, T]), "oth": din("rope_oth", [2, 128, T])}
    cmat = din("cmat", [3, 128, 128])
    hmask = din("hmask", [128, 4])
    pedge = {t: din("pedge_" + t, [128, 4, 16]) for t in tiles}
    outT = nc.dram_tensor("outT", [D, T], F32, kind="ExternalOutput").ap()

    h_s = {t: dscr("h_" + t, [D, tiles[t]["n"]], BF16) for t in tiles}
    xs = {t: dscr("xs_" + t, [D, tiles[t]["n"]], F32) for t in tiles}
    a_s = [{t: dscr("a%d_%s" % (l, t), [1024, tiles[t]["n"]], F32) for t in tiles} for l in range(2)]
    KT = [dscr("KT%d" % l, [NH, 128, NK], BF16) for l in range(2)]
    VV = [dscr("VV%d" % l, [NK, 1024], BF16) for l in range(2)]
    u_s = dscr("u_s", [1024, T], BF16)
    vn_s = dscr("vn_s", [T, 1024], BF16)
    q_s = dscr("q_s", [1024, T], BF16)
    g_s = dscr("g_s", [6144, T], BF16)
    ao_s = dscr("ao_s", [1024, T], BF16)
    bo_s = dscr("bo_s", [1024, T], BF16)
    co_s = dscr("co_s", [1024, T], BF16)
    h2_s = dscr("h2_s", [D, T], BF16)
    comb_s = dscr("comb_s", [NE, 128, T], F32)
    xcur = dict(xin)

    ctxs = []

    def enter(cm):
        ctxs.append(cm)
        return cm.__enter__()

    arena_t = enter(nc.sbuf_tensor("arena", [128, ARENA32], F32))
    psall = enter(nc.psum_tensor("psall", [128, 8 * 512], F32))
    PS = [psall[:, i * 512:(i + 1) * 512] for i in range(8)]
    PSR = [Reg() for _ in range(8)]
    csem = {e: enter(nc.semaphore("c_" + e)) for e in Sched.ENG}
    dsem = {e: [enter(nc.semaphore("d_%s%d" % (e, i))) for i in range(NDS)] for e in ("sp", "pool")}

    class Arena:
        def __init__(self):
            self.off = 0
            self.base = 0

        def alloc(self, shape, dt):
            ne = int(np.prod(shape))
            n32 = ne if dt == F32 else (ne + 1) // 2
            n32 = (n32 + 7) // 8 * 8
            a = arena_t[:, self.off:self.off + n32]
            self.off += n32
            assert self.off <= ARENA32, ("arena overflow", self.off)
            if dt != F32:
                a = a.bitcast(dt)
            a = a[:, 0:ne]
            if len(shape) == 2:
                a = a.rearrange("p (a b) -> p a b", a=shape[0])
            elif len(shape) == 3:
                a = a.rearrange("p (a b c) -> p a b c", a=shape[0], b=shape[1])
            elif len(shape) == 4:
                a = a.rearrange("p (a b c d) -> p a b c d", a=shape[0], b=shape[1], c=shape[2])
            return a

        def reset(self):
            self.off = self.base

    A = Arena()

    def stage():
        S.barrier()
        A.reset()

    ones32 = A.alloc([128], F32)
    ident32 = A.alloc([128], F32)
    rmat32 = A.alloc([128], F32)
    onesbf = A.alloc([128], BF16)
    hmask_t = A.alloc([4], F32)
    modv = A.alloc([2, 2, 6, KC], F32)
    lam_t = A.alloc([2, 2], F32)
    subg_t = A.alloc([2], F32)
    fng_t = A.alloc([KC], F32)
    cR = Reg()
    S.add("sp", DMA(ones32, cmat[0]), writes=[cR], dma=True)
    S.add("sp", DMA(ident32, cmat[1]), writes=[cR], dma=True)
    S.add("sp", DMA(rmat32, cmat[2]), writes=[cR], dma=True)
    S.add("sp", DMA(hmask_t, hmask), writes=[cR], dma=True)
    S.add("sp", DMA(fng_t, fng), writes=[cR], dma=True)
    S.add("dve", CP(onesbf, ones32), reads=[cR], writes=[cR])
    A.base = A.off

    def wsrc(w2d, c0, c1):
        return w2d.rearrange("(kc p) n -> p kc n", p=128)[:, :, c0:c1]

    def stage_mod():
        stage()
        cv = A.alloc([KC, 2], F32)
        sc = A.alloc([KC, 2], BF16)
        r0 = Reg()
        S.add("sp", DMA(cv, cvec), writes=[r0], dma=True)
        S.add("act", ACT(sc, cv, AF.Silu), reads=[r0], writes=[r0])
        ws = Rot([A.alloc([KC, 512], BF16) for _ in range(2)])
        lam_init = [0.8 - 0.6 * float(np.exp(-0.3 * l)) for l in range(2)]
        for l in range(2):
            bm = A.alloc([96], F32)
            mraw = A.alloc([96, 2], F32)
            g1 = A.alloc([KC], F32)
            g2 = A.alloc([KC], F32)
            rb = Reg()
            S.add("sp", DMA(bm, b_mod[l]), writes=[rb], dma=True)
            S.add("sp", DMA(g1, n1g[l]), writes=[rb], dma=True)
            S.add("sp", DMA(g2, n2g[l]), writes=[rb], dma=True)
            for bi in range(24):
                w, wr = ws.next()
                S.add("pool", DMA(w, wsrc(w_mod[l], bi * 512, (bi + 1) * 512)), writes=[wr], dma=True)
                bank = bi % 2
                for c in range(4):
                    j = bi * 4 + c
                    for kc in range(KC):
                        S.add("pe", MM(PS[bank][:, 2 * c:2 * c + 2], w[:, kc, c * 128:(c + 1) * 128], sc[:, kc, :],
                                       kc == 0, kc == KC - 1), reads=[wr, r0], writes=[PSR[bank]])
                S.add("dve", CP(mraw[:, bi * 4:(bi + 1) * 4, :], PS[bank][:, 0:8].rearrange("p (a b) -> p a b", a=4)),
                      reads=[PSR[bank]], writes=[rb])
            for col in range(2):
                S.add("dve", TT(mraw[:, :, col], mraw[:, :, col], bm, ALU.add), reads=[rb], writes=[rb])
            for col in range(2):
                mv = modv[:, l, col]
                for half, g in ((0, g1), (1, g2)):
                    sh = mraw[:, (3 * half + 0) * KC:(3 * half + 1) * KC, col]
                    scl = mraw[:, (3 * half + 1) * KC:(3 * half + 2) * KC, col]
                    gt = mraw[:, (3 * half + 2) * KC:(3 * half + 3) * KC, col]
                    S.add("dve", STT(mv[:, 3 * half + 0, :], scl, 1.0, g, ALU.add, ALU.mult), reads=[rb], writes=[rb])
                    S.add("dve", CP(mv[:, 3 * half + 1, :], sh), reads=[rb], writes=[rb])
                    S.add("dve", CP(mv[:, 3 * half + 2, :], gt), reads=[rb], writes=[rb])
            lv = A.alloc([4, 64], F32)
            lp = A.alloc([2, 64], F32)
            ls = A.alloc([2], F32)
            sg = A.alloc([1], F32)
            S.add("sp", DMA(lv, lamv[l].partition_broadcast(128)), writes=[rb], dma=True)
            S.add("sp", DMA(sg, subln[l]), writes=[rb], dma=True)
            S.add("dve", TT(lp[:, 0, :], lv[:, 0, :], lv[:, 1, :], ALU.mult), reads=[rb], writes=[rb])
            S.add("dve", TT(lp[:, 1, :], lv[:, 2, :], lv[:, 3, :], ALU.mult), reads=[rb], writes=[rb])
            S.add("dve", lambda e, lp=lp, ls=ls: e.tensor_reduce(out=ls, in_=lp, axis=AX.X, op=ALU.add), reads=[rb], writes=[rb])
            S.add("act", ACT(ls, ls, AF.Exp), reads=[rb], writes=[rb])
            S.add("dve", STT(lam_t[:, l, 0:1], ls[:, 1:2], -lam_init[l], ls[:, 0:1], ALU.add, ALU.subtract), reads=[rb], writes=[rb])
            S.add("dve", TS(subg_t[:, l:l + 1], sg, 1.0 - lam_init[l], ALU.mult), reads=[rb], writes=[rb])

    def stage_norm(l, tile, which, dst, moe=False):
        ti = tiles[tile]
        n = ti["n"]
        mc = ti["mc"]
        src = xcur[tile]
        stage()
        blks = blocks(n)
        X = A.alloc([KC, n], F32)
        Xr = [Reg() for _ in range(KC)]
        for kc in range(KC):
            S.add("sp", DMA(X[:, kc, :], src[kc * 128:(kc + 1) * 128, :]), writes=[Xr[kc]], dma=True)
        sq = Rot([A.alloc([n], F32) for _ in range(2)])
        acc = A.alloc([n], F32)
        accr = Reg()
        rstd = A.alloc([n], F32)
        rstd_r = [Reg() for _ in blks]
        tmp = Rot([A.alloc([n], F32) for _ in range(2)])
        hf = Rot([A.alloc([n], F32) for _ in range(2)])
        hst = Rot([A.alloc([n], BF16) for _ in range(3)])
        Av = modv[:, l, mc, 3 * which + 0, :]
        Bv = modv[:, l, mc, 3 * which + 1, :]
        for kc in range(KC):
            if kc == 0:
                S.add("act", ACT(acc, X[:, kc, :], AF.Square), reads=[Xr[kc]], writes=[accr])
            else:
                s_, sr = sq.next()
                S.add("act", ACT(s_, X[:, kc, :], AF.Square), reads=[Xr[kc]], writes=[sr])
                S.add("dve", TT(acc, acc, s_, ALU.add), reads=[sr, accr], writes=[accr])
        for bi, (o, nb) in enumerate(blks):
            S.add("pe", MM(PS[bi][:, 0:nb], ones32, acc[:, o:o + nb]), reads=[accr, cR], writes=[PSR[bi]])
            s_, sr = sq.next()
            S.add("act", ACT(s_[:, 0:nb], PS[bi][:, 0:nb], AF.Sqrt, bias=EPS_AP, scale=1.0 / D), reads=[PSR[bi], cR], writes=[sr])
            S.add("dve", lambda e, a=rstd[:, o:o + nb], b=s_[:, 0:nb]: e.reciprocal(out=a, in_=b), reads=[sr], writes=[rstd_r[bi]])
        if moe:
            wr_t = A.alloc([KC, NE], F32)
            wrr = Reg()
            S.add("sp", DMA(wr_t, router), writes=[wrr], dma=True)
        ntt = n // 128
        for kc in range(KC):
            x, xreg = X[:, kc, :], Xr[kc]
            t, tr = tmp.next()
            for bi, (o, nb) in enumerate(blks):
                S.add("dve", STT(t[:, o:o + nb], x[:, o:o + nb], Av[:, kc:kc + 1], rstd[:, o:o + nb], ALU.mult, ALU.mult),
                      reads=[xreg, rstd_r[bi]], writes=[tr])
            hs, hr = hst.next()
            if not moe:
                S.add("act", ACT(hs, t, AF.Identity, bias=Bv[:, kc:kc + 1]), reads=[tr], writes=[hr])
            else:
                f, fr = hf.next()
                S.add("act", ACT(f, t, AF.Identity, bias=Bv[:, kc:kc + 1]), reads=[tr], writes=[fr])
                S.add("dve", CP(hs, f), reads=[fr], writes=[hr])
                for tt in range(ntt):
                    S.add("pe", MM(PS[tt][:, 0:NE], f[:, tt * 128:(tt + 1) * 128], wr_t[:, kc, :], kc == 0, kc == KC - 1),
                          reads=[fr, wrr], writes=[PSR[tt]])
            S.add("sp", DMA(dst[kc * 128:(kc + 1) * 128, :], hs), reads=[hr], dma=True)
        if moe:
            comb = A.alloc([ntt, NE], F32)
            cr = [Reg() for _ in range(ntt)]
            sm = A.alloc([ntt, 24], F32)
            for tt in range(ntt):
                lg = sm[:, tt, 0:8]
                m8 = sm[:, tt, 8:16]
                ex = sm[:, tt, 16:24]
                r = cr[tt]
                S.add("dve", CP(lg, PS[tt][:, 0:NE]), reads=[PSR[tt]], writes=[r])
                S.add("dve", lambda e, a=m8, b=lg: e.max(out=a, in_=b), reads=[r], writes=[r])
                S.add("dve", TS(comb[:, tt, 0:1], m8[:, 0:1], -1.0, ALU.mult), reads=[r], writes=[r])
                S.add("act", ACT(ex, lg, AF.Exp, bias=comb[:, tt, 0:1]), reads=[r], writes=[r])
                S.add("dve", TS(lg, lg, m8[:, 1:2], ALU.is_ge), reads=[r], writes=[r])
                S.add("dve", TT(ex, ex, lg, ALU.mult), reads=[r], writes=[r])
                S.add("dve", lambda e, a=m8[:, 2:3], b=ex: e.tensor_reduce(out=a, in_=b, axis=AX.X, op=ALU.add), reads=[r], writes=[r])
                S.add("dve", lambda e, a=m8[:, 3:4], b=m8[:, 2:3]: e.reciprocal(out=a, in_=b), reads=[r], writes=[r])
                S.add("dve", TS(comb[:, tt, :], ex, m8[:, 3:4], ALU.mult), reads=[r], writes=[r])
            cb = Rot([A.alloc([128], F32) for _ in range(3)])
            cst = Rot([A.alloc([n], F32) for _ in range(2)])
            for e_ in range(NE):
                for tt in range(ntt):
                    c, creg = cb.next()
                    S.add("dve", CP(c, comb[:, tt, e_:e_ + 1].to_broadcast([128, 128])), reads=[cr[tt]], writes=[creg])
                    bank = 4 + 2 * (e_ % 2) + tt // 4
                    S.add("pe", MM(PS[bank][:, (tt % 4) * 128:(tt % 4 + 1) * 128], c, ident32), reads=[creg, cR], writes=[PSR[bank]])
                st, sr = cst.next()
                for bi, (o, nb) in enumerate(blks):
                    bank = 4 + 2 * (e_ % 2) + bi
                    S.add("act", ACT(st[:, o:o + nb], PS[bank][:, 0:nb], AF.Copy), reads=[PSR[bank]], writes=[sr])
                S.add("sp", DMA(comb_s[e_, :, 0:n], st), reads=[sr], dma=True)

    EPS_AP = None

    def stage_win(l, tile, kinds):
        ti = tiles[tile]
        n = ti["n"]
        koff = ti["koff"]
        rope = ti["rope"]
        blks = blocks(n)
        ntt = n // 128
        stage()
        H = A.alloc([KC, n], BF16)
        Hr = Reg()
        S.add("sp", DMA(H, h_s[tile].rearrange("(kc p) t -> p kc t", p=128)), writes=[Hr], dma=True)
        ws = Rot([A.alloc([KC, 512], BF16) for _ in range(2)])
        st32 = Rot([A.alloc([n], F32) for _ in range(2)])
        st16 = Rot([A.alloc([n], BF16) for _ in range(3)])
        stv = Rot([A.alloc([512], BF16) for _ in range(3)])
        if rope and ("Q" in kinds or "K" in kinds):
            cs = A.alloc([2, n], F32)
            csr = Reg()
            S.add("sp", DMA(cs, rope_t[tile].rearrange("a p t -> p a t")), writes=[csr], dma=True)
            t1 = Rot([A.alloc([512], F32) for _ in range(2)])
            t2 = Rot([A.alloc([512], F32) for _ in range(2)])
        if "VS" in kinds:
            vg = A.alloc([ntt, 1024], F32)
            vgr = [Reg() for _ in range(ntt)]
            ngt = A.alloc([1024], F32)
            ngr = Reg()
            S.add("sp", DMA(ngt, sgu_ng[l].partition_broadcast(128)), writes=[ngr], dma=True)
            bst = A.alloc([ntt, 2, 6], F32)
            mvv = A.alloc([ntt, 4], F32)
            stn = Rot([A.alloc([1024], BF16) for _ in range(2)])
        plan = {"A": (0, 2), "U": (2, 2), "VS": (4, 2), "Q": (6, 2), "K": (8, 2), "V": (10, 2), "G": (12, 12)}
        bank_i = [0]

        def nextbank():
            b = bank_i[0] % 6
            bank_i[0] += 1
            return b

        for kind in kinds:
            b0, nbk = plan[kind]
            for bb in range(nbk):
                bi = b0 + bb
                w, wr = ws.next()
                S.add("pool", DMA(w, wsrc(w_in[l], bi * 512, (bi + 1) * 512)), writes=[wr], dma=True)
                if kind in ("VS", "V"):
                    for tt in range(ntt):
                        bank = nextbank()
                        for kc in range(KC):
                            S.add("pe", MM(PS[bank], H[:, kc, tt * 128:(tt + 1) * 128], w[:, kc, :], kc == 0, kc == KC - 1),
                                  reads=[Hr, wr], writes=[PSR[bank]])
                        if kind == "V":
                            s, sr = stv.next()
                            S.add("act", ACT(s, PS[bank], AF.Copy), reads=[PSR[bank]], writes=[sr])
                            S.add("sp", DMA(VV[l][koff + tt * 128:koff + (tt + 1) * 128, bb * 512:(bb + 1) * 512], s), reads=[sr], dma=True)
                        else:
                            S.add("act", ACT(vg[:, tt, bb * 512:(bb + 1) * 512], PS[bank], AF.Gelu), reads=[PSR[bank]], writes=[vgr[tt]])
                            S.add("dve", lambda e, a=bst[:, tt, bb, :], b=vg[:, tt, bb * 512:(bb + 1) * 512]: e.bn_stats(out=a, in_=b),
                                  reads=[vgr[tt]], writes=[vgr[tt]])
                            if bb == 1:
                                r = vgr[tt]
                                S.add("dve", lambda e, a=mvv[:, tt, 0:2], b=bst[:, tt].rearrange("p a b -> p (a b)"): e.bn_aggr(out=a, in_=b),
                                      reads=[r], writes=[r])
                                S.add("act", ACT(mvv[:, tt, 2:3], mvv[:, tt, 1:2], AF.Sqrt, bias=EPS_AP), reads=[r, cR], writes=[r])
                                S.add("dve", lambda e, a=mvv[:, tt, 3:4], b=mvv[:, tt, 2:3]: e.reciprocal(out=a, in_=b), reads=[r], writes=[r])
                                S.add("dve", TS(vg[:, tt, :], vg[:, tt, :], mvv[:, tt, 0:1], ALU.subtract, mvv[:, tt, 3:4], ALU.mult),
                                      reads=[r], writes=[r])
                                s, sr = stn.next()
                                S.add("dve", TT(s, vg[:, tt, :], ngt, ALU.mult), reads=[r, ngr], writes=[sr])
                                S.add("sp", DMA(vn_s[tt * 128:(tt + 1) * 128, :], s), reads=[sr], dma=True)
                    continue
                for c in range(4):
                    gc = bb * 4 + c
                    if kind == "A":
                        s, sr = st32.next()
                    else:
                        s, sr = st16.next()
                    for (o, nb) in blks:
                        bank = nextbank()
                        for kc in range(KC):
                            S.add("pe", MM(PS[bank][:, 0:nb], w[:, kc, c * 128:(c + 1) * 128], H[:, kc, o:o + nb], kc == 0, kc == KC - 1),
                                  reads=[Hr, wr], writes=[PSR[bank]])
                        if kind == "A":
                            S.add("act", ACT(s[:, o:o + nb], PS[bank][:, 0:nb], AF.Copy), reads=[PSR[bank]], writes=[sr])
                        elif kind == "U":
                            S.add("act", ACT(s[:, o:o + nb], PS[bank][:, 0:nb], AF.Gelu), reads=[PSR[bank]], writes=[sr])
                        elif kind == "G":
                            S.add("act", ACT(s[:, o:o + nb], PS[bank][:, 0:nb], AF.Sigmoid), reads=[PSR[bank]], writes=[sr])
                        elif not rope:
                            S.add("act", ACT(s[:, o:o + nb], PS[bank][:, 0:nb], AF.Copy), reads=[PSR[bank]], writes=[sr])
                        else:
                            a1, r1 = t1.next()
                            a2, r2 = t2.next()
                            S.add("act", ACT(a1[:, 0:nb], PS[bank][:, 0:nb], AF.Copy), reads=[PSR[bank]], writes=[r1])
                            S.add("pe", MM(PS[6][:, 0:nb], rmat32, a1[:, 0:nb]), reads=[r1, cR], writes=[PSR[6]])
                            S.add("dve", TT(a2[:, 0:nb], PS[6][:, 0:nb], cs[:, 1, o:o + nb], ALU.mult), reads=[PSR[6], csr], writes=[r2])
                            S.add("dve", TT(a1[:, 0:nb], a1[:, 0:nb], cs[:, 0, o:o + nb], ALU.mult), reads=[r1, csr], writes=[r1])
                            S.add("dve", TT(s[:, o:o + nb], a1[:, 0:nb], a2[:, 0:nb], ALU.add), reads=[r1, r2], writes=[sr])
                    if kind == "A":
                        dst = a_s[l][tile][gc * 128:(gc + 1) * 128, :]
                    elif kind == "U":
                        dst = u_s[gc * 128:(gc + 1) * 128, 0:n]
                    elif kind == "G":
                        dst = g_s[gc * 128:(gc + 1) * 128, 0:n]
                    elif kind == "Q":
                        dst = q_s[gc * 128:(gc + 1) * 128, 0:n]
                    else:
                        dst = KT[l][gc, :, koff:koff + n]
                    S.add("sp", DMA(dst, s), reads=[sr], dma=True)

    def stage_pool(l, tile):
        ti = tiles[tile]
        n = ti["n"]
        L = n + 16
        blks = blocks(n)
        stage()
        P = A.alloc([8, n], BF16)
        Pr = [Reg() for _ in range(8)]
        pw = A.alloc([4, 2, 256], BF16)
        pwr = Reg()
        for g in range(4):
            S.add("pool", DMA(pw[:, g], pool_w[l, g].rearrange("(cc p) d -> p cc d", p=128)), writes=[pwr], dma=True)
        psc = A.alloc([8], F32)
        S.add("sp", DMA(psc, pool_sc[l]), writes=[pwr], dma=True)
        pe_t = A.alloc([4, 16], F32)
        S.add("sp", DMA(pe_t, pedge[tile]), writes=[pwr], dma=True)
        ab = Rot([A.alloc([L], F32) for _ in range(2)])
        wa = Rot([A.alloc([L], F32) for _ in range(2)])
        wb = Rot([A.alloc([L], F32) for _ in range(2)])
        ed = Rot([A.alloc([16], F32) for _ in range(2)])
        nb_tile = {"own": "oth", "oth": "own"}.get(tile)
        mi = {"own": 0, "oth": 2}.get(tile, 0)
        for c in range(8):
            g = c // 2
            a, ar = ab.next()
            src = a_s[l][tile][c * 128:(c + 1) * 128, :]
            S.add("sp", DMA(a[:, 8:8 + n], src), writes=[ar], dma=True)
            if nb_tile is None:
                S.add("dve", lambda e, x=a[:, 0:8]: e.memset(x, 0.0), writes=[ar])
                S.add("dve", lambda e, x=a[:, 8 + n:L]: e.memset(x, 0.0), writes=[ar])
            else:
                nsrc = a_s[l][nb_tile][c * 128:(c + 1) * 128, :]
                S.add("sp", DMA(a[:, 0:8], nsrc[:, n - 8:n]), writes=[ar], dma=True)
                S.add("sp", DMA(a[:, 8 + n:L], nsrc[:, 0:8]), writes=[ar], dma=True)
                S.add("dve", TS(a[:, 0:8], a[:, 0:8], hmask_t[:, mi:mi + 1], ALU.mult), reads=[ar, cR], writes=[ar])
                S.add("dve", TS(a[:, 8 + n:L], a[:, 8 + n:L], hmask_t[:, mi + 1:mi + 2], ALU.mult), reads=[ar, cR], writes=[ar])
            w1, w1r = wa.next()
            w2, w2r = wb.next()
            S.add("dve", TT(w1[:, 1:L], a[:, 0:L - 1], a[:, 1:L], ALU.add), reads=[ar], writes=[w1r])
            cur, curr = w1, w1r
            if g >= 1:
                S.add("dve", TT(w2[:, 2:L - 1], w1[:, 1:L - 2], w1[:, 3:L], ALU.add), reads=[w1r], writes=[w2r])
                cur, curr = w2, w2r
            if g >= 2:
                S.add("dve", TT(w1[:, 4:L - 3], w2[:, 2:L - 5], w2[:, 6:L - 1], ALU.add), reads=[w2r], writes=[w1r])
                cur, curr = w1, w1r
            if g >= 3:
                S.add("dve", TT(w2[:, 8:L - 7], w1[:, 4:L - 11], w1[:, 12:L - 3], ALU.add), reads=[w1r], writes=[w2r])
                cur, curr = w2, w2r
            wsz = float(2 ** (g + 1))
            S.add("dve", STT(P[:, c, :], cur[:, 8:8 + n], 1.0 / wsz, a[:, 8:8 + n], ALU.mult, ALU.subtract), reads=[curr, ar], writes=[Pr[c]])
            e_, er = ed.next()
            for (lo, eo) in ((0, 0), (n - 8, 8)):
                S.add("dve", TT(e_[:, eo:eo + 8], cur[:, 8 + lo:16 + lo], pe_t[:, g, eo:eo + 8], ALU.mult), reads=[curr, pwr], writes=[er])
                S.add("dve", TT(P[:, c, lo:lo + 8], e_[:, eo:eo + 8], a[:, 8 + lo:16 + lo], ALU.subtract), reads=[er, ar, Pr[c]], writes=[Pr[c]])
        st = Rot([A.alloc([n], BF16) for _ in range(2)])
        bk = 0
        for g in range(4):
            for dd in range(2):
                s, sr = st.next()
                for (o, nb) in blks:
                    bank = bk % 4
                    bk += 1
                    for cc in range(2):
                        S.add("pe", MM(PS[bank][:, 0:nb], pw[:, g, cc, dd * 128:(dd + 1) * 128], P[:, 2 * g + cc, o:o + nb], cc == 0, cc == 1),
                              reads=[pwr, Pr[2 * g + cc]], writes=[PSR[bank]])
                    S.add("act", ACT(s[:, o:o + nb], PS[bank][:, 0:nb], AF.Copy, scale=psc[:, 2 * g + dd:2 * g + dd + 1]), reads=[PSR[bank], pwr], writes=[sr])
                S.add("sp", DMA(ao_s[(2 * g + dd) * 128:(2 * g + dd + 1) * 128, 0:n], s), reads=[sr], dma=True)

    def stage_sgu(l, tile):
        n = tiles[tile]["n"]
        ntt = n // 128
        stage()
        U = A.alloc([8, n], BF16)
        Ur = Reg()
        S.add("sp", DMA(U, u_s[:, 0:n].rearrange("(g p) t -> p g t", p=128)), writes=[Ur], dma=True)
        wt = A.alloc([8, 128], BF16)
        S.add("pool", DMA(wt, sgu_wT[l].rearrange("g q p -> q g p")), writes=[Ur], dma=True)
        bt = A.alloc([8, 128], F32)
        S.add("sp", DMA(bt, sgu_b[l].partition_broadcast(128).rearrange("p (g q) -> p g q", g=8)), writes=[Ur], dma=True)
        vn = Rot([A.alloc([1024], BF16) for _ in range(2)])
        tp = Rot([A.alloc([4, 128], F32) for _ in range(2)])
        BO = A.alloc([8, n], BF16)
        BOr = Reg()
        for tt in range(ntt):
            v, vr = vn.next()
            S.add("sp", DMA(v, vn_s[tt * 128:(tt + 1) * 128, :]), writes=[vr], dma=True)
            for gh in range(2):
                bank = (2 * tt + gh) % 4
                for gg in range(4):
                    g = gh * 4 + gg
                    S.add("pe", MM(PS[bank][:, gg * 128:(gg + 1) * 128], v[:, g * 128:(g + 1) * 128], wt[:, g, :]), reads=[vr, Ur], writes=[PSR[bank]])
                t, tr = tp.next()
                S.add("dve", TT(t, PS[bank].rearrange("p (a b) -> p a b", a=4), bt[:, gh * 4:(gh + 1) * 4, :], ALU.add), reads=[PSR[bank], Ur], writes=[tr])
                S.add("dve", TT(BO[:, gh * 4:(gh + 1) * 4, tt * 128:(tt + 1) * 128], t, U[:, gh * 4:(gh + 1) * 4, tt * 128:(tt + 1) * 128], ALU.mult),
                      reads=[tr, Ur], writes=[BOr])
        S.add("sp", DMA(bo_s[:, 0:n].rearrange("(g p) t -> p g t", p=128), BO), reads=[BOr], dma=True)

    def stage_att(l, tile):
        ti = tiles[tile]
        n = ti["n"]
        blks = blocks(n)
        if tile == "ctx":
            k0, nk = 2 * T, CTX
        else:
            k0, nk = 0, NK
        nkc = nk // 128
        stage()
        qb = Rot([A.alloc([n], BF16) for _ in range(2)])
        kb = Rot([A.alloc([nk], BF16) for _ in range(2)])
        vb = Rot([A.alloc([nkc, 128], BF16) for _ in range(2)])
        e2 = Rot([A.alloc([2, 512], BF16) for _ in range(3)])
        rc = A.alloc([2, 512], F32)
        oc = A.alloc([2, 512], F32)
        o1 = A.alloc([512], F32)
        osq = A.alloc([512], F32)
        rs = A.alloc([512], F32)
        fo, frc, fo1, frs = Reg(), Reg(), Reg(), Reg()
        cst = Rot([A.alloc([n], BF16) for _ in range(2)])
        items = [(h, o, nb) for h in range(NH) for (o, nb) in blks]
        loaded = {}

        def pair(p, nb):
            return psall[:, 2 * p * 512:(2 * p + 2) * 512].rearrange("p (a b) -> p a b", a=2)[:, :, 0:nb]

        def load_head(h):
            q, qr = qb.next()
            k, kr = kb.next()
            v, vr = vb.next()
            S.add("sp", DMA(q, q_s[h * 128:(h + 1) * 128, 0:n]), writes=[qr], dma=True)
            S.add("sp", DMA(k, KT[l][h, :, k0:k0 + nk]), writes=[kr], dma=True)
            S.add("sp", DMA(v, VV[l][k0:k0 + nk, h * 128:(h + 1) * 128].rearrange("(c p) d -> p c d", p=128)), writes=[vr], dma=True)
            loaded[h] = (q, qr, k, kr, v, vr)

        def emit_S(h, o, nb, kc):
            q, qr, k, kr, v, vr = loaded[h]
            for i in range(2):
                bank = 2 * (kc % 2) + i
                S.add("pe", MM(PS[bank][:, 0:nb], k[i * 64:(i + 1) * 64, kc * 128:(kc + 1) * 128], q[i * 64:(i + 1) * 64, o:o + nb]),
                      reads=[kr, qr], writes=[PSR[bank]])

        load_head(0)
        cs_, csr = None, None
        for idx, (h, o, nb) in enumerate(items):
            if o == 0:
                if h + 1 < NH:
                    load_head(h + 1)
                cs_, csr = cst.next()
            q, qr, k, kr, v, vr = loaded[h]
            if idx == 0:
                emit_S(h, o, nb, 0)
            for kc in range(nkc):
                if kc + 1 < nkc:
                    emit_S(h, o, nb, kc + 1)
                p = kc % 2
                e_, er = e2.next()
                S.add("act", ACT(e_[:, :, 0:nb], pair(p, nb), AF.Exp, scale=0.125), reads=[PSR[2 * p], PSR[2 * p + 1]], writes=[er])
                for i in range(2):
                    S.add("pe", MM(PS[4 + i][:, 0:nb], v[:, kc, :], e_[:, i, 0:nb], kc == 0, kc == nkc - 1), reads=[vr, er], writes=[PSR[4 + i]])
                    S.add("pe", MM(PS[6 + i][:, 0:nb], onesbf, e_[:, i, 0:nb], kc == 0, kc == nkc - 1), reads=[cR, er], writes=[PSR[6 + i]])
            if idx + 1 < len(items):
                emit_S(items[idx + 1][0], items[idx + 1][1], items[idx + 1][2], 0)
            S.add("dve", CP(oc[:, :, 0:nb], pair(2, nb)), reads=[PSR[4], PSR[5]], writes=[fo])
            S.add("act", ACT(rc[:, :, 0:nb], pair(3, nb), AF.Ln), reads=[PSR[6], PSR[7]], writes=[frc])
            S.add("act", ACT(rc[:, :, 0:nb], rc[:, :, 0:nb], AF.Exp, scale=-1.0), reads=[frc], writes=[frc])
            S.add("dve", TT(oc[:, :, 0:nb], oc[:, :, 0:nb], rc[:, :, 0:nb], ALU.mult), reads=[fo, frc], writes=[fo])
            S.add("dve", STT(o1[:, 0:nb], oc[:, 1, 0:nb], lam_t[:, l, 0:1], oc[:, 0, 0:nb], ALU.mult, ALU.add), reads=[fo, cR], writes=[fo1])
            S.add("dve", TT(osq[:, 0:nb], o1[:, 0:nb], o1[:, 0:nb], ALU.mult), reads=[fo1], writes=[fo1])
            S.add("pe", MM(PS[2][:, 0:nb], ones32, osq[:, 0:nb]), reads=[fo1, cR], writes=[PSR[2]])
            S.add("act", ACT(rs[:, 0:nb], PS[2][:, 0:nb], AF.Ln, bias=EPS_AP, scale=1.0 / 128), reads=[PSR[2], cR], writes=[frs])
            S.add("act", ACT(rs[:, 0:nb], rs[:, 0:nb], AF.Exp, scale=-0.5), reads=[frs], writes=[frs])
            S.add("dve", STT(cs_[:, o:o + nb], o1[:, 0:nb], subg_t[:, l:l + 1], rs[:, 0:nb], ALU.mult, ALU.mult), reads=[fo1, frs, cR], writes=[csr])
            if o + nb >= n:
                S.add("sp", DMA(co_s[h * 128:(h + 1) * 128, 0:n], cs_), reads=[csr], dma=True)

    def stage_merge(l, tile):
        ti = tiles[tile]
        n = ti["n"]
        mc = ti["mc"]
        blks = blocks(n)
        stage()
        BR = []
        Rr = Reg()
        for src in (ao_s, bo_s, co_s):
            b = A.alloc([8, n], BF16)
            S.add("sp", DMA(b, src[:, 0:n].rearrange("(g p) t -> p g t", p=128)), writes=[Rr], dma=True)
            BR.append(b)
        M = A.alloc([KC, n], BF16)
        Mr = [Reg() for _ in range(KC)]
        wp = Rot([A.alloc([3, 8, 128], BF16) for _ in range(3)])
        gb = Rot([A.alloc([3, n], BF16) for _ in range(3)])
        tm = Rot([A.alloc([512], F32) for _ in range(2)])
        tm2 = Rot([A.alloc([512], F32) for _ in range(2)])
        bk = 0
        for j in range(KC):
            w, wr = wp.next()
            for xi, wsrc_ in enumerate((w_pa, w_pb, w_pc)):
                S.add("pool", DMA(w[:, xi], wsrc_[l].rearrange("(kc p) d -> p kc d", p=128)[:, :, j * 128:(j + 1) * 128]), writes=[wr], dma=True)
            g, gr = gb.next()
            S.add("sp", DMA(g, g_s[:, 0:n].rearrange("(x j p) t -> j p x t", x=3, p=128)[j]), writes=[gr], dma=True)
            for (o, nb) in blks:
                t, tr = tm.next()
                t2, t2r = tm2.next()
                for xi in range(3):
                    bank = bk % 6
                    bk += 1
                    for kc in range(8):
                        S.add("pe", MM(PS[bank][:, 0:nb], w[:, xi, kc, :], BR[xi][:, kc, o:o + nb], kc == 0, kc == 7), reads=[wr, Rr], writes=[PSR[bank]])
                    if xi == 0:
                        S.add("dve", TT(t[:, 0:nb], PS[bank][:, 0:nb], g[:, 0, o:o + nb], ALU.mult), reads=[PSR[bank], gr], writes=[tr])
                    elif xi == 1:
                        S.add("dve", TT(t2[:, 0:nb], PS[bank][:, 0:nb], g[:, 1, o:o + nb], ALU.mult), reads=[PSR[bank], gr], writes=[t2r])
                        S.add("dve", TT(t[:, 0:nb], t[:, 0:nb], t2[:, 0:nb], ALU.add), reads=[tr, t2r], writes=[tr])
                    else:
                        S.add("dve", TT(t2[:, 0:nb], PS[bank][:, 0:nb], g[:, 2, o:o + nb], ALU.mult), reads=[PSR[bank], gr, tr], writes=[t2r])
                        S.add("dve", TT(M[:, j, o:o + nb], t[:, 0:nb], t2[:, 0:nb], ALU.add), reads=[tr, t2r], writes=[Mr[j]])
        ws = Rot([A.alloc([KC, 512], BF16) for _ in range(2)])
        xr = Rot([A.alloc([n], F32) for _ in range(3)])
        gt = modv[:, l, mc, 2, :]
        src = xcur[tile]
        for bi in range(4):
            w, wr = ws.next()
            S.add("pool", DMA(w, wsrc(w_o[l], bi * 512, (bi + 1) * 512)), writes=[wr], dma=True)
            for c in range(4):
                jj = bi * 4 + c
                x, xreg = xr.next()
                S.add("sp", DMA(x, src[jj * 128:(jj + 1) * 128, :]), writes=[xreg], dma=True)
                for (o, nb) in blks:
                    bank = bk % 6
                    bk += 1
                    for kc in range(KC):
                        S.add("pe", MM(PS[bank][:, 0:nb], w[:, kc, c * 128:(c + 1) * 128], M[:, kc, o:o + nb], kc == 0, kc == KC - 1),
                              reads=[wr, Mr[kc]], writes=[PSR[bank]])
                    S.add("dve", STT(x[:, o:o + nb], PS[bank][:, 0:nb], gt[:, jj:jj + 1], x[:, o:o + nb], ALU.mult, ALU.add), reads=[PSR[bank], xreg, cR], writes=[xreg])
                S.add("sp", DMA(xs[tile][jj * 128:(jj + 1) * 128, :], x), reads=[xreg], dma=True)
        xcur[tile] = xs[tile]

    def stage_ffn(l, tile, moe, final, store_x=True):
        ti = tiles[tile]
        n = ti["n"]
        mc = ti["mc"]
        blks = blocks(n)
        stage()
        X = A.alloc([KC, n], F32)
        Xr = [[Reg() for _ in blks] for _ in range(KC)]
        for kc in range(KC):
            S.add("sp", DMA(X[:, kc, :], xcur[tile][kc * 128:(kc + 1) * 128, :]), writes=Xr[kc], dma=True)
        H2 = A.alloc([KC, n], BF16)
        Hr = Reg()
        S.add("sp", DMA(H2, h2_s[:, 0:n].rearrange("(kc p) t -> p kc t", p=128)), writes=[Hr], dma=True)
        FG = 2
        w1b = Rot([A.alloc([KC, FG * 128], BF16) for _ in range(2)])
        w3b = Rot([A.alloc([KC, FG * 128], BF16) for _ in range(2)])
        w2b = Rot([A.alloc([FG, D], BF16) for _ in range(3)])
        Gb = Rot([A.alloc([FG, n], BF16) for _ in range(2)])
        sb = Rot([A.alloc([512], F32) for _ in range(2)])
        cbuf = Rot([A.alloc([n], F32) for _ in range(2)]) if moe else None
        gt = modv[:, l, mc, 5, :]
        F = DFFE if moe else DFF
        nfc = F // 128
        assert nfc % FG == 0
        bk = [0]
        groups = [(e_, fg) for e_ in range(NE if moe else 1) for fg in range(nfc // FG)]
        wl = {}
        cbs = {}

        def load_w(gi):
            e_, fg = groups[gi]
            W1 = moe_w1[e_] if moe else ffn_w1[0]
            W3 = moe_w3[e_] if moe else ffn_w3[0]
            W2 = moe_w2[e_] if moe else ffn_w2[0]
            f0 = fg * FG * 128
            w1, w1r = w1b.next()
            w3, w3r = w3b.next()
            w2, w2r = w2b.next()
            S.add("pool", DMA(w1, wsrc(W1, f0, f0 + FG * 128)), writes=[w1r], dma=True)
            S.add("pool", DMA(w3, wsrc(W3, f0, f0 + FG * 128)), writes=[w3r], dma=True)
            S.add("pool", DMA(w2, W2[f0:f0 + FG * 128, :].rearrange("(fc p) d -> p fc d", p=128)), writes=[w2r], dma=True)
            wl[gi] = (w1, w1r, w3, w3r, w2, w2r)
            if moe and fg == 0:
                cb, cbr = cbuf.next()
                S.add("sp", DMA(cb, comb_s[e_, :, 0:n]), writes=[cbr], dma=True)
                cbs[e_] = (cb, cbr)

        Gs = {}

        def up_parts(gi):
            e_, fg = groups[gi]
            w1, w1r, w3, w3r, w2, w2r = wl[gi]
            G, Gr = Gb.next()
            Gs[gi] = (G, Gr)
            parts = []
            for fc in range(FG):
                for (o, nb) in blks:
                    def part(fc=fc, o=o, nb=nb):
                        b1 = bk[0] % 4
                        b3 = (bk[0] + 1) % 4
                        bk[0] += 2
                        for kc in range(KC):
                            S.add("pe", MM(PS[b1][:, 0:nb], w1[:, kc, fc * 128:(fc + 1) * 128], H2[:, kc, o:o + nb], kc == 0, kc == KC - 1),
                                  reads=[w1r, Hr], writes=[PSR[b1]])
                        for kc in range(KC):
                            S.add("pe", MM(PS[b3][:, 0:nb], w3[:, kc, fc * 128:(fc + 1) * 128], H2[:, kc, o:o + nb], kc == 0, kc == KC - 1),
                                  reads=[w3r, Hr], writes=[PSR[b3]])
                        s_, sr = sb.next()
                        S.add("act", ACT(s_[:, 0:nb], PS[b1][:, 0:nb], AF.Silu), reads=[PSR[b1]], writes=[sr])
                        if moe:
                            cb, cbr = cbs[e_]
                            S.add("dve", TT(s_[:, 0:nb], s_[:, 0:nb], PS[b3][:, 0:nb], ALU.mult), reads=[sr, PSR[b3]], writes=[sr])
                            S.add("dve", TT(G[:, fc, o:o + nb], s_[:, 0:nb], cb[:, o:o + nb], ALU.mult), reads=[sr, cbr], writes=[Gr])
                        else:
                            S.add("dve", TT(G[:, fc, o:o + nb], s_[:, 0:nb], PS[b3][:, 0:nb], ALU.mult), reads=[sr, PSR[b3]], writes=[Gr])
                    parts.append(part)
            return parts

        def down_parts(gi, nparts):
            w1, w1r, w3, w3r, w2, w2r = wl[gi]
            G, Gr = Gs[gi]
            units = [(jj, bi, o, nb) for jj in range(KC) for bi, (o, nb) in enumerate(blks)]
            per = (len(units) + nparts - 1) // nparts
            parts = []
            for k in range(nparts):
                def part(us=units[k * per:(k + 1) * per]):
                    for (jj, bi, o, nb) in us:
                        bank = 4 + (jj * len(blks) + bi) % 4
                        for fc in range(FG):
                            S.add("pe", MM(PS[bank][:, 0:nb], w2[:, fc, jj * 128:(jj + 1) * 128], G[:, fc, o:o + nb], fc == 0, fc == FG - 1),
                                  reads=[w2r, Gr], writes=[PSR[bank]])
                        S.add("dve", STT(X[:, jj, o:o + nb], PS[bank][:, 0:nb], gt[:, jj:jj + 1], X[:, jj, o:o + nb], ALU.mult, ALU.add),
                              reads=[PSR[bank], Xr[jj][bi], cR], writes=[Xr[jj][bi]])
                parts.append(part)
            return parts

        load_w(0)
        for gi in range(len(groups)):
            if gi + 1 < len(groups):
                load_w(gi + 1)
            ups = up_parts(gi)
            downs = down_parts(gi - 1, len(ups)) if gi > 0 else [None] * len(ups)
            for k in range(len(ups)):
                ups[k]()
                if downs[k] is not None:
                    downs[k]()
        for p_ in down_parts(len(groups) - 1, 1):
            p_()
        acc = A.alloc([n], F32)
        accr = [Reg() for _ in blks]
        if moe:
            rstd, creg0 = cbuf.items[0]
            extra = [creg0]
        else:
            rstd = A.alloc([n], F32)
            extra = []
        rr = [Reg() for _ in blks]
        for kc in range(KC):
            for bi, (o, nb) in enumerate(blks):
                if kc == 0:
                    S.add("act", ACT(acc[:, o:o + nb], X[:, kc, o:o + nb], AF.Square), reads=[Xr[kc][bi]], writes=[accr[bi]])
                else:
                    s_, sr = sb.next()
                    S.add("act", ACT(s_[:, 0:nb], X[:, kc, o:o + nb], AF.Square), reads=[Xr[kc][bi]], writes=[sr])
                    S.add("dve", TT(acc[:, o:o + nb], acc[:, o:o + nb], s_[:, 0:nb], ALU.add), reads=[sr, accr[bi]], writes=[accr[bi]])
        for bi, (o, nb) in enumerate(blks):
            S.add("pe", MM(PS[bi][:, 0:nb], ones32, acc[:, o:o + nb]), reads=[accr[bi], cR], writes=[PSR[bi]])
            s_, sr = sb.next()
            S.add("act", ACT(s_[:, 0:nb], PS[bi][:, 0:nb], AF.Sqrt, bias=EPS_AP, scale=1.0 / D), reads=[PSR[bi], cR], writes=[sr])
            S.add("dve", lambda e, a=rstd[:, o:o + nb], b=s_[:, 0:nb]: e.reciprocal(out=a, in_=b), reads=[sr], writes=[rr[bi]] + extra)
        if not final:
            if store_x:
                for kc in range(KC):
                    S.add("sp", DMA(xs[tile][kc * 128:(kc + 1) * 128, :], X[:, kc, :]), reads=Xr[kc], dma=True)
                xcur[tile] = xs[tile]
            Av = modv[:, l + 1, mc, 0, :]
            Bv = modv[:, l + 1, mc, 1, :]
            for kc in range(KC):
                for bi, (o, nb) in enumerate(blks):
                    s_, sr = sb.next()
                    S.add("dve", STT(s_[:, 0:nb], X[:, kc, o:o + nb], Av[:, kc:kc + 1], rstd[:, o:o + nb], ALU.mult, ALU.mult),
                          reads=[Xr[kc][bi], rr[bi], cR], writes=[sr])
                    S.add("act", ACT(H2[:, kc, o:o + nb], s_[:, 0:nb], AF.Identity, bias=Bv[:, kc:kc + 1]), reads=[sr, cR], writes=[Hr])
            S.add("sp", DMA(h_s[tile].rearrange("(kc p) t -> p kc t", p=128), H2), reads=[Hr], dma=True)
            return
        for kc in range(KC):
            for bi, (o, nb) in enumerate(blks):
                S.add("dve", STT(X[:, kc, o:o + nb], X[:, kc, o:o + nb], fng_t[:, kc:kc + 1], rstd[:, o:o + nb], ALU.mult, ALU.mult),
                      reads=[Xr[kc][bi], rr[bi], cR], writes=[Xr[kc][bi]])
            S.add("sp", DMA(outT[kc * 128:(kc + 1) * 128, :], X[:, kc, :]), reads=Xr[kc], dma=True)

    A.base = A.off
    eps_t = A.alloc([1], F32)
    S.add("dve", lambda e: e.memset(eps_t, EPS), writes=[cR])
    EPS_AP = eps_t
    A.base = A.off

    upto = cfg.get("upto", 99)
    stage_mod()
    step = [0]

    def go(fn, *a, **k):
        step[0] += 1
        if step[0] <= upto:
            fn(*a, **k)

    for t in ("oth", "ctx", "own"):
        go(stage_norm, 0, t, 0, h_s[t])
        go(stage_win, 0, t, ["K", "V", "A"])
    for t in ("oth", "ctx", "own"):
        go(stage_win, 0, t, ["U", "VS", "Q", "G"])
        go(stage_pool, 0, t)
        go(stage_sgu, 0, t)
        go(stage_att, 0, t)
        go(stage_merge, 0, t)
        go(stage_norm, 0, t, 1, h2_s[:, 0:tiles[t]["n"]])
        go(stage_ffn, 0, t, False, False, store_x=(t == "own"))
        go(stage_win, 1, t, ["K", "V", "A"] if t != "ctx" else ["K", "V"])
    t = "own"
    go(stage_win, 1, t, ["U", "VS", "Q", "G"])
    go(stage_pool, 1, t)
    go(stage_sgu, 1, t)
    go(stage_att, 1, t)
    go(stage_merge, 1, t)
    go(stage_norm, 1, t, 1, h2_s[:, 0:T], moe=True)
    go(stage_ffn, 1, t, True, True)
    S.barrier()

    S.finalize()
    with nc.Block() as block:
        @block.tensor
        def _(e):
            S.emit("pe", e, csem, dsem)

        @block.scalar
        def _(e):
            S.emit("act", e, csem, dsem)

        @block.vector
        def _(e):
            S.emit("dve", e, csem, dsem)

        @block.gpsimd
        def _(e):
            S.emit("pool", e, csem, dsem)

        @block.sync
        def _(e):
            S.emit("sp", e, csem, dsem)
    for cm in reversed(ctxs):
        cm.__exit__(None, None, None)
    return nc


def _fm(v):
    v = np.asarray(v)
    n = v.shape[-1] // 128
    return np.ascontiguousarray(np.swapaxes(v.reshape(v.shape[:-1] + (n, 128)), -1, -2))


def _consts(cfg, s):
    SEQ, CTX = cfg["SEQ"], cfg["CTX"]
    T = SEQ // 2
    half = 32
    inv = np.power(10000.0, -np.arange(0, half, 2, dtype=np.float32) / half).astype(np.float32)
    out = {}
    for name, t0 in (("rope_own", s * T), ("rope_oth", (1 - s) * T)):
        t = np.arange(t0, t0 + T)
        row = (t // 64).astype(np.float32)
        col = (t % 64).astype(np.float32)
        ar = row[:, None] * inv[None, :]
        ac = col[:, None] * inv[None, :]
        ang = np.concatenate([ar, ar, ac, ac], axis=-1).astype(np.float32)
        cs = np.stack([np.cos(ang), np.sin(ang)]).astype(np.float32)
        cs = np.transpose(cs, (0, 2, 1))
        out[name] = np.ascontiguousarray(np.concatenate([cs, cs], axis=1))
    cm = np.zeros((3, 128, 128), np.float32)
    cm[0] = 1.0
    cm[1] = np.eye(128, dtype=np.float32)
    for m in range(128):
        if m % 32 < 16:
            cm[2, m + 16, m] = -1.0
        else:
            cm[2, m - 16, m] = 1.0
    out["cmat"] = cm
    hm = np.zeros((128, 4), np.float32)
    hm[:, 0], hm[:, 1] = float(s == 1), float(s == 0)
    hm[:, 2], hm[:, 3] = float(s == 0), float(s == 1)
    out["hmask"] = hm

    def edges(t0, n, Sq):
        e = np.zeros((128, 4, 16), np.float32)
        for g, w in enumerate((2, 4, 8, 16)):
            for idx, t in enumerate(list(range(t0, t0 + 8)) + list(range(t0 + n - 8, t0 + n))):
                lo = min(max(t - w // 2, 0), Sq)
                hi = min(max(t + w - w // 2, 0), Sq)
                e[:, g, idx] = 1.0 / float(hi - lo)
        return e
    out["pedge_own"] = edges(s * T, T, SEQ)
    out["pedge_oth"] = edges((1 - s) * T, T, SEQ)
    out["pedge_ctx"] = edges(0, CTX, CTX)
    return out


def prep_inputs(inp, cfg):
    SEQ = cfg["SEQ"]
    T = SEQ // 2
    f32 = lambda a: np.ascontiguousarray(np.asarray(a, dtype=np.float32))
    shared = dict(
        w_mod=f32(inp["w_mod"]), b_mod=_fm(f32(inp["b_mod"])), n1g=_fm(f32(inp["norm1_g"])), n2g=_fm(f32(inp["norm2_g"])),
        fng=_fm(f32(inp["final_norm_g"])), w_in=f32(inp["w_in"]), pool_w=f32(inp["pool_w"]), pool_sc=_fm(f32(inp["pool_scale"])),
        sgu_ng=f32(inp["sgu_norm_g"]), sgu_wT=np.ascontiguousarray(np.swapaxes(f32(inp["sgu_w"]), -1, -2)),
        sgu_b=f32(inp["sgu_b"]).reshape(2, 1024),
        lamv=np.ascontiguousarray(np.stack([f32(inp["lambda_q1"]), f32(inp["lambda_k1"]), f32(inp["lambda_q2"]), f32(inp["lambda_k2"])], axis=1)),
        subln=f32(inp["attn_subln_g"]).reshape(2, 128, 1),
        w_pa=f32(inp["w_proj_a"]), w_pb=f32(inp["w_proj_b"]), w_pc=f32(inp["w_proj_c"]), w_o=f32(inp["w_o"]),
        ffn_w1=f32(inp["ffn_w1"]), ffn_w3=f32(inp["ffn_w3"]), ffn_w2=f32(inp["ffn_w2"]),
        router=np.ascontiguousarray(np.transpose(f32(inp["moe_router"])[0].reshape(KC, 128, NE), (1, 0, 2))),
        moe_w1=f32(inp["moe_w1"])[0], moe_w3=f32(inp["moe_w3"])[0], moe_w2=f32(inp["moe_w2"])[0],
    )
    x = f32(inp["x"])
    ctx = f32(inp["ctx"])
    c = f32(inp["c"])
    cc = f32(inp["c_ctx"])
    maps = []
    for core in range(8):
        b, s = core // 2, core % 2
        m = dict(shared)
        m["xT_own"] = np.ascontiguousarray(x[b, s * T:(s + 1) * T, :].T)
        m["xT_oth"] = np.ascontiguousarray(x[b, (1 - s) * T:(2 - s) * T, :].T)
        m["xT_ctx"] = np.ascontiguousarray(ctx[b].T)
        m["cvec"] = np.ascontiguousarray(np.stack([_fm(c[b]), _fm(cc)], axis=-1))
        m.update(_consts(cfg, s))
        maps.append(m)
    return maps


def assemble(results, cfg):
    SEQ = cfg["SEQ"]
    T = SEQ // 2
    out = np.zeros((4, SEQ, D), np.float32)
    for core in range(8):
        b, s = core // 2, core % 2
        out[b, s * T:(s + 1) * T, :] = np.asarray(results[core]["outT"]).T
    return out


def kernel(**inputs):
    cfg = FULL_CFG
    nc = build(cfg)
    maps = prep_inputs(inputs, cfg)
    res = run_bass_kernel_spmd(nc, maps, core_ids=list(range(8)))
    return assemble(res.results, cfg)
```
